# Optimizing a Trainium2 kernel written in Bass

```python
import math
import jax, jax.numpy as jnp
from jax import lax
import numpy as np

D_MODEL = 1024
BATCH = 8
SEQ = 4096
DEPTH = 1

N_META = 16
EPS = 1e-6

D_HY = 512
HY_GROUPS = 8
HY_SHORT = 3
HY_EMB = 33
HY_BANDS = (HY_EMB - 1) // 2
HY_FFN = 64
HY_TARGET = 1e-2
HY_DECAY_HI = 0.3
HY_DECAY_LO = 1.5

D_RG = 1024
RG_HEADS = 8
RG_HEAD_DIM = D_RG // RG_HEADS
RG_CONV = 4
RG_C = 8.0

PEER_HEADS = 8
N_KEYS = 128
N_EXPERTS = N_KEYS * N_KEYS
D_KEY = 256
D_HALF = D_KEY // 2
PEER_TOPK = 16
PEER_CHUNK = 16

D_IN = 3 * D_HY + 2 * D_RG + 2 * D_MODEL

kernel_name = 'hybrid_hyena_rglru_peer_encoder'


def rmsnorm(x, g):
    xf = x.astype(jnp.float32)
    y = xf * lax.rsqrt(jnp.mean(xf * xf, axis=-1, keepdims=True) + EPS)
    return (y * g.astype(jnp.float32)).astype(x.dtype)


def hyena_filter(L, w1, b1, w2, b2, w3, b3, freq):
    f32 = jnp.float32
    t = jnp.linspace(0.0, 1.0, L, dtype=f32)[:, None]
    w = (2.0 * math.pi / L) * jnp.arange(L, dtype=f32)[:, None]
    bands = jnp.linspace(1e-4, HY_BANDS - 1, HY_BANDS, dtype=f32)[None, :]
    z = jnp.concatenate([t, jnp.cos(bands * w), -jnp.sin(bands * w)], axis=-1)
    freq = freq.astype(f32)
    hdn = jnp.sin(freq[0] * (z @ w1.astype(f32) + b1.astype(f32)))
    hdn = jnp.sin(freq[1] * (hdn @ w2.astype(f32) + b2.astype(f32)))
    k = (hdn @ w3.astype(f32) + b3.astype(f32)).reshape(L, 2, D_HY)
    deltas = jnp.abs(jnp.linspace(math.log(HY_TARGET) / HY_DECAY_HI,
                                  math.log(HY_TARGET) / HY_DECAY_LO, D_HY, dtype=f32))
    k = k * jnp.exp(-t * deltas)[:, None, :]
    k_fwd, k_bwd = k[:, 0], k[:, 1]
    return jnp.concatenate([k_fwd, jnp.zeros((1, D_HY), f32), k_bwd[:0:-1]], axis=0)


def fft_long_conv(u, k_full, skip):
    L = u.shape[1]
    uf = u.astype(jnp.float32)
    U = jnp.fft.rfft(uf, n=2 * L, axis=1)
    K = jnp.fft.rfft(k_full, axis=0)
    y = jnp.fft.irfft(U * K[None], n=2 * L, axis=1)[:, :L]
    return (y + uf * skip.astype(jnp.float32)).astype(u.dtype)


def hyena_branch(hy_in, conv_w, conv_b, w1, b1, w2, b2, w3, b3, freq, skip, w_proj):
    B, L, _ = hy_in.shape
    hp = jnp.pad(hy_in, ((0, 0), (1, 1), (0, 0)))
    u = hp[:, 0:L] * conv_w[0] + hp[:, 1:L + 1] * conv_w[1] + hp[:, 2:L + 2] * conv_w[2] + conv_b
    x0, x1, v = u[..., :D_HY], u[..., D_HY:2 * D_HY], u[..., 2 * D_HY:]
    k_full = hyena_filter(L, w1, b1, w2, b2, w3, b3, freq)
    y = fft_long_conv(v * x1, k_full, skip) * x0
    return y @ w_proj


def rglru_direction(x, conv_w, conv_b, gate_w, gate_b, lam):
    B, L, _ = x.shape
    xp = jnp.pad(x, ((0, 0), (RG_CONV - 1, 0), (0, 0)))
    xc = conv_b + sum(xp[:, RG_CONV - 1 - j:RG_CONV - 1 - j + L] * conv_w[j] for j in range(RG_CONV))
    xh = xc.reshape(B, L, RG_HEADS, RG_HEAD_DIM)
    gates = jnp.einsum('blhi,ghij->gblhj', xh, gate_w).reshape(2, B, L, D_RG)
    gates = gates.astype(jnp.float32) + gate_b.astype(jnp.float32)[:, None, None, :]
    r = jax.nn.sigmoid(gates[0])
    i = jax.nn.sigmoid(gates[1])
    log_a = -RG_C * r * jax.nn.softplus(-lam.astype(jnp.float32))
    a = jnp.exp(log_a)
    mult = jnp.sqrt(-jnp.expm1(2.0 * log_a))
    mult = mult.at[:, 0].set(1.0)
    b = mult * i * xc.astype(jnp.float32)

    def combine(p, q):
        a1, b1 = p
        a2, b2 = q
        return a1 * a2, a2 * b1 + b2

    _, h = lax.associative_scan(combine, (a, b), axis=1)
    return h.astype(x.dtype)


def rglru_branch(rg_x, rg_g, conv_w, conv_b, gate_w, gate_b, lam, w_proj):
    h_f = rglru_direction(rg_x, conv_w[0], conv_b[0], gate_w[0], gate_b[0], lam[0])
    h_b = rglru_direction(rg_x[:, ::-1], conv_w[1], conv_b[1], gate_w[1], gate_b[1], lam[1])[:, ::-1]
    y = (h_f + h_b) * jax.nn.gelu(rg_g)
    return y @ w_proj


def peer(x, wq, keys, u_tab, v_tab):
    B, L, D = x.shape
    K = PEER_TOPK
    q = (x @ wq).reshape(B, L, PEER_HEADS, 2, D_HALF)
    s = jnp.einsum('blhpd,hpkd->blhpk', q, keys).astype(jnp.float32)
    ts, ti = lax.top_k(s, K)
    cand = ts[..., 0, :, None] + ts[..., 1, None, :]
    cs, ci = lax.top_k(cand.reshape(B, L, PEER_HEADS, K * K), K)
    e1 = jnp.take_along_axis(ti[..., 0, :], ci // K, axis=-1)
    e2 = jnp.take_along_axis(ti[..., 1, :], ci % K, axis=-1)
    experts = e1 * N_KEYS + e2
    g = jax.nn.softmax(cs, axis=-1).astype(x.dtype)

    nc = L // PEER_CHUNK
    hk = PEER_HEADS * K

    def to_chunks(a, last):
        return a.reshape(B, nc, PEER_CHUNK, last).transpose(1, 0, 2, 3).reshape(nc, B * PEER_CHUNK, last)

    xs = (to_chunks(x, D), to_chunks(experts.reshape(B, L, hk), hk), to_chunks(g.reshape(B, L, hk), hk))

    def chunk_fn(args):
        xt, et, gt = args
        act = jax.nn.gelu(jnp.einsum('td,tkd->tk', xt, u_tab[et])) * gt
        return jnp.einsum('tk,tkd->td', act, v_tab[et])

    out = lax.map(chunk_fn, xs)
    return out.reshape(nc, B, PEER_CHUNK, D).transpose(1, 0, 2, 3).reshape(B, L, D)


def setup_inputs(seed: int = 0) -> dict:
    key = jax.random.key(seed)
    ks = iter(jax.random.split(key, 40))
    f32 = jnp.float32

    def nrm(shape, scale):
        return jax.random.normal(next(ks), shape, f32) * scale

    L_TOT = SEQ + N_META
    x = nrm((BATCH, SEQ, D_MODEL), 1.0)
    meta = nrm((N_META, D_MODEL), 1.0)
    g_final = 1.0 + nrm((D_MODEL,), 0.02)
    g_mix = 1.0 + nrm((DEPTH, D_MODEL), 0.02)
    w_in = nrm((DEPTH, D_MODEL, D_IN), D_MODEL ** -0.5)
    b_gate = nrm((DEPTH, 2 * D_MODEL), 0.02)
    hy_conv_w = nrm((DEPTH, HY_SHORT, 3 * D_HY), HY_SHORT ** -0.5)
    hy_conv_b = nrm((DEPTH, 3 * D_HY), 0.02)
    hy_w1 = nrm((DEPTH, HY_EMB, HY_FFN), HY_EMB ** -0.5)
    hy_b1 = nrm((DEPTH, HY_FFN), 0.1)
    hy_w2 = nrm((DEPTH, HY_FFN, HY_FFN), HY_FFN ** -0.5)
    hy_b2 = nrm((DEPTH, HY_FFN), 0.1)
    hy_w3 = nrm((DEPTH, HY_FFN, 2 * D_HY), (HY_FFN * L_TOT / 4.0) ** -0.5)
    hy_b3 = nrm((DEPTH, 2 * D_HY), L_TOT ** -0.5)
    hy_freq = 1.0 + nrm((DEPTH, 2, HY_FFN), 0.02)
    hy_skip = nrm((DEPTH, D_HY), 1.0)
    hy_out = nrm((DEPTH, D_HY, D_MODEL), D_HY ** -0.5)
    rg_conv_w = nrm((DEPTH, 2, RG_CONV, D_RG), RG_CONV ** -0.5)
    rg_conv_b = nrm((DEPTH, 2, D_RG), 0.02)
    rg_gate_w = nrm((DEPTH, 2, 2, RG_HEADS, RG_HEAD_DIM, RG_HEAD_DIM), RG_HEAD_DIM ** -0.5)
    rg_gate_b = nrm((DEPTH, 2, 2, D_RG), 0.02)
    a_c = jax.random.uniform(next(ks), (DEPTH, 2, D_RG), f32, minval=0.9, maxval=0.999)
    a_base = a_c ** (1.0 / RG_C)
    rg_lambda = jnp.log(a_base) - jnp.log1p(-a_base)
    rg_out = nrm((DEPTH, D_RG, D_MODEL), D_RG ** -0.5)
    w_out = nrm((DEPTH, D_MODEL, D_MODEL), D_MODEL ** -0.5)
    g_ffn = 1.0 + nrm((DEPTH, D_MODEL), 0.02)
    peer_wq = nrm((DEPTH, D_MODEL, PEER_HEADS * D_KEY), D_MODEL ** -0.5)
    peer_keys = nrm((DEPTH, PEER_HEADS, 2, N_KEYS, D_HALF), D_HALF ** -0.5)
    peer_u = nrm((DEPTH, N_EXPERTS, D_MODEL), D_MODEL ** -0.5)
    peer_v = nrm((DEPTH, N_EXPERTS, D_MODEL), PEER_HEADS ** -0.5)
    return {'x': x, 'meta': meta, 'g_final': g_final, 'g_mix': g_mix, 'w_in': w_in, 'b_gate': b_gate,
            'hy_conv_w': hy_conv_w, 'hy_conv_b': hy_conv_b, 'hy_w1': hy_w1, 'hy_b1': hy_b1,
            'hy_w2': hy_w2, 'hy_b2': hy_b2, 'hy_w3': hy_w3, 'hy_b3': hy_b3, 'hy_freq': hy_freq,
            'hy_skip': hy_skip, 'hy_out': hy_out, 'rg_conv_w': rg_conv_w, 'rg_conv_b': rg_conv_b,
            'rg_gate_w': rg_gate_w, 'rg_gate_b': rg_gate_b, 'rg_lambda': rg_lambda, 'rg_out': rg_out,
            'w_out': w_out, 'g_ffn': g_ffn, 'peer_wq': peer_wq, 'peer_keys': peer_keys,
            'peer_u': peer_u, 'peer_v': peer_v}


def reference(x, meta, g_final, g_mix, w_in, b_gate, hy_conv_w, hy_conv_b, hy_w1, hy_b1, hy_w2, hy_b2,
              hy_w3, hy_b3, hy_freq, hy_skip, hy_out, rg_conv_w, rg_conv_b, rg_gate_w, rg_gate_b,
              rg_lambda, rg_out, w_out, g_ffn, peer_wq, peer_keys, peer_u, peer_v):
    B = x.shape[0]
    h = jnp.concatenate([jnp.broadcast_to(meta.astype(x.dtype)[None], (B, N_META, D_MODEL)), x], axis=1)
    s_hy = 3 * D_HY
    s_rx = s_hy + D_RG
    s_rg = s_rx + D_RG
    for l in range(DEPTH):
        n = rmsnorm(h, g_mix[l])
        proj = n @ w_in[l]
        y_hy = hyena_branch(proj[..., :s_hy], hy_conv_w[l], hy_conv_b[l], hy_w1[l], hy_b1[l], hy_w2[l],
                            hy_b2[l], hy_w3[l], hy_b3[l], hy_freq[l], hy_skip[l], hy_out[l])
        y_rg = rglru_branch(proj[..., s_hy:s_rx], proj[..., s_rx:s_rg], rg_conv_w[l], rg_conv_b[l],
                            rg_gate_w[l], rg_gate_b[l], rg_lambda[l], rg_out[l])
        gl = proj[..., s_rg:] + b_gate[l]
        merged = jax.nn.sigmoid(gl[..., :D_MODEL]) * y_hy + jax.nn.sigmoid(gl[..., D_MODEL:]) * y_rg
        h = h + merged @ w_out[l]
        h = h + peer(rmsnorm(h, g_ffn[l]), peer_wq[l], peer_keys[l], peer_u[l], peer_v[l])
    h = rmsnorm(h, g_final)
    return h[:, N_META:]
```

```python
import math
import os
from contextlib import ExitStack

import numpy as np
import ml_dtypes

import concourse.bass as bass
import concourse.mybir as mybir
from concourse.bass_utils import run_bass_kernel_spmd

F32 = mybir.dt.float32
BF16 = mybir.dt.bfloat16
I32 = mybir.dt.int32
U32 = mybir.dt.uint32
ALU = mybir.AluOpType
AF = mybir.ActivationFunctionType
AX = mybir.AxisListType

D = 1024
SEQ = 4096
NMETA = 16
L = SEQ + NMETA
NT = 33
LP = NT * 128
NFFT = 2 * L
NF = L + 1
D_HY = 512
D_RG = 1024
D_IN = 3 * D_HY + 2 * D_RG + 2 * D
EPS = 1e-6
NCH = [(n * 512, min(512, L - n * 512)) for n in range(9)]


def _rows(i):
    return 128 if i < NT - 1 else L - 128 * (NT - 1)


class Buf:
    __slots__ = ("name", "w", "r")

    def __init__(self, name=""):
        self.name = name
        self.w = None
        self.r = {}


class Sched:
    ENG = ("pe", "act", "dve", "pool", "sp")
    NDMA = {"sp": 24, "pool": 16, "act": 8}

    def __init__(self, nc, stack):
        self.nc = nc
        self.items = {e: [] for e in self.ENG}
        self.sems = {}
        self.cnt = {}
        self.seen = {e: {} for e in self.ENG}
        for e in self.ENG:
            self.sems[e] = stack.enter_context(nc.semaphore("s_" + e))
            self.cnt[e] = 0
        self.dma_pool = {}
        self.dma_next = {}
        for q, n in self.NDMA.items():
            keys = []
            for i in range(n):
                k = "d_%s_%d" % (q, i)
                self.sems[k] = stack.enter_context(nc.semaphore(k))
                self.cnt[k] = 0
                keys.append(k)
            self.dma_pool[q] = keys
            self.dma_next[q] = 0
        self.n_ops = 0

    def _wait(self, eng, ev):
        if ev is None:
            return
        k, v = ev
        if self.seen[eng].get(k, 0) >= v:
            return
        self.seen[eng][k] = v
        self.items[eng].append(("w", k, v))

    def _deps(self, eng, reads, writes):
        for b in reads:
            if b.w is not None:
                if not (eng == "pe" and b.w[0] == "pe"):
                    self._wait(eng, b.w)
        for b in writes:
            if b.w is not None:
                if not (eng == "pe" and b.w[0] == "pe"):
                    self._wait(eng, b.w)
            for k, v in b.r.items():
                if eng == "pe" and k == "pe":
                    continue
                self._wait(eng, (k, v))

    def _commit(self, ev, reads, writes):
        for b in writes:
            b.w = ev
            b.r = {}
        for b in reads:
            if b in writes:
                continue
            if b.r.get(ev[0], 0) < ev[1]:
                b.r[ev[0]] = ev[1]

    def op(self, eng, fn, reads=(), writes=(), weak_reads=()):
        self._deps(eng, list(reads) + list(weak_reads), writes)
        self.cnt[eng] += 1
        ev = (eng, self.cnt[eng])
        self.items[eng].append(("o", fn, eng, 1))
        self._commit(ev, reads, writes)
        self.n_ops += 1
        return ev

    def dma(self, q, fn, reads=(), writes=(), slot_covered=False):
        pool = self.dma_pool[q]
        k = pool[self.dma_next[q] % len(pool)]
        self.dma_next[q] += 1
        if slot_covered:
            self.seen[q][k] = max(self.seen[q].get(k, 0), self.cnt[k])
        else:
            self._wait(q, (k, self.cnt[k]))
        self._deps(q, reads, writes)
        self.cnt[k] += 16
        ev = (k, self.cnt[k])
        self.items[q].append(("o", fn, k, 16))
        self._commit(ev, reads, writes)
        self.n_ops += 1
        return ev

    def barrier(self):
        for e in self.ENG:
            for k, v in self.cnt.items():
                if v > 0 and k != e:
                    self._wait(e, (k, v))

    def finish(self, eng="sp"):
        for k, v in self.cnt.items():
            if v > 0 and k != eng:
                self._wait(eng, (k, v))

    def emit(self):
        nc = self.nc
        sems = self.sems
        items = self.items

        def replay(engine, lst):
            for it in lst:
                if it[0] == "w":
                    engine.wait_ge(sems[it[1]], it[2])
                else:
                    it[1](engine).then_inc(sems[it[2]], it[3])

        with nc.Block() as block:
            @block.sync
            def _(e):
                replay(e, items["sp"])

            @block.tensor
            def _(e):
                replay(e, items["pe"])

            @block.scalar
            def _(e):
                replay(e, items["act"])

            @block.vector
            def _(e):
                replay(e, items["dve"])

            @block.gpsimd
            def _(e):
                replay(e, items["pool"])


_CONST_CACHE = {}


def _host_consts():
    if _CONST_CACHE:
        return _CONST_CACHE
    f32 = np.float32
    t = np.linspace(0.0, 1.0, L, dtype=f32)[:, None]
    w = (f32(2.0 * math.pi / L)) * np.arange(L, dtype=f32)[:, None]
    bands = np.linspace(1e-4, 15, 16, dtype=f32)[None, :]
    z = np.concatenate([t, np.cos(bands * w), -np.sin(bands * w)], axis=-1).astype(f32)
    zT = np.ascontiguousarray(z.T)
    deltas = np.abs(np.linspace(math.log(1e-2) / 0.3, math.log(1e-2) / 1.5, D_HY, dtype=f32)).astype(f32)
    negdelta = np.ascontiguousarray((-deltas).reshape(4, 128).T)
    tlin = np.ascontiguousarray(t.reshape(1, L))
    fidx = np.arange(LP)
    wfv = np.where((fidx == 0) | (fidx == L), 1.0 / NFFT, 2.0 / NFFT)
    wfv = np.where(fidx <= L, wfv, 0.0).astype(f32)
    wf = np.ascontiguousarray(wfv.reshape(NT, 128).T)
    a = np.arange(LP, dtype=np.int64)
    prod = (a[:, None] * a[None, :]) % NFFT
    ang = prod.astype(np.float64) * (2.0 * math.pi / NFFT)
    C = np.cos(ang).astype(f32)
    S = np.sin(ang).astype(f32)
    del ang, prod

    def lay(M):
        M4 = M.reshape(NT, 128, NT, 128)
        return np.ascontiguousarray(M4.transpose(2, 1, 0, 3)).reshape(NT, 128, NT * 128).astype(ml_dtypes.bfloat16)

    _CONST_CACHE.update(dict(
        c_zT=zT, c_negdelta=negdelta, c_tlin=tlin, c_wf=wf, c_tabC=lay(C), c_tabS=lay(S),
        c_identf=np.eye(128, dtype=f32), c_identb=np.eye(128, dtype=f32).astype(ml_dtypes.bfloat16),
    ))
    return _CONST_CACHE


class Prog:
    def __init__(self, dbg=None):
        self.dbg = dbg or ()
        self.nc = bass.Bass("TRN2", target_bir_lowering=False)
        self.stack = ExitStack()
        self.S = None
        self.dram = {}
        self.dbufs = {}

    def din(self, name, shape, dtype=F32):
        t = self.nc.dram_tensor(name, list(shape), dtype, kind="ExternalInput")
        self.dram[name] = t
        self.dbufs[name] = Buf(name)
        return t

    def dout(self, name, shape, dtype=F32):
        t = self.nc.dram_tensor(name, list(shape), dtype, kind="ExternalOutput")
        self.dram[name] = t
        self.dbufs[name] = Buf(name)
        return t

    def dscr(self, name, shape, dtype=F32):
        t = self.nc.dram_tensor(name, list(shape), dtype)
        self.dram[name] = t
        self.dbufs[name] = Buf(name)
        return t

    def sb(self, name, shape, dtype=F32, stack=None):
        t = (stack or self.stack).enter_context(self.nc.sbuf_tensor(name, list(shape), dtype))
        return t

    def ps(self, name, shape, dtype=F32, stack=None):
        t = (stack or self.stack).enter_context(self.nc.psum_tensor(name, list(shape), dtype))
        return t


def _round_reduce(S, arg, tmp, buf_arg, buf_tmp, n):
    MAGIC = 12582912.0
    TWO_PI = 2.0 * math.pi
    S.op("dve", lambda e: e.tensor_scalar(out=tmp, in0=arg, scalar1=1.0 / TWO_PI, scalar2=MAGIC,
                                          op0=ALU.mult, op1=ALU.add), reads=[buf_arg], writes=[buf_tmp])
    S.op("dve", lambda e: e.tensor_scalar(out=tmp, in0=tmp, scalar1=MAGIC, scalar2=-TWO_PI,
                                          op0=ALU.subtract, op1=ALU.mult), reads=[buf_tmp], writes=[buf_tmp])
    S.op("dve", lambda e: e.tensor_tensor(out=arg, in0=arg, in1=tmp, op=ALU.add),
         reads=[buf_arg, buf_tmp], writes=[buf_arg])
    S.op("dve", lambda e: e.tensor_scalar(out=arg, in0=arg, scalar1=3.1415925, scalar2=-3.1415925,
                                          op0=ALU.min, op1=ALU.max), reads=[buf_arg], writes=[buf_arg])


def phase_filter(P, inp, uv_hook=None):
    nc, S = P.nc, P.S
    kspec = P.dscr("kspec", [NT, 128, 2, 512])
    bk = P.dbufs["kspec"]
    with ExitStack() as st0, ExitStack() as st:
        kst = P.sb("fkst", [128, NT, 512], BF16, st0); b_kst = Buf()
        kdt = P.sb("fkdt", [128, NT, 512], BF16, st0); b_kdt = Buf()
        wf = P.sb("fwf", [128, NT], F32, st0); b_wf = Buf()
        w1 = P.sb("fw1", [33, 64], F32, st); b_w1 = Buf()
        w2 = P.sb("fw2", [64, 64], F32, st); b_w2 = Buf()
        w3 = P.sb("fw3", [64, 1024], F32, st); b_w3 = Buf()
        sm = P.sb("fsm", [64, 8], F32, st); b_sm = Buf()
        b3 = P.sb("fb3", [128, 8], F32, st); b_b3 = Buf()
        ndl = P.sb("fndl", [128, 4], F32, st); b_ndl = Buf()
        tl = P.sb("ftl", [128, L], F32, st); b_tl = Buf()
        identb = P.sb("fidb", [128, 128], BF16, st); b_idb = Buf()
        kf = P.sb("fkf", [128, L], F32, st); b_kf = Buf()
        kb = P.sb("fkb", [128, L], F32, st); b_kb = Buf()
        hd2 = P.sb("fhd2", [64, L], F32, st); b_hd2 = Buf()
        win = P.sb("fwin", [128, L], F32, st); b_win = Buf()
        z_sb, b_z = kf, b_kf
        hd1, b_hd1 = kb, b_kb
        tmp, b_tmp = win, b_win
        ksb = P.sb("fksb", [128, LP], BF16, st); b_ksb = Buf()
        kdb = P.sb("fkdb", [128, LP], BF16, st); b_kdb = Buf()
        pa = [P.ps("fpa%d" % i, [128, 512], F32, st0) for i in range(2)]
        b_pa = [Buf(), Buf()]
        pb = [P.ps("fpb%d" % i, [128, 512], F32, st0) for i in range(2)]
        b_pb = [Buf(), Buf()]
        pt = [P.ps("fpt%d" % i, [128, 8, 128], BF16, st0) for i in range(2)]
        b_pt = [Buf(), Buf()]

        D_ = P.dram
        ld = lambda o, i, bw, name=None: S.dma("sp", lambda e: e.dma_start(out=o, in_=i), writes=[bw])
        ld(z_sb[0:33, :], D_["c_zT"].ap(), b_z)
        ld(w1[:], inp["hy_w1"].ap()[0], b_w1)
        ld(w2[:], inp["hy_w2"].ap()[0], b_w2)
        ld(w3[:], inp["hy_w3"].ap()[0], b_w3)
        ld(sm[:, 0:1], inp["hy_b1"].ap().rearrange("o (p u) -> (o p) u", u=1), b_sm)
        ld(sm[:, 1:2], inp["hy_b2"].ap().rearrange("o (p u) -> (o p) u", u=1), b_sm)
        ld(sm[:, 2:3], inp["hy_freq"].ap()[0, 0:1, :].rearrange("o (p u) -> (o p) u", u=1), b_sm)
        ld(sm[:, 3:4], inp["hy_freq"].ap()[0, 1:2, :].rearrange("o (p u) -> (o p) u", u=1), b_sm)
        for cc in range(8):
            ld(b3[:, cc:cc + 1], inp["hy_b3"].ap()[0:1, cc * 128:(cc + 1) * 128].rearrange("o (p u) -> (o p) u", u=1), b_b3)
        ld(ndl[:], D_["c_negdelta"].ap(), b_ndl)
        ld(tl[:], D_["c_tlin"].ap().partition_broadcast(128), b_tl)
        ld(wf[:], D_["c_wf"].ap(), b_wf)
        ld(identb[:], D_["c_identb"].ap(), b_idb)

        S.op("dve", lambda e: e.tensor_tensor(out=sm[:, 4:6], in0=sm[:, 0:2], in1=sm[:, 2:4], op=ALU.mult),
             reads=[b_sm], writes=[b_sm])

        def layer(wt, b_wt, kdim, src, b_src, arg, b_arg, fcol, fbcol):
            for n, (c0, cw) in enumerate(NCH):
                p = pa[n % 2]; bp = b_pa[n % 2]
                S.op("pe", lambda e, p=p, c0=c0, cw=cw: e.matmul(p[0:64, 0:cw], lhsT=wt[0:kdim, :], rhs=src[0:kdim, c0:c0 + cw],
                                                                   start=True, stop=True),
                     reads=[b_wt, b_src], writes=[bp])
                S.op("dve", lambda e, p=p, c0=c0, cw=cw: e.tensor_scalar(out=arg[0:64, c0:c0 + cw], in0=p[0:64, 0:cw],
                                                                       scalar1=sm[:, fcol:fcol + 1], scalar2=sm[:, fbcol:fbcol + 1],
                                                                       op0=ALU.mult, op1=ALU.add),
                     reads=[bp, b_sm], writes=[b_arg])
            _round_reduce(S, arg[0:64, :], tmp[0:64, :], b_arg, b_tmp, L)
            S.op("act", lambda e: e.activation(out=arg[0:64, :], in_=arg[0:64, :], func=AF.Sin), reads=[b_arg], writes=[b_arg])

        layer(w1, b_w1, 33, z_sb, b_z, hd1, b_hd1, 2, 4)
        layer(w2, b_w2, 64, hd1, b_hd1, hd2, b_hd2, 3, 5)

        tcount = [0]
        for q in range(4):
            S.op("act", lambda e, q=q: e.activation(out=win[:], in_=tl[:], func=AF.Exp, scale=ndl[:, q:q + 1]),
                 reads=[b_tl, b_ndl], writes=[b_win])
            for (cc, dst, b_dst) in ((q, kf, b_kf), (q + 4, kb, b_kb)):
                for n, (c0, cw) in enumerate(NCH):
                    p = pa[n % 2]; bp = b_pa[n % 2]
                    S.op("pe", lambda e, p=p, c0=c0, cw=cw, cc=cc: e.matmul(p[:, 0:cw], lhsT=w3[:, cc * 128:(cc + 1) * 128],
                                                                              rhs=hd2[:, c0:c0 + cw], start=True, stop=True),
                         reads=[b_w3, b_hd2], writes=[bp])
                    S.op("dve", lambda e, p=p, c0=c0, cw=cw, cc=cc, dst=dst: e.scalar_tensor_tensor(
                        out=dst[:, c0:c0 + cw], in0=p[:, 0:cw], scalar=b3[:, cc:cc + 1], in1=win[:, c0:c0 + cw],
                        op0=ALU.add, op1=ALU.mult), reads=[bp, b_b3, b_win], writes=[b_dst])
            S.op("dve", lambda e: e.memset(kb[:, 0:1], 0.0), writes=[b_kb])
            S.op("dve", lambda e: e.tensor_tensor(out=ksb[:, 0:L], in0=kf[:], in1=kb[:], op=ALU.add),
                 reads=[b_kf, b_kb], writes=[b_ksb])
            S.op("dve", lambda e: e.tensor_tensor(out=kdb[:, 0:L], in0=kf[:], in1=kb[:], op=ALU.subtract),
                 reads=[b_kf, b_kb], writes=[b_kdb])
            for (src, b_src, dstt, b_dstt) in ((ksb, b_ksb, kst, b_kst), (kdb, b_kdb, kdt, b_kdt)):
                for g0 in range(0, NT, 8):
                    g1 = min(NT, g0 + 8)
                    k = tcount[0] % 2; tcount[0] += 1
                    for i in range(g0, g1):
                        r = _rows(i)
                        S.op("pe", lambda e, i=i, r=r, k=k, g0=g0, src=src: e.transpose(pt[k][0:r, i - g0, :], src[:, i * 128:i * 128 + r],
                                                                                        identb[:]),
                             reads=[b_src, b_idb], writes=[b_pt[k]])
                    full = [i for i in range(g0, g1) if _rows(i) == 128]
                    if full:
                        nfull = len(full)
                        S.op("act", lambda e, k=k, g0=g0, nfull=nfull, q=q, dstt=dstt: e.copy(
                            out=dstt[:, g0:g0 + nfull, q * 128:(q + 1) * 128], in_=pt[k][:, 0:nfull, :]),
                            reads=[b_pt[k]], writes=[b_dstt])
                    if g1 == NT:
                        r = _rows(NT - 1)
                        S.op("act", lambda e, k=k, g0=g0, r=r, q=q, dstt=dstt: e.copy(
                            out=dstt[0:r, NT - 1, q * 128:(q + 1) * 128], in_=pt[k][0:r, NT - 1 - g0, :]),
                            reads=[b_pt[k]], writes=[b_dstt])

        S.barrier()
        st.close()
        tabc = [P.sb("ftabc%d" % i, [128, NT * 128], BF16, st0) for i in range(2)]
        tabs = [P.sb("ftabs%d" % i, [128, NT * 128], BF16, st0) for i in range(2)]
        b_tabc = [Buf(), Buf()]; b_tabs = [Buf(), Buf()]
        osp = [P.sb("fosp%d" % i, [128, 2, 512], F32, st0) for i in range(2)]
        b_osp = [Buf(), Buf()]
        if uv_hook is not None:
            uv_hook(st0)
        for j in range(NT):
            k = j % 2
            S.dma("sp", lambda e, j=j, k=k: e.dma_start(out=tabc[k][:], in_=D_["c_tabC"].ap()[j]), writes=[b_tabc[k]])
            S.dma("sp", lambda e, j=j, k=k: e.dma_start(out=tabs[k][:], in_=D_["c_tabS"].ap()[j]), writes=[b_tabs[k]])
            for i in range(NT):
                r = _rows(i)
                S.op("pe", lambda e, i=i, r=r, k=k: e.matmul(pa[k][:], lhsT=tabc[k][0:r, i * 128:(i + 1) * 128], rhs=kst[0:r, i, :],
                                                             start=(i == 0), stop=(i == NT - 1)),
                     reads=[b_tabc[k], b_kst], writes=[b_pa[k]])
            for i in range(NT):
                r = _rows(i)
                S.op("pe", lambda e, i=i, r=r, k=k: e.matmul(pb[k][:], lhsT=tabs[k][0:r, i * 128:(i + 1) * 128], rhs=kdt[0:r, i, :],
                                                             start=(i == 0), stop=(i == NT - 1)),
                     reads=[b_tabs[k], b_kdt], writes=[b_pb[k]])
            S.op("dve", lambda e, j=j, k=k: e.tensor_scalar(out=osp[k][:, 0, :], in0=pa[k][:], scalar1=wf[:, j:j + 1], scalar2=None,
                                                            op0=ALU.mult), reads=[b_pa[k], b_wf], writes=[b_osp[k]])
            S.op("dve", lambda e, j=j, k=k: e.tensor_scalar(out=osp[k][:, 1, :], in0=pb[k][:], scalar1=wf[:, j:j + 1], scalar2=None,
                                                            op0=ALU.mult), reads=[b_pb[k], b_wf], writes=[b_osp[k]])
            S.dma("sp", lambda e, j=j, k=k: e.dma_start(out=kspec.ap()[j], in_=osp[k][:]), reads=[b_osp[k]], writes=[bk])
        S.barrier()
    return kspec


from concourse.bass import IndirectOffsetOnAxis

def c_hcw(k, cc): return k * 12 + cc
def c_hcb(cc): return 36 + cc
def c_skip(i): return 48 + i
def c_rcw(d, j, h): return 52 + (d * 4 + j) * 8 + h
def c_gmix(k): return 116 + k
def c_rcb(d, h): return 128 + d * 8 + h
def c_rgb(d, g, h): return 144 + (d * 2 + g) * 8 + h
def c_lam(d, h): return 176 + d * 8 + h
def c_bg(m): return 192 + m
NPC = 208


def phase_params(P, inp, G):
    nc, S = P.nc, P.S
    st = P.stack
    PC = P.sb("PC", [128, NPC], F32, st); b_PC = Buf()
    CL = P.sb("CL", [128, 32], F32, st); b_CL = Buf()
    identf = P.sb("identf", [128, 128], F32, st); b_idf = Buf()
    identb = P.sb("identb", [128, 128], BF16, st); b_idb = Buf()
    G.update(PC=PC, b_PC=b_PC, CL=CL, b_CL=b_CL, identf=identf, b_idf=b_idf, identb=identb, b_idb=b_idb)
    S.dma("sp", lambda e: e.dma_start(out=identf[:], in_=P.dram["c_identf"].ap()), writes=[b_idf])
    S.dma("sp", lambda e: e.dma_start(out=identb[:], in_=P.dram["c_identb"].ap()), writes=[b_idb])
    with ExitStack() as s2:
        PR = [P.sb("PR0", [128, 128], F32, s2), P.sb("PR1", [128, 128], F32, s2)]
        b_PR = [Buf(), Buf()]
        pp = P.ps("pprm", [128, 128], F32, s2); b_pp = Buf()
        tmp = P.sb("prm_t", [128, 16 * 6], F32, s2); b_tmp = Buf()
        S.op("dve", lambda e: e.memset(PR[0][:], 0.0), writes=[b_PR[0]])
        S.op("dve", lambda e: e.memset(PR[1][:], 0.0), writes=[b_PR[1]])

        def ldrows(t, r0, src, n):
            S.dma("sp", lambda e: e.dma_start(out=PR[t][r0:r0 + n, :], in_=src), writes=[b_PR[t]])
        ldrows(0, 0, inp["hy_conv_w"].ap().rearrange("o k (c p) -> (o k c) p", p=128), 36)
        ldrows(0, 36, inp["hy_conv_b"].ap().rearrange("o (c p) -> (o c) p", p=128), 12)
        ldrows(0, 48, inp["hy_skip"].ap().rearrange("o (c p) -> (o c) p", p=128), 4)
        ldrows(0, 52, inp["rg_conv_w"].ap().rearrange("o d j (c p) -> (o d j c) p", p=128), 64)
        ldrows(0, 116, inp["g_mix"].ap().rearrange("o (c p) -> (o c) p", p=128), 8)
        ldrows(1, 0, inp["rg_conv_b"].ap().rearrange("o d (c p) -> (o d c) p", p=128), 16)
        ldrows(1, 16, inp["rg_gate_b"].ap().rearrange("o d g (c p) -> (o d g c) p", p=128), 32)
        ldrows(1, 48, inp["rg_lambda"].ap().rearrange("o d (c p) -> (o d c) p", p=128), 16)
        ldrows(1, 64, inp["b_gate"].ap().rearrange("o (c p) -> (o c) p", p=128), 16)
        for t, ncol in ((0, 128), (1, 80)):
            S.op("pe", lambda e, t=t, ncol=ncol: e.transpose(pp[:, 0:ncol], PR[t][0:ncol, :], identf[0:ncol, 0:ncol]),
                 reads=[b_PR[t], b_idf], writes=[b_pp])
            S.op("dve", lambda e, t=t, ncol=ncol: e.tensor_copy(out=PC[:, t * 128:t * 128 + ncol], in_=pp[:, 0:ncol]),
                 reads=[b_pp], writes=[b_PC])
        lam = PC[:, 176:192]
        al, xx, ss, s2_, acc, pw = [tmp[:, i * 16:(i + 1) * 16] for i in range(6)]
        R = [b_PC, b_tmp]; W = [b_tmp]
        S.op("dve", lambda e: e.tensor_scalar(out=al, in0=lam, scalar1=-1.0, scalar2=None, op0=ALU.mult), reads=R, writes=W)
        S.op("dve", lambda e: e.tensor_tensor(out=al, in0=al, in1=lam, op=ALU.max), reads=R, writes=W)
        S.op("act", lambda e: e.activation(out=xx, in_=al, func=AF.Exp, scale=-1.0), reads=R, writes=W)
        S.op("dve", lambda e: e.tensor_scalar(out=ss, in0=xx, scalar1=2.0, scalar2=None, op0=ALU.add), reads=R, writes=W)
        S.op("dve", lambda e: e.reciprocal(out=ss, in_=ss), reads=R, writes=W)
        S.op("dve", lambda e: e.tensor_tensor(out=ss, in0=ss, in1=xx, op=ALU.mult), reads=R, writes=W)
        S.op("dve", lambda e: e.tensor_tensor(out=s2_, in0=ss, in1=ss, op=ALU.mult), reads=R, writes=W)
        S.op("dve", lambda e: e.tensor_copy(out=acc, in_=ss), reads=R, writes=W)
        S.op("dve", lambda e: e.tensor_copy(out=pw, in_=ss), reads=R, writes=W)
        for kk in (3, 5, 7, 9, 11, 13):
            S.op("dve", lambda e: e.tensor_tensor(out=pw, in0=pw, in1=s2_, op=ALU.mult), reads=R, writes=W)
            S.op("dve", lambda e, kk=kk: e.scalar_tensor_tensor(out=acc, in0=pw, scalar=1.0 / kk, in1=acc, op0=ALU.mult, op1=ALU.add),
                 reads=R, writes=W)
        S.op("dve", lambda e: e.tensor_scalar(out=al, in0=lam, scalar1=-1.0, scalar2=0.0, op0=ALU.mult, op1=ALU.max), reads=R, writes=W)
        S.op("dve", lambda e: e.scalar_tensor_tensor(out=acc, in0=acc, scalar=2.0, in1=al, op0=ALU.mult, op1=ALU.add), reads=R, writes=W)
        S.op("dve", lambda e: e.tensor_scalar(out=CL[:, 0:16], in0=acc, scalar1=-8.0, scalar2=None, op0=ALU.mult), reads=R, writes=[b_CL])
        S.op("dve", lambda e: e.tensor_scalar(out=CL[:, 16:32], in0=acc, scalar1=-16.0, scalar2=None, op0=ALU.mult), reads=R, writes=[b_CL])
        S.barrier()


def phase_uvtab(P, inp, st):
    nc, S = P.nc, P.S
    uvtab = P.dscr("uvtab", [16384, 2048], BF16)
    utab = inp["peer_u"].ap()[0]; vtab = inp["peer_v"].ap()[0]
    us = [P.sb("t_us%d" % i, [128, D], F32, st) for i in range(2)]; b_us = [Buf(), Buf()]
    vs = [P.sb("t_vs%d" % i, [128, D], F32, st) for i in range(2)]; b_vs = [Buf(), Buf()]
    ob = [P.sb("t_ob%d" % i, [128, 2 * D], BF16, st) for i in range(2)]; b_ob = [Buf(), Buf()]
    for c in range(128):
        k = c % 2
        S.dma("pool", lambda e, c=c, k=k: e.dma_start(out=us[k][:], in_=utab[128 * c:128 * (c + 1), :]), writes=[b_us[k]])
        S.dma("pool", lambda e, c=c, k=k: e.dma_start(out=vs[k][:], in_=vtab[128 * c:128 * (c + 1), :]), writes=[b_vs[k]])
        if c >= 1:
            kp = (c - 1) % 2
            S.op("act", lambda e, kp=kp: e.copy(out=ob[kp][:, 0:D], in_=us[kp][:]), reads=[b_us[kp]], writes=[b_ob[kp]])
            S.op("act", lambda e, kp=kp: e.copy(out=ob[kp][:, D:2 * D], in_=vs[kp][:]), reads=[b_vs[kp]], writes=[b_ob[kp]])
            S.dma("pool", lambda e, c=c, kp=kp: e.dma_start(out=uvtab.ap()[128 * (c - 1):128 * c, :], in_=ob[kp][:]), reads=[b_ob[kp]],
                  writes=[P.dbufs["uvtab"]])
    kp = 127 % 2
    S.op("act", lambda e: e.copy(out=ob[kp][:, 0:D], in_=us[kp][:]), reads=[b_us[kp]], writes=[b_ob[kp]])
    S.op("act", lambda e: e.copy(out=ob[kp][:, D:2 * D], in_=vs[kp][:]), reads=[b_vs[kp]], writes=[b_ob[kp]])
    S.dma("pool", lambda e: e.dma_start(out=uvtab.ap()[128 * 127:128 * 128, :], in_=ob[kp][:]), reads=[b_ob[kp]], writes=[P.dbufs["uvtab"]])


def phase_norm(P, inp, G):
    nc, S = P.nc, P.S
    nT = G["nT"]; b_nT = G["b_nT"]
    identb, b_idb = G["identb"], G["b_idb"]
    x = inp["x"].ap(); meta = inp["meta"].ap()
    with ExitStack() as st:
        ht = [P.sb("n_h%d" % i, [128, D], F32, st) for i in range(2)]; b_ht = [Buf(), Buf()]
        sq = P.sb("n_sq", [128, D], F32, st); b_sq = Buf()
        nb = [P.sb("n_nb%d" % i, [128, D], BF16, st) for i in range(2)]; b_nb = [Buf(), Buf()]
        ssq = P.sb("n_ss", [128, 2 * NT], F32, st); b_ss = Buf()
        pt = [P.ps("n_pt%d" % i, [128, 8, 128], BF16, st) for i in range(2)]; b_pt = [Buf(), Buf()]
        for j in range(NT):
            k = j % 2
            r = _rows(j)
            if j == 0:
                S.dma("sp", lambda e, k=k: e.dma_start(out=ht[k][0:NMETA, :], in_=meta), writes=[b_ht[k]])
                S.dma("sp", lambda e, k=k: e.dma_start(out=ht[k][NMETA:128, :], in_=x[0:128 - NMETA, :]), writes=[b_ht[k]])
            else:
                S.dma("sp", lambda e, k=k, j=j, r=r: e.dma_start(out=ht[k][0:r, :], in_=x[128 * j - NMETA:128 * j - NMETA + r, :]),
                      writes=[b_ht[k]])
            S.op("act", lambda e, k=k, j=j, r=r: e.activation(out=sq[0:r, :], in_=ht[k][0:r, :], func=AF.Square,
                                                              accum_out=ssq[0:r, 2 * j:2 * j + 1]),
                 reads=[b_ht[k]], writes=[b_sq, b_ss])
            S.op("act", lambda e, j=j, r=r: e.activation(out=ssq[0:r, 2 * j + 1:2 * j + 2], in_=ssq[0:r, 2 * j:2 * j + 1], func=AF.Sqrt,
                                                         scale=1.0 / D, bias=EPS), reads=[b_ss], writes=[b_ss])
            S.op("dve", lambda e, j=j, r=r: e.reciprocal(out=ssq[0:r, 2 * j + 1:2 * j + 2], in_=ssq[0:r, 2 * j + 1:2 * j + 2]),
                 reads=[b_ss], writes=[b_ss])
            S.op("dve", lambda e, k=k, j=j, r=r: e.tensor_scalar(out=nb[k][0:r, :], in0=ht[k][0:r, :], scalar1=ssq[0:r, 2 * j + 1:2 * j + 2],
                                                                  scalar2=None, op0=ALU.mult), reads=[b_ht[k], b_ss], writes=[b_nb[k]])
            for c in range(8):
                S.op("pe", lambda e, k=k, c=c, r=r: e.transpose(pt[k][:, c, 0:r], nb[k][0:r, c * 128:(c + 1) * 128], identb[0:r, 0:r]),
                     reads=[b_nb[k], b_idb], writes=[b_pt[k]])
            S.op("act", lambda e, k=k, j=j, r=r: e.copy(out=nT[:, :, 128 * j:128 * j + r], in_=pt[k][:, :, 0:r]),
                 reads=[b_pt[k]], writes=[b_nT])
        S.barrier()


def phase_mixer_in(P, inp, G):
    nc, S = P.nc, P.S
    nT, b_nT, PC, b_PC, CL, b_CL = G["nT"], G["b_nT"], G["PC"], G["b_PC"], G["CL"], G["b_CL"]
    identb, b_idb = G["identb"], G["b_idb"]
    w_in = inp["w_in"].ap()[0].rearrange("(k p) c -> p k c", p=128)
    yrgT = P.dscr("yrgT", [8, 128, L], BF16)
    sgT = P.dscr("sgT", [16, 128, L], BF16)
    x0cT = P.dscr("x0cT", [4, 128, L], F32)
    zT = P.dscr("zT", [4, 128, L], F32)
    with ExitStack() as st0:
        wst = [P.sb("m_wst%d" % i, [128, 8, 128], F32, st0) for i in range(2)]; b_wst = [Buf(), Buf()]
        wbf = [P.sb("m_wbf%d" % i, [128, 8, 128], BF16, st0) for i in range(2)]; b_wbf = [Buf(), Buf()]
        pp = [P.ps("m_pp%d" % i, [128, 512], F32, st0) for i in range(2)]; b_pp = [Buf(), Buf()]
        pg = [P.ps("m_pg%d" % i, [128, 512], F32, st0) for i in range(2)]; b_pg = [Buf(), Buf()]
        pt = [P.ps("m_pt%d" % i, [128, 8, 128], BF16, st0) for i in range(2)]; b_pt = [Buf(), Buf()]
        cnt = {"w": 0, "p": 0, "g": 0, "t": 0}
        gm_b = PC[:, 116:124].unsqueeze(2).to_broadcast([128, 8, 128])

        def project(cc, evac):
            k = cnt["w"] % 2; cnt["w"] += 1
            S.dma("sp", lambda e: e.dma_start(out=wst[k][:], in_=w_in[:, :, cc * 128:(cc + 1) * 128]), writes=[b_wst[k]])
            S.op("pool", lambda e: e.tensor_tensor(out=wbf[k][:], in0=wst[k][:], in1=gm_b, op=ALU.mult),
                 reads=[b_wst[k], b_PC], writes=[b_wbf[k]])
            for n, (c0, cw) in enumerate(NCH):
                q = cnt["p"] % 2; cnt["p"] += 1
                for kk in range(8):
                    S.op("pe", lambda e, kk=kk, q=q, c0=c0, cw=cw: e.matmul(pp[q][:, 0:cw], lhsT=wbf[k][:, kk, :], rhs=nT[:, kk, c0:c0 + cw],
                                                                             start=(kk == 0), stop=(kk == 7)),
                         reads=[b_wbf[k], b_nT], writes=[b_pp[q]])
                evac(pp[q], b_pp[q], c0, cw)

        with ExitStack() as st:
            xr = P.sb("r_xr", [128, L + 6], F32, st); b_xr = Buf()
            ggs = [P.sb("r_gg%d" % i, [128, L], BF16, st) for i in range(2)]; b_ggs = [Buf(), Buf()]
            xcb = P.sb("r_xcb", [128, L], BF16, st); b_xcb = Buf()
            rr = P.sb("r_rr", [128, L], F32, st); b_rr = Buf()
            ii = P.sb("r_ii", [128, L], F32, st); b_ii = Buf()
            aa = P.sb("r_aa", [128, L], F32, st); b_aa = Buf()
            hacc = P.sb("r_ha", [128, L], F32, st); b_ha = Buf()
            yb = P.sb("r_yb", [128, L], BF16, st); b_yb = Buf()
            gwss = [P.sb("r_gws%d" % i, [128, 4, 128], F32, st) for i in range(2)]; b_gwss = [Buf(), Buf()]
            gwbs = [P.sb("r_gwb%d" % i, [128, 4, 128], BF16, st) for i in range(2)]; b_gwbs = [Buf(), Buf()]
            S.op("dve", lambda e: e.memset(xr[:, 0:3], 0.0), writes=[b_xr])
            S.op("dve", lambda e: e.memset(xr[:, L + 3:L + 6], 0.0), writes=[b_xr])
            gw = inp["rg_gate_w"].ap()[0]
            def prefetch(h):
                gg, b_gg = ggs[h % 2], b_ggs[h % 2]
                gws, b_gws, gwb, b_gwb = gwss[h % 2], b_gwss[h % 2], gwbs[h % 2], b_gwbs[h % 2]
                project(12 + h, lambda p, bp, c0, cw: S.op("act", lambda e: e.copy(out=xr[:, 3 + c0:3 + c0 + cw], in_=p[:, 0:cw]),
                                                          reads=[bp], writes=[b_xr]))
                project(20 + h, lambda p, bp, c0, cw: S.op("act", lambda e: e.activation(out=gg[:, c0:c0 + cw], in_=p[:, 0:cw],
                                                                                        func=AF.Gelu_apprx_tanh),
                                                          reads=[bp], writes=[b_gg]))
                S.dma("sp", lambda e: e.dma_start(out=gws[:], in_=gw[:, :, h].rearrange("d g i j -> i (d g) j")), writes=[b_gws])
                S.op("pool", lambda e: e.tensor_copy(out=gwb[:], in_=gws[:]), reads=[b_gws], writes=[b_gwb])

            prefetch(0)
            for h in range(8):
                gg, b_gg = ggs[h % 2], b_ggs[h % 2]
                gwb, b_gwb = gwbs[h % 2], b_gwbs[h % 2]
                for d in range(2):
                    def xs(j, d=d):
                        off = 3 - j if d == 0 else 3 + j
                        return xr[:, off:off + L]
                    S.op("dve", lambda e, d=d, h=h, xv=xs(0): e.tensor_scalar(out=ii[:], in0=xv, scalar1=PC[:, c_rcw(d, 0, h):c_rcw(d, 0, h) + 1],
                                                                   scalar2=PC[:, c_rcb(d, h):c_rcb(d, h) + 1], op0=ALU.mult, op1=ALU.add),
                         reads=[b_xr, b_PC], writes=[b_ii])
                    for j in (1, 2):
                        S.op("dve", lambda e, d=d, h=h, j=j, xv=xs(j): e.scalar_tensor_tensor(out=ii[:], in0=xv, scalar=PC[:, c_rcw(d, j, h):c_rcw(d, j, h) + 1],
                                                                                   in1=ii[:], op0=ALU.mult, op1=ALU.add),
                             reads=[b_xr, b_PC, b_ii], writes=[b_ii])
                    S.op("dve", lambda e, d=d, h=h, xv=xs(3): e.scalar_tensor_tensor(out=xcb[:], in0=xv, scalar=PC[:, c_rcw(d, 3, h):c_rcw(d, 3, h) + 1],
                                                                          in1=ii[:], op0=ALU.mult, op1=ALU.add),
                         reads=[b_xr, b_PC, b_ii], writes=[b_xcb])
                    for g, (dst, b_dst) in enumerate(((rr, b_rr), (ii, b_ii))):
                        for n, (c0, cw) in enumerate(NCH):
                            q = cnt["g"] % 2; cnt["g"] += 1
                            S.op("pe", lambda e, q=q, c0=c0, cw=cw, d=d, g=g, gwb=gwb: e.matmul(pg[q][:, 0:cw], lhsT=gwb[:, d * 2 + g, :],
                                                                                      rhs=xcb[:, c0:c0 + cw], start=True, stop=True),
                                 reads=[b_gwb, b_xcb], writes=[b_pg[q]])
                            S.op("act", lambda e, q=q, c0=c0, cw=cw, d=d, g=g, h=h, dst=dst: e.activation(
                                out=dst[:, c0:c0 + cw], in_=pg[q][:, 0:cw], func=AF.Sigmoid,
                                bias=PC[:, c_rgb(d, g, h):c_rgb(d, g, h) + 1]), reads=[b_pg[q], b_PC], writes=[b_dst])
                    col = d * 8 + h
                    S.op("act", lambda e, col=col: e.activation(out=aa[:], in_=rr[:], func=AF.Exp, scale=CL[:, col:col + 1]),
                         reads=[b_rr, b_CL], writes=[b_aa])
                    S.op("act", lambda e, col=col: e.activation(out=rr[:], in_=rr[:], func=AF.Exp, scale=CL[:, 16 + col:17 + col]),
                         reads=[b_rr, b_CL], writes=[b_rr])
                    S.op("dve", lambda e: e.tensor_scalar(out=rr[:], in0=rr[:], scalar1=-1.0, scalar2=1.0, op0=ALU.mult, op1=ALU.add),
                         reads=[b_rr], writes=[b_rr])
                    S.op("dve", lambda e: e.tensor_scalar(out=rr[:], in0=rr[:], scalar1=0.0, scalar2=None, op0=ALU.max),
                         reads=[b_rr], writes=[b_rr])
                    S.op("act", lambda e: e.activation(out=rr[:], in_=rr[:], func=AF.Sqrt), reads=[b_rr], writes=[b_rr])
                    if d == 1 and h + 1 < 8:
                        prefetch(h + 1)
                    first = 0 if d == 0 else L - 1
                    S.op("dve", lambda e, first=first: e.memset(rr[:, first:first + 1], 1.0), writes=[b_rr])
                    S.op("dve", lambda e: e.tensor_tensor(out=ii[:], in0=ii[:], in1=rr[:], op=ALU.mult), reads=[b_ii, b_rr], writes=[b_ii])
                    S.op("dve", lambda e: e.tensor_tensor(out=ii[:], in0=ii[:], in1=xcb[:], op=ALU.mult), reads=[b_ii, b_xcb], writes=[b_ii])
                    if d == 0:
                        S.op("dve", lambda e: e.tensor_tensor_scan(out=hacc[:], data0=aa[:], data1=ii[:], initial=0.0,
                                                                   op0=ALU.mult, op1=ALU.add), reads=[b_aa, b_ii], writes=[b_ha])
                    else:
                        S.op("dve", lambda e: e.tensor_tensor_scan(out=rr[:, ::-1], data0=aa[:, ::-1], data1=ii[:, ::-1], initial=0.0,
                                                                   op0=ALU.mult, op1=ALU.add), reads=[b_aa, b_ii], writes=[b_rr])
                S.op("dve", lambda e: e.tensor_tensor(out=hacc[:], in0=hacc[:], in1=rr[:], op=ALU.add), reads=[b_ha, b_rr], writes=[b_ha])
                S.op("dve", lambda e, gg=gg: e.tensor_tensor(out=yb[:], in0=hacc[:], in1=gg[:], op=ALU.mult), reads=[b_ha, b_gg], writes=[b_yb])
                S.dma("sp", lambda e, h=h: e.dma_start(out=yrgT.ap()[h], in_=yb[:]), reads=[b_yb], writes=[P.dbufs["yrgT"]])
            S.barrier()

        with ExitStack() as st:
            sg = [P.sb("g_sg%d" % i, [128, L], BF16, st) for i in range(2)]; b_sg = [Buf(), Buf()]
            for m in range(16):
                k = m % 2
                project(28 + m, lambda p, bp, c0, cw, m=m, k=k: S.op("act", lambda e: e.activation(
                    out=sg[k][:, c0:c0 + cw], in_=p[:, 0:cw], func=AF.Sigmoid, bias=PC[:, c_bg(m):c_bg(m) + 1]),
                    reads=[bp, b_PC], writes=[b_sg[k]]))
                S.dma("sp", lambda e, m=m, k=k: e.dma_start(out=sgT.ap()[m], in_=sg[k][:]), reads=[b_sg[k]], writes=[P.dbufs["sgT"]])
            S.barrier()

        ztd = P.dscr("ztd", [128, NT, 512], BF16)
        with ExitStack() as st:
            zt = P.sb("h_zt", [128, NT, 512], BF16, st); b_zt = Buf()
            S.op("pool", lambda e: e.memset(zt[:, NT - 1, :], 0.0), writes=[b_zt])
            xa = P.sb("h_xa", [128, L + 2], F32, st); b_xa = Buf()
            ua = P.sb("h_ua", [128, L], F32, st); b_ua = Buf()
            ub = P.sb("h_ub", [128, L], F32, st); b_ub = Buf()
            zb = P.sb("h_zb", [128, LP], BF16, st); b_zb = Buf()
            S.op("dve", lambda e: e.memset(xa[:, 0:1], 0.0), writes=[b_xa])
            S.op("dve", lambda e: e.memset(xa[:, L + 1:L + 2], 0.0), writes=[b_xa])

            def conv3(cc, dst, b_dst):
                project(cc, lambda p, bp, c0, cw: S.op("act", lambda e: e.copy(out=xa[:, 1 + c0:1 + c0 + cw], in_=p[:, 0:cw]),
                                                      reads=[bp], writes=[b_xa]))
                S.op("dve", lambda e: e.tensor_scalar(out=dst[:], in0=xa[:, 1:L + 1], scalar1=PC[:, c_hcw(1, cc):c_hcw(1, cc) + 1],
                                                      scalar2=PC[:, c_hcb(cc):c_hcb(cc) + 1], op0=ALU.mult, op1=ALU.add),
                     reads=[b_xa, b_PC], writes=[b_dst])
                S.op("dve", lambda e: e.scalar_tensor_tensor(out=dst[:], in0=xa[:, 0:L], scalar=PC[:, c_hcw(0, cc):c_hcw(0, cc) + 1],
                                                             in1=dst[:], op0=ALU.mult, op1=ALU.add), reads=[b_xa, b_PC, b_dst], writes=[b_dst])
                S.op("dve", lambda e: e.scalar_tensor_tensor(out=dst[:], in0=xa[:, 2:L + 2], scalar=PC[:, c_hcw(2, cc):c_hcw(2, cc) + 1],
                                                             in1=dst[:], op0=ALU.mult, op1=ALU.add), reads=[b_xa, b_PC, b_dst], writes=[b_dst])

            for i in range(4):
                conv3(i, ua, b_ua)
                S.dma("sp", lambda e, i=i: e.dma_start(out=x0cT.ap()[i], in_=ua[:]), reads=[b_ua], writes=[P.dbufs["x0cT"]])
            for i in range(4):
                conv3(4 + i, ua, b_ua)
                conv3(8 + i, ub, b_ub)
                S.op("dve", lambda e: e.tensor_tensor(out=ua[:], in0=ua[:], in1=ub[:], op=ALU.mult), reads=[b_ua, b_ub], writes=[b_ua])
                S.dma("sp", lambda e, i=i: e.dma_start(out=zT.ap()[i], in_=ua[:]), reads=[b_ua], writes=[P.dbufs["zT"]])
                S.op("act", lambda e: e.copy(out=zb[:, 0:L], in_=ua[:]), reads=[b_ua], writes=[b_zb])
                for g0 in range(0, NT, 8):
                    g1 = min(NT, g0 + 8)
                    k = cnt["t"] % 2; cnt["t"] += 1
                    for t in range(g0, g1):
                        r = _rows(t)
                        S.op("pe", lambda e, t=t, r=r, k=k, g0=g0: e.transpose(pt[k][0:r, t - g0, :], zb[:, t * 128:t * 128 + r], identb[:]),
                             reads=[b_zb, b_idb], writes=[b_pt[k]])
                    nfull = len([t for t in range(g0, g1) if _rows(t) == 128])
                    if nfull:
                        S.op("act", lambda e, k=k, g0=g0, nfull=nfull, i=i: e.copy(out=zt[:, g0:g0 + nfull, i * 128:(i + 1) * 128],
                                                                                 in_=pt[k][:, 0:nfull, :]), reads=[b_pt[k]], writes=[b_zt])
                    if g1 == NT:
                        r = _rows(NT - 1)
                        S.op("act", lambda e, k=k, g0=g0, r=r, i=i: e.copy(out=zt[0:r, NT - 1, i * 128:(i + 1) * 128],
                                                                         in_=pt[k][0:r, NT - 1 - g0, :]), reads=[b_pt[k]], writes=[b_zt])
            S.dma("sp", lambda e: e.dma_start(out=ztd.ap(), in_=zt[:]), reads=[b_zt], writes=[P.dbufs["ztd"]])
            S.barrier()


def phase_dft(P, inp, G):
    nc, S = P.nc, P.S
    D_ = P.dram
    PC, b_PC = G["PC"], G["b_PC"]
    identf, b_idf = G["identf"], G["b_idf"]
    yyT, b_yyT = G["yyT"], G["b_yyT"]
    kspec = D_["kspec"]; bk = P.dbufs["kspec"]
    with ExitStack() as st:
        zt = P.sb("d_zt", [128, NT, 512], BF16, st); b_zt = Buf()
        S.dma("sp", lambda e: e.dma_start(out=zt[:], in_=D_["ztd"].ap()), reads=[P.dbufs["ztd"]], writes=[b_zt])
        Yre = P.sb("d_yre", [128, NT, 512], BF16, st); b_yre = Buf()
        Yim = P.sb("d_yim", [128, NT, 512], BF16, st); b_yim = Buf()
        tabc = [P.sb("d_tabc%d" % i, [128, NT * 128], BF16, st) for i in range(2)]
        tabs = [P.sb("d_tabs%d" % i, [128, NT * 128], BF16, st) for i in range(2)]
        b_tabc = [Buf(), Buf()]; b_tabs = [Buf(), Buf()]
        ksp = [P.sb("d_ksp%d" % i, [128, 2, 512], F32, st) for i in range(2)]; b_ksp = [Buf(), Buf()]
        t1 = P.sb("d_t1", [128, 512], F32, st); b_t1 = Buf()
        t2 = P.sb("d_t2", [128, 512], F32, st); b_t2 = Buf()
        pa = [P.ps("d_pa%d" % i, [128, 512], F32, st) for i in range(2)]; b_pa = [Buf(), Buf()]
        pb = [P.ps("d_pb%d" % i, [128, 512], F32, st) for i in range(2)]; b_pb = [Buf(), Buf()]
        ptf = [P.ps("d_ptf%d" % i, [128, 4, 128], F32, st) for i in range(2)]; b_ptf = [Buf(), Buf()]
        ysb = [P.sb("d_ysb%d" % i, [128, 512], F32, st) for i in range(2)]; b_ysb = [Buf(), Buf()]
        zx = [P.sb("d_zx%d" % i, [128, 2, 4, 128], F32, st) for i in range(2)]; b_zx = [Buf(), Buf()]

        def load_tabs(j, k):
            S.dma("sp", lambda e: e.dma_start(out=tabc[k][:], in_=D_["c_tabC"].ap()[j]), writes=[b_tabc[k]])
            S.dma("sp", lambda e: e.dma_start(out=tabs[k][:], in_=D_["c_tabS"].ap()[j]), writes=[b_tabs[k]])

        for j in range(NT):
            k = j % 2
            load_tabs(j, k)
            S.dma("sp", lambda e, j=j, k=k: e.dma_start(out=ksp[k][:], in_=kspec.ap()[j]), reads=[bk], writes=[b_ksp[k]])
            for (tab, b_tab, ps_, b_ps) in ((tabc, b_tabc, pa, b_pa), (tabs, b_tabs, pb, b_pb)):
                for i in range(NT):
                    r = _rows(i)
                    S.op("pe", lambda e, i=i, r=r, k=k, tab=tab, ps_=ps_: e.matmul(ps_[k][:], lhsT=tab[k][0:r, i * 128:(i + 1) * 128],
                                                                                  rhs=zt[0:r, i, :], start=(i == 0), stop=(i == NT - 1)),
                         reads=[b_tab[k], b_zt], writes=[b_ps[k]])
            S.op("dve", lambda e, k=k: e.tensor_tensor(out=t1[:], in0=pa[k][:], in1=ksp[k][:, 0, :], op=ALU.mult), reads=[b_pa[k], b_ksp[k]], writes=[b_t1])
            S.op("dve", lambda e, k=k: e.tensor_tensor(out=t2[:], in0=pb[k][:], in1=ksp[k][:, 1, :], op=ALU.mult), reads=[b_pb[k], b_ksp[k]], writes=[b_t2])
            S.op("dve", lambda e, j=j: e.tensor_tensor(out=Yre[:, j, :], in0=t1[:], in1=t2[:], op=ALU.subtract), reads=[b_t1, b_t2], writes=[b_yre])
            S.op("dve", lambda e, k=k: e.tensor_tensor(out=t1[:], in0=pb[k][:], in1=ksp[k][:, 0, :], op=ALU.mult), reads=[b_pb[k], b_ksp[k]], writes=[b_t1])
            S.op("dve", lambda e, k=k: e.tensor_tensor(out=t2[:], in0=pa[k][:], in1=ksp[k][:, 1, :], op=ALU.mult), reads=[b_pa[k], b_ksp[k]], writes=[b_t2])
            S.op("dve", lambda e, j=j: e.tensor_tensor(out=Yim[:, j, :], in0=t1[:], in1=t2[:], op=ALU.add), reads=[b_t1, b_t2], writes=[b_yim])

        rf = NF - 128 * (NT - 1)
        skip_b = PC[:, 48:52].unsqueeze(2).to_broadcast([128, 4, 128])
        zTd = D_["zT"].ap(); x0d = D_["x0cT"].ap()
        for j in range(NT):
            k = j % 2
            r = _rows(j)
            load_tabs(j, k)
            S.dma("sp", lambda e, j=j, k=k, r=r: e.dma_start(out=zx[k][:, 0, :, 0:r], in_=zTd[:, :, 128 * j:128 * j + r].rearrange("i p t -> p i t")),
                  reads=[P.dbufs["zT"]], writes=[b_zx[k]])
            S.dma("sp", lambda e, j=j, k=k, r=r: e.dma_start(out=zx[k][:, 1, :, 0:r], in_=x0d[:, :, 128 * j:128 * j + r].rearrange("i p t -> p i t")),
                  reads=[P.dbufs["x0cT"]], writes=[b_zx[k]])
            n_mm = 2 * NT
            c = 0
            for (tab, b_tab, Y, b_Y) in ((tabc, b_tabc, Yre, b_yre), (tabs, b_tabs, Yim, b_yim)):
                for i in range(NT):
                    rk = 128 if i < NT - 1 else rf
                    S.op("pe", lambda e, i=i, rk=rk, k=k, tab=tab, Y=Y, c=c: e.matmul(pa[k][:], lhsT=tab[k][0:rk, i * 128:(i + 1) * 128],
                                                                                     rhs=Y[0:rk, i, :], start=(c == 0), stop=(c == n_mm - 1)),
                         reads=[b_tab[k], b_Y], writes=[b_pa[k]])
                    c += 1
            S.op("act", lambda e, k=k: e.copy(out=ysb[k][:], in_=pa[k][:]), reads=[b_pa[k]], writes=[b_ysb[k]])
            for i in range(4):
                S.op("pe", lambda e, i=i, k=k, r=r: e.transpose(ptf[k][:, i, 0:r], ysb[k][0:r, i * 128:(i + 1) * 128], identf[0:r, 0:r]),
                     reads=[b_ysb[k], b_idf], writes=[b_ptf[k]])
            S.op("dve", lambda e, k=k, r=r: e.tensor_tensor(out=zx[k][:, 0, :, 0:r], in0=zx[k][:, 0, :, 0:r], in1=skip_b[:, :, 0:r], op=ALU.mult),
                 reads=[b_zx[k], b_PC], writes=[b_zx[k]])
            S.op("dve", lambda e, k=k, r=r: e.tensor_tensor(out=zx[k][:, 0, :, 0:r], in0=zx[k][:, 0, :, 0:r], in1=ptf[k][:, :, 0:r], op=ALU.add),
                 reads=[b_zx[k], b_ptf[k]], writes=[b_zx[k]])
            S.op("dve", lambda e, k=k, r=r, j=j: e.tensor_tensor(out=yyT[:, :, 128 * j:128 * j + r], in0=zx[k][:, 0, :, 0:r], in1=zx[k][:, 1, :, 0:r],
                                                                op=ALU.mult), reads=[b_zx[k]], writes=[b_yyT])
        S.barrier()


def phase_merge(P, inp, G):
    nc, S = P.nc, P.S
    D_ = P.dram
    yyT, b_yyT = G["yyT"], G["b_yyT"]
    mgT = P.dscr("mgT", [8, 128, L], BF16)
    hy_out = inp["hy_out"].ap()[0].rearrange("(k p) c -> p k c", p=128)
    rg_out = inp["rg_out"].ap()[0].rearrange("(k p) c -> p k c", p=128)
    with ExitStack() as st:
        yrg = P.sb("g_yrg", [128, 8, L], BF16, st); b_yrg = Buf()
        hws = P.sb("g_hws", [128, 4, 128], F32, st); b_hws = Buf()
        rws = P.sb("g_rws", [128, 8, 128], F32, st); b_rws = Buf()
        hwb = [P.sb("g_hwb%d" % i, [128, 4, 128], BF16, st) for i in range(2)]; b_hwb = [Buf(), Buf()]
        rwb = [P.sb("g_rwb%d" % i, [128, 8, 128], BF16, st) for i in range(2)]; b_rwb = [Buf(), Buf()]
        sga = [P.sb("g_sga%d" % i, [128, L], BF16, st) for i in range(2)]; b_sga = [Buf(), Buf()]
        sgb = [P.sb("g_sgb%d" % i, [128, L], BF16, st) for i in range(2)]; b_sgb = [Buf(), Buf()]
        mg = [P.sb("g_mg%d" % i, [128, L], BF16, st) for i in range(2)]; b_mg = [Buf(), Buf()]
        t1 = P.sb("g_t1", [128, 512], F32, st); b_t1 = Buf()
        t2 = P.sb("g_t2", [128, 512], F32, st); b_t2 = Buf()
        ph = [P.ps("g_ph%d" % i, [128, 512], F32, st) for i in range(2)]; b_ph = [Buf(), Buf()]
        pr = [P.ps("g_pr%d" % i, [128, 512], F32, st) for i in range(2)]; b_pr = [Buf(), Buf()]
        for h in range(8):
            S.dma("sp", lambda e, h=h: e.dma_start(out=yrg[:, h, :], in_=D_["yrgT"].ap()[h]), reads=[P.dbufs["yrgT"]], writes=[b_yrg])
        c = 0
        for m in range(8):
            k = m % 2
            S.dma("sp", lambda e, m=m: e.dma_start(out=hws[:], in_=hy_out[:, :, m * 128:(m + 1) * 128]), writes=[b_hws])
            S.dma("sp", lambda e, m=m: e.dma_start(out=rws[:], in_=rg_out[:, :, m * 128:(m + 1) * 128]), writes=[b_rws])
            S.op("pool", lambda e, k=k: e.tensor_copy(out=hwb[k][:], in_=hws[:]), reads=[b_hws], writes=[b_hwb[k]])
            S.op("pool", lambda e, k=k: e.tensor_copy(out=rwb[k][:], in_=rws[:]), reads=[b_rws], writes=[b_rwb[k]])
            S.dma("sp", lambda e, m=m, k=k: e.dma_start(out=sga[k][:], in_=D_["sgT"].ap()[m]), reads=[P.dbufs["sgT"]], writes=[b_sga[k]])
            S.dma("sp", lambda e, m=m, k=k: e.dma_start(out=sgb[k][:], in_=D_["sgT"].ap()[8 + m]), reads=[P.dbufs["sgT"]], writes=[b_sgb[k]])
            for n, (c0, cw) in enumerate(NCH):
                q = c % 2; c += 1
                for i in range(4):
                    S.op("pe", lambda e, i=i, q=q, k=k, c0=c0, cw=cw: e.matmul(ph[q][:, 0:cw], lhsT=hwb[k][:, i, :], rhs=yyT[:, i, c0:c0 + cw],
                                                                                start=(i == 0), stop=(i == 3)),
                         reads=[b_hwb[k], b_yyT], writes=[b_ph[q]])
                for i in range(8):
                    S.op("pe", lambda e, i=i, q=q, k=k, c0=c0, cw=cw: e.matmul(pr[q][:, 0:cw], lhsT=rwb[k][:, i, :], rhs=yrg[:, i, c0:c0 + cw],
                                                                                start=(i == 0), stop=(i == 7)),
                         reads=[b_rwb[k], b_yrg], writes=[b_pr[q]])
                S.op("dve", lambda e, q=q, k=k, c0=c0, cw=cw: e.tensor_tensor(out=t1[:, 0:cw], in0=ph[q][:, 0:cw], in1=sga[k][:, c0:c0 + cw], op=ALU.mult),
                     reads=[b_ph[q], b_sga[k]], writes=[b_t1])
                S.op("dve", lambda e, q=q, k=k, c0=c0, cw=cw: e.tensor_tensor(out=t2[:, 0:cw], in0=pr[q][:, 0:cw], in1=sgb[k][:, c0:c0 + cw], op=ALU.mult),
                     reads=[b_pr[q], b_sgb[k]], writes=[b_t2])
                S.op("dve", lambda e, k=k, c0=c0, cw=cw: e.tensor_tensor(out=mg[k][:, c0:c0 + cw], in0=t1[:, 0:cw], in1=t2[:, 0:cw], op=ALU.add),
                     reads=[b_t1, b_t2], writes=[b_mg[k]])
            S.dma("sp", lambda e, m=m, k=k: e.dma_start(out=mgT.ap()[m], in_=mg[k][:]), reads=[b_mg[k]], writes=[P.dbufs["mgT"]])
        S.barrier()


def phase_post(P, inp, G):
    nc, S = P.nc, P.S
    D_ = P.dram
    identb, b_idb = G["identb"], G["b_idb"]
    n2Td = P.dscr("n2Td", [128, 8, SEQ], BF16)
    h2d = P.dscr("h2d", [SEQ, D], F32)
    n2d = P.dscr("n2d", [SEQ, D], F32)
    x = inp["x"].ap()
    w_out = inp["w_out"].ap()[0].rearrange("(k p) c -> p k c", p=128)
    with ExitStack() as st:
        mg = P.sb("p_mg", [128, 8, L], BF16, st); b_mg = Buf()
        wos = P.sb("p_wos", [128, D], F32, st); b_wos = Buf()
        wob = P.sb("p_wob", [128, 8, D], BF16, st); b_wob = Buf()
        gf = P.sb("p_gf", [128, D], F32, st); b_gf = Buf()
        xt = [P.sb("p_xt%d" % i, [128, D], F32, st) for i in range(2)]; b_xt = [Buf(), Buf()]
        h2 = [P.sb("p_h2%d" % i, [128, D], F32, st) for i in range(2)]; b_h2 = [Buf(), Buf()]
        n2 = [P.sb("p_n2%d" % i, [128, D], F32, st) for i in range(2)]; b_n2 = [Buf(), Buf()]
        n2b = [P.sb("p_n2b%d" % i, [128, D], BF16, st) for i in range(2)]; b_n2b = [Buf(), Buf()]
        sq = P.sb("p_sq", [128, D], F32, st); b_sq = Buf()
        ssq = P.sb("p_ss", [128, 64], F32, st); b_ss = Buf()
        pm = [P.ps("p_pm%d" % i, [128, 2, 512], F32, st) for i in range(2)]; b_pm = [Buf(), Buf()]
        pt = [P.ps("p_pt%d" % i, [128, 8, 128], BF16, st) for i in range(2)]; b_pt = [Buf(), Buf()]
        n2Tt = [P.sb("p_n2Tt%d" % i, [128, 8, 128], BF16, st) for i in range(2)]; b_n2Tt = [Buf(), Buf()]
        for kk in range(8):
            S.dma("sp", lambda e, kk=kk: e.dma_start(out=mg[:, kk, :], in_=D_["mgT"].ap()[kk]), reads=[P.dbufs["mgT"]], writes=[b_mg])
            S.dma("sp", lambda e, kk=kk: e.dma_start(out=wos[:], in_=w_out[:, kk, :]), writes=[b_wos])
            S.op("pool", lambda e, kk=kk: e.tensor_copy(out=wob[:, kk, :], in_=wos[:]), reads=[b_wos], writes=[b_wob])
        S.dma("sp", lambda e: e.dma_start(out=gf[:], in_=inp["g_ffn"].ap().partition_broadcast(128)), writes=[b_gf])
        for j in range(32):
            k = j % 2
            p0 = NMETA + 128 * j
            S.dma("sp", lambda e, j=j, k=k: e.dma_start(out=xt[k][:], in_=x[128 * j:128 * (j + 1), :]), writes=[b_xt[k]])
            for half in range(2):
                for kk in range(8):
                    S.op("pe", lambda e, kk=kk, half=half, k=k, p0=p0: e.matmul(pm[k][:, half, :], lhsT=mg[:, kk, p0:p0 + 128],
                                                                                 rhs=wob[:, kk, half * 512:(half + 1) * 512],
                                                                                 start=(kk == 0), stop=(kk == 7)),
                         reads=[b_mg, b_wob], writes=[b_pm[k]])
            S.op("dve", lambda e, k=k: e.tensor_tensor(out=h2[k][:], in0=xt[k][:], in1=pm[k][:].rearrange("p a c -> p (a c)"), op=ALU.add),
                 reads=[b_xt[k], b_pm[k]], writes=[b_h2[k]])
            S.dma("sp", lambda e, j=j, k=k: e.dma_start(out=h2d.ap()[128 * j:128 * (j + 1), :], in_=h2[k][:]), reads=[b_h2[k]], writes=[P.dbufs["h2d"]])
            S.op("act", lambda e, k=k, j=j: e.activation(out=sq[:], in_=h2[k][:], func=AF.Square, accum_out=ssq[:, 2 * j:2 * j + 1]),
                 reads=[b_h2[k]], writes=[b_sq, b_ss])
            S.op("act", lambda e, j=j: e.activation(out=ssq[:, 2 * j + 1:2 * j + 2], in_=ssq[:, 2 * j:2 * j + 1], func=AF.Sqrt, scale=1.0 / D, bias=EPS),
                 reads=[b_ss], writes=[b_ss])
            S.op("dve", lambda e, j=j: e.reciprocal(out=ssq[:, 2 * j + 1:2 * j + 2], in_=ssq[:, 2 * j + 1:2 * j + 2]), reads=[b_ss], writes=[b_ss])
            S.op("dve", lambda e, k=k, j=j: e.scalar_tensor_tensor(out=n2[k][:], in0=h2[k][:], scalar=ssq[:, 2 * j + 1:2 * j + 2], in1=gf[:],
                                                                   op0=ALU.mult, op1=ALU.mult), reads=[b_h2[k], b_ss, b_gf], writes=[b_n2[k]])
            S.dma("sp", lambda e, j=j, k=k: e.dma_start(out=n2d.ap()[128 * j:128 * (j + 1), :], in_=n2[k][:]), reads=[b_n2[k]], writes=[P.dbufs["n2d"]])
            S.op("act", lambda e, k=k: e.copy(out=n2b[k][:], in_=n2[k][:]), reads=[b_n2[k]], writes=[b_n2b[k]])
            for c in range(8):
                S.op("pe", lambda e, k=k, c=c: e.transpose(pt[k][:, c, :], n2b[k][:, c * 128:(c + 1) * 128], identb[:]),
                     reads=[b_n2b[k], b_idb], writes=[b_pt[k]])
            S.op("act", lambda e, k=k: e.copy(out=n2Tt[k][:], in_=pt[k][:]), reads=[b_pt[k]], writes=[b_n2Tt[k]])
            S.dma("sp", lambda e, k=k, j=j: e.dma_start(out=n2Td.ap()[:, :, 128 * j:128 * (j + 1)], in_=n2Tt[k][:]), reads=[b_n2Tt[k]],
                  writes=[P.dbufs["n2Td"]])
        S.barrier()


def phase_peer_scores(P, inp, G):
    nc, S = P.nc, P.S
    identb, b_idb = G["identb"], G["b_idb"]
    TS, b_TS, TIf, b_TIf = G["TS"], G["b_TS"], G["TIf"], G["b_TIf"]
    wq = inp["peer_wq"].ap()[0].rearrange("(k p) c -> p k c", p=128)
    keys = inp["peer_keys"].ap()[0]
    with ExitStack() as st:
        n2T = P.sb("s_n2T", [128, 8, SEQ], BF16, st); b_n2T = Buf()
        for kk in range(8):
            S.dma("sp", lambda e, kk=kk: e.dma_start(out=n2T[:, kk, :], in_=P.dram["n2Td"].ap()[:, kk, :]), reads=[P.dbufs["n2Td"]], writes=[b_n2T])
        TIu = P.sb("s_tiu", [128, 32, 16, 16], U32, st); b_TIu = Buf()
        wqs = P.sb("s_wqs", [128, 8, 128], F32, st); b_wqs = Buf()
        wqb = [P.sb("s_wqb%d" % i, [128, 8, 128], BF16, st) for i in range(2)]; b_wqb = [Buf(), Buf()]
        kys = P.sb("s_kys", [128, 128], F32, st); b_kys = Buf()
        kyb = P.sb("s_kyb", [128, 128], BF16, st); b_kyb = Buf()
        kyT = [P.sb("s_kyT%d" % i, [128, 128], BF16, st) for i in range(2)]; b_kyT = [Buf(), Buf()]
        qTb = [P.sb("s_qT%d" % i, [128, SEQ], BF16, st) for i in range(2)]; b_qT = [Buf(), Buf()]
        sc = [P.sb("s_sc%d" % i, [128, 4, 128], F32, st) for i in range(2)]; b_sc = [Buf(), Buf()]
        sc2 = [P.sb("s_sc2_%d" % i, [128, 128], F32, st) for i in range(2)]; b_sc2 = [Buf(), Buf()]
        b_TSx = [Buf(), Buf()]; b_TIx = [Buf(), Buf()]
        pq = [P.ps("s_pq%d" % i, [128, 512], F32, st) for i in range(2)]; b_pq = [Buf(), Buf()]
        psc = [P.ps("s_ps%d" % i, [128, 4, 128], F32, st) for i in range(2)]; b_psc = [Buf(), Buf()]
        pkt = P.ps("s_pkt", [128, 128], BF16, st); b_pkt = Buf()
        c = 0; c2 = 0
        for qc in range(16):
            k = qc % 2
            S.dma("sp", lambda e, qc=qc: e.dma_start(out=wqs[:], in_=wq[:, :, qc * 128:(qc + 1) * 128]), writes=[b_wqs])
            S.op("pool", lambda e, k=k: e.tensor_copy(out=wqb[k][:], in_=wqs[:]), reads=[b_wqs], writes=[b_wqb[k]])
            S.dma("sp", lambda e, qc=qc: e.dma_start(out=kys[:], in_=keys[qc // 2, qc % 2]), writes=[b_kys])
            S.op("pool", lambda e: e.tensor_copy(out=kyb[:], in_=kys[:]), reads=[b_kys], writes=[b_kyb])
            S.op("pe", lambda e: e.transpose(pkt[:], kyb[:], identb[:]), reads=[b_kyb, b_idb], writes=[b_pkt])
            S.op("act", lambda e, k=k: e.copy(out=kyT[k][:], in_=pkt[:]), reads=[b_pkt], writes=[b_kyT[k]])
            for n in range(8):
                q = c % 2; c += 1
                for kk in range(8):
                    S.op("pe", lambda e, kk=kk, q=q, k=k, n=n: e.matmul(pq[q][:], lhsT=wqb[k][:, kk, :], rhs=n2T[:, kk, n * 512:(n + 1) * 512],
                                                                        start=(kk == 0), stop=(kk == 7)),
                         reads=[b_wqb[k], b_n2T], writes=[b_pq[q]])
                S.op("act", lambda e, q=q, k=k, n=n: e.copy(out=qTb[k][:, n * 512:(n + 1) * 512], in_=pq[q][:]), reads=[b_pq[q]], writes=[b_qT[k]])
            for jg in range(8):
                q = c2 % 2; c2 += 1
                for jj in range(4):
                    j = jg * 4 + jj
                    S.op("pe", lambda e, q=q, k=k, j=j, jj=jj: e.matmul(psc[q][:, jj, :], lhsT=qTb[k][:, 128 * j:128 * (j + 1)], rhs=kyT[k][:],
                                                                        start=True, stop=True),
                         reads=[b_qT[k], b_kyT[k]], writes=[b_psc[q]])
                S.op("act", lambda e, q=q: e.copy(out=sc[q][:], in_=psc[q][:]), reads=[b_psc[q]], writes=[b_sc[q]])
                for jp in range(0, 4, 2):
                    pair = [(jg * 4 + jp + u, jp + u, u) for u in range(2)]
                    for (j, jj, u) in pair:
                        S.op("dve", lambda e, q=q, j=j, jj=jj, qc=qc: e.max(out=TS[:, j, qc, 0:8], in_=sc[q][:, jj, :]), reads=[b_sc[q]], writes=[b_TSx[u]])
                    for (j, jj, u) in pair:
                        S.op("dve", lambda e, q=q, j=j, jj=jj, qc=qc: e.max_index(out=TIu[:, j, qc, 0:8], in_max=TS[:, j, qc, 0:8], in_values=sc[q][:, jj, :]),
                             reads=[b_sc[q], b_TSx[u]], writes=[b_TIx[u]])
                    for (j, jj, u) in pair:
                        S.op("dve", lambda e, q=q, j=j, jj=jj, qc=qc, u=u: e.match_replace(out=sc2[u][:], in_to_replace=TS[:, j, qc, 0:8], in_values=sc[q][:, jj, :],
                                                                                      imm_value=-1e30), reads=[b_sc[q], b_TSx[u]], writes=[b_sc2[u]])
                    for (j, jj, u) in pair:
                        S.op("dve", lambda e, j=j, qc=qc, u=u: e.max(out=TS[:, j, qc, 8:16], in_=sc2[u][:]), reads=[b_sc2[u]], writes=[b_TSx[u]])
                    for (j, jj, u) in pair:
                        S.op("dve", lambda e, j=j, qc=qc, u=u: e.max_index(out=TIu[:, j, qc, 8:16], in_max=TS[:, j, qc, 8:16], in_values=sc2[u][:]),
                             reads=[b_sc2[u], b_TSx[u]], writes=[b_TIx[u]])
        for u in range(2):
            S.op("dve", lambda e: e.engine_nop(), reads=[b_TSx[u], b_TIx[u]], writes=[b_TS, b_TIu])
        S.op("dve", lambda e: e.tensor_copy(out=TIf[:].rearrange("p a b c -> p (a b c)"), in_=TIu[:].rearrange("p a b c -> p (a b c)")),
             reads=[b_TIu], writes=[b_TIf])
        S.barrier()


def phase_peer_out(P, inp, G, out):
    nc, S = P.nc, P.S
    D_ = P.dram
    TS, b_TS, TIf, b_TIf = G["TS"], G["b_TS"], G["TIf"], G["b_TIf"]
    utab = inp["peer_u"].ap()[0]
    vtab = inp["peer_v"].ap()[0]
    h2d = D_["h2d"].ap(); n2d = D_["n2d"].ap()
    NEG = -1e30
    with ExitStack() as st:
        iota4 = P.sb("o_iota", [128, 8, 16, 16], F32, st); b_iota = Buf()
        io1 = P.sb("o_io1", [128, 16], I32, st); b_io1 = Buf()
        gfin = P.sb("o_gfin", [128, D], F32, st); b_gfin = Buf()
        cand = P.sb("o_cand", [128, 8, 256], F32, st); b_cand = Buf()
        cand2 = P.sb("o_cand2", [128, 256], F32, st); b_cand2 = Buf()
        cs = P.sb("o_cs", [128, 8, 16], F32, st); b_cs = Buf()
        ci = P.sb("o_ci", [128, 8, 16], U32, st); b_ci = Buf()
        ik = P.sb("o_ik", [128, 8, 16], U32, st); b_ik = Buf()
        jk = P.sb("o_jk", [128, 8, 16], U32, st); b_jk = Buf()
        ikf = P.sb("o_ikf", [128, 8, 16], F32, st); b_ikf = Buf()
        jkf = P.sb("o_jkf", [128, 8, 16], F32, st); b_jkf = Buf()
        oh = P.sb("o_oh", [128, 8, 16, 16], F32, st); b_oh = Buf()
        e1 = P.sb("o_e1", [128, 8, 16], F32, st); b_e1 = Buf()
        e2 = P.sb("o_e2", [128, 8, 16], F32, st); b_e2 = Buf()
        EI = [P.sb("o_ei%d" % i, [128, 128], U32, st) for i in range(2)]; b_EI = [Buf(), Buf()]
        gt = [P.sb("o_g%d" % i, [128, 8, 16], F32, st) for i in range(2)]; b_gt = [Buf(), Buf()]
        sm = P.sb("o_sm", [128, 16], F32, st); b_sm = Buf()
        scr = P.sb("o_scr", [128, 128], F32, st); b_scrc = [Buf() for _ in range(128)]
        act = P.sb("o_act", [128, 128], F32, st); b_actc = [Buf() for _ in range(128)]
        agc = P.sb("o_agc", [128, 128], F32, st); b_agc = [Buf() for _ in range(128)]
        NG = 16
        ug = [P.sb("o_ug%d" % i, [128, 2 * D], BF16, st) for i in range(NG)]; b_ug = [Buf() for _ in range(NG)]
        ND = 8
        dg = [P.sb("o_dg%d" % i, [128, 128], BF16, st) for i in range(ND)]; b_dg = [Buf() for _ in range(ND)]
        pacc = [P.ps("o_pacc%d" % i, [128, 2, 512], F32, st) for i in range(2)]; b_pacc = [Buf(), Buf()]
        identf, b_idf = G["identf"], G["b_idf"]
        uvt = D_["uvtab"].ap()
        junk = P.sb("o_junk", [128, D], F32, st); b_junk = Buf()
        djunk = P.ps("o_djunk", [128, D], F32, st)
        n2t = [P.sb("o_n2%d" % i, [128, D], F32, st) for i in range(2)]; b_n2t = [Buf(), Buf()]
        h2t = [P.sb("o_h2", [128, D], F32, st)] * 2; b_h2t = [Buf()] * 2
        acc = P.sb("o_acc", [128, D], F32, st); b_acc = Buf()
        ssq = P.sb("o_ss", [128, 64], F32, st); b_ss = Buf()

        S.op("pool", lambda e: e.iota(io1[:], pattern=[[1, 16]], base=0, channel_multiplier=0), writes=[b_io1])
        S.op("dve", lambda e: e.tensor_copy(out=iota4[:].rearrange("p a b c -> p (a b) c"),
                                            in_=io1[:].unsqueeze(1).to_broadcast([128, 128, 16])), reads=[b_io1], writes=[b_iota])
        S.dma("sp", lambda e: e.dma_start(out=gfin[:], in_=inp["g_final"].ap().partition_broadcast(128)), writes=[b_gfin])
        gcount = 0

        pend = []

        def QO(*a, **kw):
            pend.append((S.op, a, kw))

        def QD(*a, **kw):
            pend.append((S.dma, a, kw))

        def drain(n):
            for _ in range(min(n, len(pend))):
                f, a, kw = pend.pop(0)
                f(*a, **kw)

        def select(j):
            k = j % 2
            QD("sp", lambda e, j=j, k=k: e.dma_start(out=n2t[k][:], in_=n2d[128 * j:128 * (j + 1), :]), reads=[P.dbufs["n2d"]], writes=[b_n2t[k]])
            tsj = TS[:, j].rearrange("p (h two) i -> p h two i", two=2)
            tij = TIf[:, j].rearrange("p (h two) i -> p h two i", two=2)
            QO("dve", lambda e, tsj=tsj: e.tensor_tensor(out=cand[:].rearrange("p h (a b) -> p h a b", a=16),
                                                           in0=tsj[:, :, 0, :].unsqueeze(3).to_broadcast([128, 8, 16, 16]),
                                                           in1=tsj[:, :, 1, :].unsqueeze(2).to_broadcast([128, 8, 16, 16]), op=ALU.add),
                 reads=[b_TS], writes=[b_cand])
            for h in range(8):
                QO("dve", lambda e, h=h: e.max(out=cs[:, h, 0:8], in_=cand[:, h, :]), reads=[b_cand], writes=[b_cs])
                QO("dve", lambda e, h=h: e.max_index(out=ci[:, h, 0:8], in_max=cs[:, h, 0:8], in_values=cand[:, h, :]), reads=[b_cand, b_cs], writes=[b_ci])
                QO("dve", lambda e, h=h: e.match_replace(out=cand2[:], in_to_replace=cs[:, h, 0:8], in_values=cand[:, h, :], imm_value=NEG),
                     reads=[b_cand, b_cs], writes=[b_cand2])
                QO("dve", lambda e, h=h: e.max(out=cs[:, h, 8:16], in_=cand2[:]), reads=[b_cand2], writes=[b_cs])
                QO("dve", lambda e, h=h: e.max_index(out=ci[:, h, 8:16], in_max=cs[:, h, 8:16], in_values=cand2[:]), reads=[b_cand2, b_cs], writes=[b_ci])
            QO("dve", lambda e: e.tensor_single_scalar(out=ik[:], in_=ci[:], scalar=4, op=ALU.logical_shift_right), reads=[b_ci], writes=[b_ik])
            QO("dve", lambda e: e.tensor_single_scalar(out=jk[:], in_=ci[:], scalar=15, op=ALU.bitwise_and), reads=[b_ci], writes=[b_jk])
            QO("dve", lambda e: e.tensor_copy(out=ikf[:], in_=ik[:]), reads=[b_ik], writes=[b_ikf])
            QO("dve", lambda e: e.tensor_copy(out=jkf[:], in_=jk[:]), reads=[b_jk], writes=[b_jkf])
            for (kf_, b_kf_, half, dst, b_dst) in ((ikf, b_ikf, 0, e1, b_e1), (jkf, b_jkf, 1, e2, b_e2)):
                QO("dve", lambda e, kf_=kf_: e.tensor_tensor(out=oh[:], in0=iota4[:], in1=kf_[:].unsqueeze(3).to_broadcast([128, 8, 16, 16]),
                                                               op=ALU.is_equal), reads=[b_iota, b_kf_], writes=[b_oh])
                QO("dve", lambda e, half=half, tij=tij: e.tensor_tensor(out=oh[:], in0=oh[:],
                                                                          in1=tij[:, :, half, :].unsqueeze(2).to_broadcast([128, 8, 16, 16]), op=ALU.mult),
                     reads=[b_oh, b_TIf], writes=[b_oh])
                QO("dve", lambda e, dst=dst: e.tensor_reduce(out=dst[:], in_=oh[:], axis=AX.X, op=ALU.add), reads=[b_oh], writes=[b_dst])
            QO("dve", lambda e: e.scalar_tensor_tensor(out=e1[:], in0=e1[:], scalar=128.0, in1=e2[:], op0=ALU.mult, op1=ALU.add),
                 reads=[b_e1, b_e2], writes=[b_e1])
            QO("dve", lambda e, k=k: e.tensor_copy(out=EI[k][:], in_=e1[:].rearrange("p h k -> p (h k)")), reads=[b_e1], writes=[b_EI[k]])
            QO("dve", lambda e, k=k: e.tensor_tensor(out=gt[k][:], in0=cs[:], in1=cs[:, :, 0:1].to_broadcast([128, 8, 16]), op=ALU.subtract),
                 reads=[b_cs], writes=[b_gt[k]])
            QO("act", lambda e, k=k: e.activation(out=gt[k][:], in_=gt[k][:], func=AF.Exp), reads=[b_gt[k]], writes=[b_gt[k]])
            QO("dve", lambda e, k=k: e.tensor_reduce(out=sm[:, 0:8], in_=gt[k][:], axis=AX.X, op=ALU.add), reads=[b_gt[k]], writes=[b_sm])
            QO("dve", lambda e: e.reciprocal(out=sm[:, 8:16], in_=sm[:, 0:8]), reads=[b_sm], writes=[b_sm])
            QO("dve", lambda e, k=k: e.tensor_tensor(out=gt[k][:], in0=gt[k][:], in1=sm[:, 8:16].unsqueeze(2).to_broadcast([128, 8, 16]), op=ALU.mult),
                 reads=[b_gt[k], b_sm], writes=[b_gt[k]])

        pool_aligned = (len(S.dma_pool["pool"]) == NG)
        S.dma_next["pool"] = 0
        select(0)
        drain(len(pend))
        for j in range(32):
            k = j % 2
            if j + 1 < 32:
                select(j + 1)
            per_hk = (len(pend) + 95) // 96
            S.dma("sp", lambda e, j=j, k=k: e.dma_start(out=h2t[k][:], in_=h2d[128 * j:128 * (j + 1), :]), reads=[P.dbufs["h2d"]], writes=[b_h2t[k]])
            gflat = gt[k][:].rearrange("p h k -> p (h k)")
            for hk in range(128):
                drain(per_hk)
                g = gcount % NG; dsl = gcount % ND; gcount += 1
                S.dma("pool", lambda e, g=g, k=k, hk=hk: e.indirect_dma_start(out=ug[g][:], out_offset=None, in_=uvt,
                                                                              in_offset=IndirectOffsetOnAxis(ap=EI[k][:, hk:hk + 1], axis=0)),
                      reads=[b_EI[k], P.dbufs["uvtab"]], writes=[b_ug[g]], slot_covered=(gcount > NG and pool_aligned))
                S.op("dve", lambda e, g=g, k=k, hk=hk: e.scalar_tensor_tensor(out=djunk[:], in0=ug[g][:, 0:D], scalar=1.0, in1=n2t[k][:], op0=ALU.mult,
                                                                              op1=ALU.mult, accum_out=scr[:, hk:hk + 1]),
                     reads=[b_n2t[k]], weak_reads=[b_ug[g]], writes=[b_scrc[hk]])
                S.op("act", lambda e, hk=hk: e.activation(out=act[:, hk:hk + 1], in_=scr[:, hk:hk + 1], func=AF.Gelu_apprx_tanh),
                     reads=[b_scrc[hk]], writes=[b_actc[hk]])
                S.op("act", lambda e, hk=hk, gflat=gflat: e.activation(out=agc[:, hk:hk + 1], in_=act[:, hk:hk + 1], func=AF.Identity,
                                                                      scale=gflat[:, hk:hk + 1]),
                     reads=[b_actc[hk], b_gt[k]], writes=[b_agc[hk]])
                S.op("act", lambda e, hk=hk, dsl=dsl: e.activation(out=dg[dsl][:], in_=identf[:], func=AF.Identity, scale=agc[:, hk:hk + 1]),
                     reads=[b_idf, b_agc[hk]], writes=[b_dg[dsl]])
                for half in range(2):
                    S.op("pe", lambda e, g=g, k=k, hk=hk, dsl=dsl, half=half: e.matmul(pacc[k][:, half, :], lhsT=dg[dsl][:],
                                                                                      rhs=ug[g][:, D + half * 512:D + (half + 1) * 512],
                                                                                      start=(hk == 0), stop=(hk == 127)),
                         reads=[b_dg[dsl], b_ug[g]], writes=[b_pacc[k]])
            drain(len(pend))
            S.op("dve", lambda e, k=k: e.tensor_tensor(out=acc[:], in0=h2t[k][:], in1=pacc[k][:].rearrange("p a c -> p (a c)"), op=ALU.add),
                 reads=[b_h2t[k], b_pacc[k]], writes=[b_acc])
            S.op("act", lambda e, j=j: e.activation(out=junk[:], in_=acc[:], func=AF.Square, accum_out=ssq[:, 2 * j:2 * j + 1]),
                 reads=[b_acc], writes=[b_junk, b_ss])
            S.op("act", lambda e, j=j: e.activation(out=ssq[:, 2 * j + 1:2 * j + 2], in_=ssq[:, 2 * j:2 * j + 1], func=AF.Sqrt, scale=1.0 / D, bias=EPS),
                 reads=[b_ss], writes=[b_ss])
            S.op("dve", lambda e, j=j: e.reciprocal(out=ssq[:, 2 * j + 1:2 * j + 2], in_=ssq[:, 2 * j + 1:2 * j + 2]), reads=[b_ss], writes=[b_ss])
            S.op("dve", lambda e, k=k, j=j: e.scalar_tensor_tensor(out=junk[:], in0=acc[:], scalar=ssq[:, 2 * j + 1:2 * j + 2], in1=gfin[:],
                                                                   op0=ALU.mult, op1=ALU.mult), reads=[b_acc, b_ss, b_gfin], writes=[b_junk])
            S.dma("sp", lambda e, j=j, k=k: e.dma_start(out=out.ap()[128 * j:128 * (j + 1), :], in_=junk[:]), reads=[b_junk], writes=[P.dbufs["out"]])
        S.barrier()


INPUT_SPECS = [
    ("x", [SEQ, D]), ("meta", [NMETA, D]), ("g_final", [1, D]), ("g_mix", [1, D]), ("w_in", [1, D, D_IN]),
    ("b_gate", [1, 2 * D]), ("hy_conv_w", [1, 3, 3 * D_HY]), ("hy_conv_b", [1, 3 * D_HY]), ("hy_w1", [1, 33, 64]),
    ("hy_b1", [1, 64]), ("hy_w2", [1, 64, 64]), ("hy_b2", [1, 64]), ("hy_w3", [1, 64, 1024]), ("hy_b3", [1, 1024]),
    ("hy_freq", [1, 2, 64]), ("hy_skip", [1, D_HY]), ("hy_out", [1, D_HY, D]), ("rg_conv_w", [1, 2, 4, D_RG]),
    ("rg_conv_b", [1, 2, D_RG]), ("rg_gate_w", [1, 2, 2, 8, 128, 128]), ("rg_gate_b", [1, 2, 2, D_RG]),
    ("rg_lambda", [1, 2, D_RG]), ("rg_out", [1, D_RG, D]), ("w_out", [1, D, D]), ("g_ffn", [1, D]),
    ("peer_wq", [1, D, 2048]), ("peer_keys", [1, 8, 2, 128, 128]), ("peer_u", [1, 16384, D]), ("peer_v", [1, 16384, D]),
]
CONST_SPECS = [
    ("c_zT", [33, L], F32), ("c_negdelta", [128, 4], F32), ("c_tlin", [1, L], F32), ("c_wf", [128, NT], F32),
    ("c_tabC", [NT, 128, NT * 128], BF16), ("c_tabS", [NT, 128, NT * 128], BF16),
    ("c_identf", [128, 128], F32), ("c_identb", [128, 128], BF16),
]


def _dump(P, src_ap_fn, nrows_tiles, out, width=D, dtype=F32):
    S = P.S
    with ExitStack() as st:
        t = P.sb("dbg_t", [128, width], dtype, st); bt = Buf()
        for j in range(nrows_tiles):
            S.dma("sp", lambda e, j=j: e.dma_start(out=t[:], in_=src_ap_fn(j)), reads=list(P.dbufs.values()), writes=[bt])
            S.dma("sp", lambda e, j=j: e.dma_start(out=out.ap()[128 * j:128 * (j + 1), 0:width], in_=t[:]), reads=[bt], writes=[P.dbufs["out"]])


def build(stage="all"):
    P = Prog()
    nc = P.nc
    inp = {}
    for name, shape in INPUT_SPECS:
        inp[name] = P.din(name, shape, F32)
    for name, shape, dt in CONST_SPECS:
        P.din(name, shape, dt)
    out = P.dout("out", [SEQ, D], F32)
    G = {}
    with P.stack:
        P.S = S = Sched(nc, P.stack)
        phase_filter(P, inp, uv_hook=lambda st: phase_uvtab(P, inp, st))
        phase_params(P, inp, G)
        with ExitStack() as s_nT:
            G["nT"] = P.sb("nT", [128, 8, LP], BF16, s_nT); G["b_nT"] = Buf()
            phase_norm(P, inp, G)
            phase_mixer_in(P, inp, G)
        if stage == "mixin":
            S.finish("sp"); S.emit(); return P
        with ExitStack() as s_yy:
            G["yyT"] = P.sb("yyT", [128, 4, LP], BF16, s_yy); G["b_yyT"] = Buf()
            phase_dft(P, inp, G)
            phase_merge(P, inp, G)
        with ExitStack() as s_ts:
            phase_post(P, inp, G)
            G["TS"] = P.sb("TS", [128, 32, 16, 16], F32, s_ts); G["b_TS"] = Buf()
            G["TIf"] = P.sb("TIf", [128, 32, 16, 16], F32, s_ts); G["b_TIf"] = Buf()
            phase_peer_scores(P, inp, G)
            phase_peer_out(P, inp, G, out)
        S.finish("sp")
        S.emit()
    return P


def make_in_maps(inputs, n_cores=8):
    consts = _host_consts()
    maps = []
    for b in range(n_cores):
        m = {}
        for name, shape in INPUT_SPECS:
            a = inputs[name]
            if name == "x":
                a = a[b]
            m[name] = np.ascontiguousarray(np.asarray(a, dtype=np.float32).reshape(shape))
        for name, shape, dt in CONST_SPECS:
            m[name] = consts[name]
        maps.append(m)
    return maps


def kernel(**inputs):
    nc = build().nc
    in_maps = make_in_maps(inputs)
    res = run_bass_kernel_spmd(nc, in_maps, core_ids=list(range(8)))
    return np.stack([np.asarray(r["out"]).reshape(SEQ, D) for r in res.results], axis=0).astype(np.float32)
```

```python
import math
import os
from contextlib import ExitStack

import numpy as np
import ml_dtypes

import concourse.bass as bass
import concourse.mybir as mybir
from concourse.bass_utils import run_bass_kernel_spmd

F32 = mybir.dt.float32
BF16 = mybir.dt.bfloat16
I32 = mybir.dt.int32
U32 = mybir.dt.uint32
ALU = mybir.AluOpType
AF = mybir.ActivationFunctionType
AX = mybir.AxisListType

D = 1024
SEQ = 4096
NMETA = 16
L = SEQ + NMETA
NT = 33
LP = NT * 128
NFFT = 2 * L
NF = L + 1
D_HY = 512
D_RG = 1024
D_IN = 3 * D_HY + 2 * D_RG + 2 * D
EPS = 1e-6
NCH = [(n * 512, min(512, L - n * 512)) for n in range(9)]


def _rows(i):
    return 128 if i < NT - 1 else L - 128 * (NT - 1)


class Buf:
    __slots__ = ("name", "w", "r")

    def __init__(self, name=""):
        self.name = name
        self.w = None
        self.r = {}


class Sched:
    ENG = ("pe", "act", "dve", "pool", "sp")
    NDMA = {"sp": 24, "pool": 16, "act": 8}

    def __init__(self, nc, stack):
        self.nc = nc
        self.items = {e: [] for e in self.ENG}
        self.sems = {}
        self.cnt = {}
        self.seen = {e: {} for e in self.ENG}
        for e in self.ENG:
            self.sems[e] = stack.enter_context(nc.semaphore("s_" + e))
            self.cnt[e] = 0
        self.dma_pool = {}
        self.dma_next = {}
        for q, n in self.NDMA.items():
            keys = []
            for i in range(n):
                k = "d_%s_%d" % (q, i)
                self.sems[k] = stack.enter_context(nc.semaphore(k))
                self.cnt[k] = 0
                keys.append(k)
            self.dma_pool[q] = keys
            self.dma_next[q] = 0
        self.n_ops = 0

    def _wait(self, eng, ev):
        if ev is None:
            return
        k, v = ev
        if self.seen[eng].get(k, 0) >= v:
            return
        self.seen[eng][k] = v
        self.items[eng].append(("w", k, v))

    def _deps(self, eng, reads, writes):
        for b in reads:
            if b.w is not None:
                if not (eng == "pe" and b.w[0] == "pe"):
                    self._wait(eng, b.w)
        for b in writes:
            if b.w is not None:
                if not (eng == "pe" and b.w[0] == "pe"):
                    self._wait(eng, b.w)
            for k, v in b.r.items():
                if eng == "pe" and k == "pe":
                    continue
                self._wait(eng, (k, v))

    def _commit(self, ev, reads, writes):
        for b in writes:
            b.w = ev
            b.r = {}
        for b in reads:
            if b in writes:
                continue
            if b.r.get(ev[0], 0) < ev[1]:
                b.r[ev[0]] = ev[1]

    def op(self, eng, fn, reads=(), writes=(), weak_reads=()):
        self._deps(eng, list(reads) + list(weak_reads), writes)
        self.cnt[eng] += 1
        ev = (eng, self.cnt[eng])
        self.items[eng].append(("o", fn, eng, 1))
        self._commit(ev, reads, writes)
        self.n_ops += 1
        return ev

    def dma(self, q, fn, reads=(), writes=(), slot_covered=False):
        pool = self.dma_pool[q]
        k = pool[self.dma_next[q] % len(pool)]
        self.dma_next[q] += 1
        if slot_covered:
            self.seen[q][k] = max(self.seen[q].get(k, 0), self.cnt[k])
        else:
            self._wait(q, (k, self.cnt[k]))
        self._deps(q, reads, writes)
        self.cnt[k] += 16
        ev = (k, self.cnt[k])
        self.items[q].append(("o", fn, k, 16))
        self._commit(ev, reads, writes)
        self.n_ops += 1
        return ev

    def barrier(self):
        for e in self.ENG:
            for k, v in self.cnt.items():
                if v > 0 and k != e:
                    self._wait(e, (k, v))

    def finish(self, eng="sp"):
        for k, v in self.cnt.items():
            if v > 0 and k != eng:
                self._wait(eng, (k, v))

    def emit(self):
        nc = self.nc
        sems = self.sems
        items = self.items

        def replay(engine, lst):
            for it in lst:
                if it[0] == "w":
                    engine.wait_ge(sems[it[1]], it[2])
                else:
                    it[1](engine).then_inc(sems[it[2]], it[3])

        with nc.Block() as block:
            @block.sync
            def _(e):
                replay(e, items["sp"])

            @block.tensor
            def _(e):
                replay(e, items["pe"])

            @block.scalar
            def _(e):
                replay(e, items["act"])

            @block.vector
            def _(e):
                replay(e, items["dve"])

            @block.gpsimd
            def _(e):
                replay(e, items["pool"])


_CONST_CACHE = {}


def _host_consts():
    if _CONST_CACHE:
        return _CONST_CACHE
    f32 = np.float32
    t = np.linspace(0.0, 1.0, L, dtype=f32)[:, None]
    w = (f32(2.0 * math.pi / L)) * np.arange(L, dtype=f32)[:, None]
    bands = np.linspace(1e-4, 15, 16, dtype=f32)[None, :]
    z = np.concatenate([t, np.cos(bands * w), -np.sin(bands * w)], axis=-1).astype(f32)
    zT = np.ascontiguousarray(z.T)
    deltas = np.abs(np.linspace(math.log(1e-2) / 0.3, math.log(1e-2) / 1.5, D_HY, dtype=f32)).astype(f32)
    negdelta = np.ascontiguousarray((-deltas).reshape(4, 128).T)
    tlin = np.ascontiguousarray(t.reshape(1, L))
    fidx = np.arange(LP)
    wfv = np.where((fidx == 0) | (fidx == L), 1.0 / NFFT, 2.0 / NFFT)
    wfv = np.where(fidx <= L, wfv, 0.0).astype(f32)
    wf = np.ascontiguousarray(wfv.reshape(NT, 128).T)
    a = np.arange(LP, dtype=np.int64)
    prod = (a[:, None] * a[None, :]) % NFFT
    ang = prod.astype(np.float64) * (2.0 * math.pi / NFFT)
    C = np.cos(ang).astype(f32)
    S = np.sin(ang).astype(f32)
    del ang, prod

    def lay(M):
        M4 = M.reshape(NT, 128, NT, 128)
        return np.ascontiguousarray(M4.transpose(2, 1, 0, 3)).reshape(NT, 128, NT * 128).astype(ml_dtypes.bfloat16)

    _CONST_CACHE.update(dict(
        c_zT=zT, c_negdelta=negdelta, c_tlin=tlin, c_wf=wf, c_tabC=lay(C), c_tabS=lay(S),
        c_identf=np.eye(128, dtype=f32), c_identb=np.eye(128, dtype=f32).astype(ml_dtypes.bfloat16),
    ))
    return _CONST_CACHE


class Prog:
    def __init__(self, dbg=None):
        self.dbg = dbg or ()
        self.nc = bass.Bass("TRN2", target_bir_lowering=False)
        self.stack = ExitStack()
        self.S = None
        self.dram = {}
        self.dbufs = {}

    def din(self, name, shape, dtype=F32):
        t = self.nc.dram_tensor(name, list(shape), dtype, kind="ExternalInput")
        self.dram[name] = t
        self.dbufs[name] = Buf(name)
        return t

    def dout(self, name, shape, dtype=F32):
        t = self.nc.dram_tensor(name, list(shape), dtype, kind="ExternalOutput")
        self.dram[name] = t
        self.dbufs[name] = Buf(name)
        return t

    def dscr(self, name, shape, dtype=F32):
        t = self.nc.dram_tensor(name, list(shape), dtype)
        self.dram[name] = t
        self.dbufs[name] = Buf(name)
        return t

    def sb(self, name, shape, dtype=F32, stack=None):
        t = (stack or self.stack).enter_context(self.nc.sbuf_tensor(name, list(shape), dtype))
        return t

    def ps(self, name, shape, dtype=F32, stack=None):
        t = (stack or self.stack).enter_context(self.nc.psum_tensor(name, list(shape), dtype))
        return t


def _round_reduce(S, arg, tmp, buf_arg, buf_tmp, n):
    MAGIC = 12582912.0
    TWO_PI = 2.0 * math.pi
    S.op("dve", lambda e: e.tensor_scalar(out=tmp, in0=arg, scalar1=1.0 / TWO_PI, scalar2=MAGIC,
                                          op0=ALU.mult, op1=ALU.add), reads=[buf_arg], writes=[buf_tmp])
    S.op("dve", lambda e: e.tensor_scalar(out=tmp, in0=tmp, scalar1=MAGIC, scalar2=-TWO_PI,
                                          op0=ALU.subtract, op1=ALU.mult), reads=[buf_tmp], writes=[buf_tmp])
    S.op("dve", lambda e: e.tensor_tensor(out=arg, in0=arg, in1=tmp, op=ALU.add),
         reads=[buf_arg, buf_tmp], writes=[buf_arg])
    S.op("dve", lambda e: e.tensor_scalar(out=arg, in0=arg, scalar1=3.1415925, scalar2=-3.1415925,
                                          op0=ALU.min, op1=ALU.max), reads=[buf_arg], writes=[buf_arg])


def phase_filter(P, inp, uv_hook=None):
    nc, S = P.nc, P.S
    kspec = P.dscr("kspec", [NT, 128, 2, 512])
    bk = P.dbufs["kspec"]
    with ExitStack() as st0, ExitStack() as st:
        kst = P.sb("fkst", [128, NT, 512], BF16, st0); b_kst = Buf()
        kdt = P.sb("fkdt", [128, NT, 512], BF16, st0); b_kdt = Buf()
        wf = P.sb("fwf", [128, NT], F32, st0); b_wf = Buf()
        w1 = P.sb("fw1", [33, 64], F32, st); b_w1 = Buf()
        w2 = P.sb("fw2", [64, 64], F32, st); b_w2 = Buf()
        w3 = P.sb("fw3", [64, 1024], F32, st); b_w3 = Buf()
        sm = P.sb("fsm", [64, 8], F32, st); b_sm = Buf()
        b3 = P.sb("fb3", [128, 8], F32, st); b_b3 = Buf()
        ndl = P.sb("fndl", [128, 4], F32, st); b_ndl = Buf()
        tl = P.sb("ftl", [128, L], F32, st); b_tl = Buf()
        identb = P.sb("fidb", [128, 128], BF16, st); b_idb = Buf()
        kf = P.sb("fkf", [128, L], F32, st); b_kf = Buf()
        kb = P.sb("fkb", [128, L], F32, st); b_kb = Buf()
        hd2 = P.sb("fhd2", [64, L], F32, st); b_hd2 = Buf()
        win = P.sb("fwin", [128, L], F32, st); b_win = Buf()
        z_sb, b_z = kf, b_kf
        hd1, b_hd1 = kb, b_kb
        tmp, b_tmp = win, b_win
        ksb = P.sb("fksb", [128, LP], BF16, st); b_ksb = Buf()
        kdb = P.sb("fkdb", [128, LP], BF16, st); b_kdb = Buf()
        pa = [P.ps("fpa%d" % i, [128, 512], F32, st0) for i in range(2)]
        b_pa = [Buf(), Buf()]
        pb = [P.ps("fpb%d" % i, [128, 512], F32, st0) for i in range(2)]
        b_pb = [Buf(), Buf()]
        pt = [P.ps("fpt%d" % i, [128, 8, 128], BF16, st0) for i in range(2)]
        b_pt = [Buf(), Buf()]

        D_ = P.dram
        ld = lambda o, i, bw, name=None: S.dma("sp", lambda e: e.dma_start(out=o, in_=i), writes=[bw])
        ld(z_sb[0:33, :], D_["c_zT"].ap(), b_z)
        ld(w1[:], inp["hy_w1"].ap()[0], b_w1)
        ld(w2[:], inp["hy_w2"].ap()[0], b_w2)
        ld(w3[:], inp["hy_w3"].ap()[0], b_w3)
        ld(sm[:, 0:1], inp["hy_b1"].ap().rearrange("o (p u) -> (o p) u", u=1), b_sm)
        ld(sm[:, 1:2], inp["hy_b2"].ap().rearrange("o (p u) -> (o p) u", u=1), b_sm)
        ld(sm[:, 2:3], inp["hy_freq"].ap()[0, 0:1, :].rearrange("o (p u) -> (o p) u", u=1), b_sm)
        ld(sm[:, 3:4], inp["hy_freq"].ap()[0, 1:2, :].rearrange("o (p u) -> (o p) u", u=1), b_sm)
        for cc in range(8):
            ld(b3[:, cc:cc + 1], inp["hy_b3"].ap()[0:1, cc * 128:(cc + 1) * 128].rearrange("o (p u) -> (o p) u", u=1), b_b3)
        ld(ndl[:], D_["c_negdelta"].ap(), b_ndl)
        ld(tl[:], D_["c_tlin"].ap().partition_broadcast(128), b_tl)
        ld(wf[:], D_["c_wf"].ap(), b_wf)
        ld(identb[:], D_["c_identb"].ap(), b_idb)

        S.op("dve", lambda e: e.tensor_tensor(out=sm[:, 4:6], in0=sm[:, 0:2], in1=sm[:, 2:4], op=ALU.mult),
             reads=[b_sm], writes=[b_sm])

        def layer(wt, b_wt, kdim, src, b_src, arg, b_arg, fcol, fbcol):
            for n, (c0, cw) in enumerate(NCH):
                p = pa[n % 2]; bp = b_pa[n % 2]
                S.op("pe", lambda e, p=p, c0=c0, cw=cw: e.matmul(p[0:64, 0:cw], lhsT=wt[0:kdim, :], rhs=src[0:kdim, c0:c0 + cw],
                                                                   start=True, stop=True),
                     reads=[b_wt, b_src], writes=[bp])
                S.op("dve", lambda e, p=p, c0=c0, cw=cw: e.tensor_scalar(out=arg[0:64, c0:c0 + cw], in0=p[0:64, 0:cw],
                                                                       scalar1=sm[:, fcol:fcol + 1], scalar2=sm[:, fbcol:fbcol + 1],
                                                                       op0=ALU.mult, op1=ALU.add),
                     reads=[bp, b_sm], writes=[b_arg])
            _round_reduce(S, arg[0:64, :], tmp[0:64, :], b_arg, b_tmp, L)
            S.op("act", lambda e: e.activation(out=arg[0:64, :], in_=arg[0:64, :], func=AF.Sin), reads=[b_arg], writes=[b_arg])

        layer(w1, b_w1, 33, z_sb, b_z, hd1, b_hd1, 2, 4)
        layer(w2, b_w2, 64, hd1, b_hd1, hd2, b_hd2, 3, 5)

        tcount = [0]
        for q in range(4):
            S.op("act", lambda e, q=q: e.activation(out=win[:], in_=tl[:], func=AF.Exp, scale=ndl[:, q:q + 1]),
                 reads=[b_tl, b_ndl], writes=[b_win])
            for (cc, dst, b_dst) in ((q, kf, b_kf), (q + 4, kb, b_kb)):
                for n, (c0, cw) in enumerate(NCH):
                    p = pa[n % 2]; bp = b_pa[n % 2]
                    S.op("pe", lambda e, p=p, c0=c0, cw=cw, cc=cc: e.matmul(p[:, 0:cw], lhsT=w3[:, cc * 128:(cc + 1) * 128],
                                                                              rhs=hd2[:, c0:c0 + cw], start=True, stop=True),
                         reads=[b_w3, b_hd2], writes=[bp])
                    S.op("dve", lambda e, p=p, c0=c0, cw=cw, cc=cc, dst=dst: e.scalar_tensor_tensor(
                        out=dst[:, c0:c0 + cw], in0=p[:, 0:cw], scalar=b3[:, cc:cc + 1], in1=win[:, c0:c0 + cw],
                        op0=ALU.add, op1=ALU.mult), reads=[bp, b_b3, b_win], writes=[b_dst])
            S.op("dve", lambda e: e.memset(kb[:, 0:1], 0.0), writes=[b_kb])
            S.op("dve", lambda e: e.tensor_tensor(out=ksb[:, 0:L], in0=kf[:], in1=kb[:], op=ALU.add),
                 reads=[b_kf, b_kb], writes=[b_ksb])
            S.op("dve", lambda e: e.tensor_tensor(out=kdb[:, 0:L], in0=kf[:], in1=kb[:], op=ALU.subtract),
                 reads=[b_kf, b_kb], writes=[b_kdb])
            for (src, b_src, dstt, b_dstt) in ((ksb, b_ksb, kst, b_kst), (kdb, b_kdb, kdt, b_kdt)):
                for g0 in range(0, NT, 8):
                    g1 = min(NT, g0 + 8)
                    k = tcount[0] % 2; tcount[0] += 1
                    for i in range(g0, g1):
                        r = _rows(i)
                        S.op("pe", lambda e, i=i, r=r, k=k, g0=g0, src=src: e.transpose(pt[k][0:r, i - g0, :], src[:, i * 128:i * 128 + r],
                                                                                        identb[:]),
                             reads=[b_src, b_idb], writes=[b_pt[k]])
                    full = [i for i in range(g0, g1) if _rows(i) == 128]
                    if full:
                        nfull = len(full)
                        S.op("act", lambda e, k=k, g0=g0, nfull=nfull, q=q, dstt=dstt: e.copy(
                            out=dstt[:, g0:g0 + nfull, q * 128:(q + 1) * 128], in_=pt[k][:, 0:nfull, :]),
                            reads=[b_pt[k]], writes=[b_dstt])
                    if g1 == NT:
                        r = _rows(NT - 1)
                        S.op("act", lambda e, k=k, g0=g0, r=r, q=q, dstt=dstt: e.copy(
                            out=dstt[0:r, NT - 1, q * 128:(q + 1) * 128], in_=pt[k][0:r, NT - 1 - g0, :]),
                            reads=[b_pt[k]], writes=[b_dstt])

        S.barrier()
        st.close()
        tabc = [P.sb("ftabc%d" % i, [128, NT * 128], BF16, st0) for i in range(2)]
        tabs = [P.sb("ftabs%d" % i, [128, NT * 128], BF16, st0) for i in range(2)]
        b_tabc = [Buf(), Buf()]; b_tabs = [Buf(), Buf()]
        osp = [P.sb("fosp%d" % i, [128, 2, 512], F32, st0) for i in range(2)]
        b_osp = [Buf(), Buf()]
        if uv_hook is not None:
            uv_hook(st0)
        for j in range(NT):
            k = j % 2
            S.dma("sp", lambda e, j=j, k=k: e.dma_start(out=tabc[k][:], in_=D_["c_tabC"].ap()[j]), writes=[b_tabc[k]])
            S.dma("sp", lambda e, j=j, k=k: e.dma_start(out=tabs[k][:], in_=D_["c_tabS"].ap()[j]), writes=[b_tabs[k]])
            for i in range(NT):
                r = _rows(i)
                S.op("pe", lambda e, i=i, r=r, k=k: e.matmul(pa[k][:], lhsT=tabc[k][0:r, i * 128:(i + 1) * 128], rhs=kst[0:r, i, :],
                                                             start=(i == 0), stop=(i == NT - 1)),
                     reads=[b_tabc[k], b_kst], writes=[b_pa[k]])
            for i in range(NT):
                r = _rows(i)
                S.op("pe", lambda e, i=i, r=r, k=k: e.matmul(pb[k][:], lhsT=tabs[k][0:r, i * 128:(i + 1) * 128], rhs=kdt[0:r, i, :],
                                                             start=(i == 0), stop=(i == NT - 1)),
                     reads=[b_tabs[k], b_kdt], writes=[b_pb[k]])
            S.op("dve", lambda e, j=j, k=k: e.tensor_scalar(out=osp[k][:, 0, :], in0=pa[k][:], scalar1=wf[:, j:j + 1], scalar2=None,
                                                            op0=ALU.mult), reads=[b_pa[k], b_wf], writes=[b_osp[k]])
            S.op("dve", lambda e, j=j, k=k: e.tensor_scalar(out=osp[k][:, 1, :], in0=pb[k][:], scalar1=wf[:, j:j + 1], scalar2=None,
                                                            op0=ALU.mult), reads=[b_pb[k], b_wf], writes=[b_osp[k]])
            S.dma("sp", lambda e, j=j, k=k: e.dma_start(out=kspec.ap()[j], in_=osp[k][:]), reads=[b_osp[k]], writes=[bk])
        S.barrier()
    return kspec


from concourse.bass import IndirectOffsetOnAxis

def c_hcw(k, cc): return k * 12 + cc
def c_hcb(cc): return 36 + cc
def c_skip(i): return 48 + i
def c_rcw(d, j, h): return 52 + (d * 4 + j) * 8 + h
def c_gmix(k): return 116 + k
def c_rcb(d, h): return 128 + d * 8 + h
def c_rgb(d, g, h): return 144 + (d * 2 + g) * 8 + h
def c_lam(d, h): return 176 + d * 8 + h
def c_bg(m): return 192 + m
NPC = 208


def phase_params(P, inp, G):
    nc, S = P.nc, P.S
    st = P.stack
    PC = P.sb("PC", [128, NPC], F32, st); b_PC = Buf()
    CL = P.sb("CL", [128, 32], F32, st); b_CL = Buf()
    identf = P.sb("identf", [128, 128], F32, st); b_idf = Buf()
    identb = P.sb("identb", [128, 128], BF16, st); b_idb = Buf()
    G.update(PC=PC, b_PC=b_PC, CL=CL, b_CL=b_CL, identf=identf, b_idf=b_idf, identb=identb, b_idb=b_idb)
    S.dma("sp", lambda e: e.dma_start(out=identf[:], in_=P.dram["c_identf"].ap()), writes=[b_idf])
    S.dma("sp", lambda e: e.dma_start(out=identb[:], in_=P.dram["c_identb"].ap()), writes=[b_idb])
    with ExitStack() as s2:
        PR = [P.sb("PR0", [128, 128], F32, s2), P.sb("PR1", [128, 128], F32, s2)]
        b_PR = [Buf(), Buf()]
        pp = P.ps("pprm", [128, 128], F32, s2); b_pp = Buf()
        tmp = P.sb("prm_t", [128, 16 * 6], F32, s2); b_tmp = Buf()
        S.op("dve", lambda e: e.memset(PR[0][:], 0.0), writes=[b_PR[0]])
        S.op("dve", lambda e: e.memset(PR[1][:], 0.0), writes=[b_PR[1]])

        def ldrows(t, r0, src, n):
            S.dma("sp", lambda e: e.dma_start(out=PR[t][r0:r0 + n, :], in_=src), writes=[b_PR[t]])
        ldrows(0, 0, inp["hy_conv_w"].ap().rearrange("o k (c p) -> (o k c) p", p=128), 36)
        ldrows(0, 36, inp["hy_conv_b"].ap().rearrange("o (c p) -> (o c) p", p=128), 12)
        ldrows(0, 48, inp["hy_skip"].ap().rearrange("o (c p) -> (o c) p", p=128), 4)
        ldrows(0, 52, inp["rg_conv_w"].ap().rearrange("o d j (c p) -> (o d j c) p", p=128), 64)
        ldrows(0, 116, inp["g_mix"].ap().rearrange("o (c p) -> (o c) p", p=128), 8)
        ldrows(1, 0, inp["rg_conv_b"].ap().rearrange("o d (c p) -> (o d c) p", p=128), 16)
        ldrows(1, 16, inp["rg_gate_b"].ap().rearrange("o d g (c p) -> (o d g c) p", p=128), 32)
        ldrows(1, 48, inp["rg_lambda"].ap().rearrange("o d (c p) -> (o d c) p", p=128), 16)
        ldrows(1, 64, inp["b_gate"].ap().rearrange("o (c p) -> (o c) p", p=128), 16)
        for t, ncol in ((0, 128), (1, 80)):
            S.op("pe", lambda e, t=t, ncol=ncol: e.transpose(pp[:, 0:ncol], PR[t][0:ncol, :], identf[0:ncol, 0:ncol]),
                 reads=[b_PR[t], b_idf], writes=[b_pp])
            S.op("dve", lambda e, t=t, ncol=ncol: e.tensor_copy(out=PC[:, t * 128:t * 128 + ncol], in_=pp[:, 0:ncol]),
                 reads=[b_pp], writes=[b_PC])
        lam = PC[:, 176:192]
        al, xx, ss, s2_, acc, pw = [tmp[:, i * 16:(i + 1) * 16] for i in range(6)]
        R = [b_PC, b_tmp]; W = [b_tmp]
        S.op("dve", lambda e: e.tensor_scalar(out=al, in0=lam, scalar1=-1.0, scalar2=None, op0=ALU.mult), reads=R, writes=W)
        S.op("dve", lambda e: e.tensor_tensor(out=al, in0=al, in1=lam, op=ALU.max), reads=R, writes=W)
        S.op("act", lambda e: e.activation(out=xx, in_=al, func=AF.Exp, scale=-1.0), reads=R, writes=W)
        S.op("dve", lambda e: e.tensor_scalar(out=ss, in0=xx, scalar1=2.0, scalar2=None, op0=ALU.add), reads=R, writes=W)
        S.op("dve", lambda e: e.reciprocal(out=ss, in_=ss), reads=R, writes=W)
        S.op("dve", lambda e: e.tensor_tensor(out=ss, in0=ss, in1=xx, op=ALU.mult), reads=R, writes=W)
        S.op("dve", lambda e: e.tensor_tensor(out=s2_, in0=ss, in1=ss, op=ALU.mult), reads=R, writes=W)
        S.op("dve", lambda e: e.tensor_copy(out=acc, in_=ss), reads=R, writes=W)
        S.op("dve", lambda e: e.tensor_copy(out=pw, in_=ss), reads=R, writes=W)
        for kk in (3, 5, 7, 9, 11, 13):
            S.op("dve", lambda e: e.tensor_tensor(out=pw, in0=pw, in1=s2_, op=ALU.mult), reads=R, writes=W)
            S.op("dve", lambda e, kk=kk: e.scalar_tensor_tensor(out=acc, in0=pw, scalar=1.0 / kk, in1=acc, op0=ALU.mult, op1=ALU.add),
                 reads=R, writes=W)
        S.op("dve", lambda e: e.tensor_scalar(out=al, in0=lam, scalar1=-1.0, scalar2=0.0, op0=ALU.mult, op1=ALU.max), reads=R, writes=W)
        S.op("dve", lambda e: e.scalar_tensor_tensor(out=acc, in0=acc, scalar=2.0, in1=al, op0=ALU.mult, op1=ALU.add), reads=R, writes=W)
        S.op("dve", lambda e: e.tensor_scalar(out=CL[:, 0:16], in0=acc, scalar1=-8.0, scalar2=None, op0=ALU.mult), reads=R, writes=[b_CL])
        S.op("dve", lambda e: e.tensor_scalar(out=CL[:, 16:32], in0=acc, scalar1=-16.0, scalar2=None, op0=ALU.mult), reads=R, writes=[b_CL])
        S.barrier()


def phase_uvtab(P, inp, st):
    nc, S = P.nc, P.S
    uvtab = P.dscr("uvtab", [16384, 2048], BF16)
    utab = inp["peer_u"].ap()[0]; vtab = inp["peer_v"].ap()[0]
    us = [P.sb("t_us%d" % i, [128, D], F32, st) for i in range(2)]; b_us = [Buf(), Buf()]
    vs = [P.sb("t_vs%d" % i, [128, D], F32, st) for i in range(2)]; b_vs = [Buf(), Buf()]
    ob = [P.sb("t_ob%d" % i, [128, 2 * D], BF16, st) for i in range(2)]; b_ob = [Buf(), Buf()]
    for c in range(128):
        k = c % 2
        S.dma("pool", lambda e, c=c, k=k: e.dma_start(out=us[k][:], in_=utab[128 * c:128 * (c + 1), :]), writes=[b_us[k]])
        S.dma("pool", lambda e, c=c, k=k: e.dma_start(out=vs[k][:], in_=vtab[128 * c:128 * (c + 1), :]), writes=[b_vs[k]])
        if c >= 1:
            kp = (c - 1) % 2
            S.op("act", lambda e, kp=kp: e.copy(out=ob[kp][:, 0:D], in_=us[kp][:]), reads=[b_us[kp]], writes=[b_ob[kp]])
            S.op("act", lambda e, kp=kp: e.copy(out=ob[kp][:, D:2 * D], in_=vs[kp][:]), reads=[b_vs[kp]], writes=[b_ob[kp]])
            S.dma("pool", lambda e, c=c, kp=kp: e.dma_start(out=uvtab.ap()[128 * (c - 1):128 * c, :], in_=ob[kp][:]), reads=[b_ob[kp]],
                  writes=[P.dbufs["uvtab"]])
    kp = 127 % 2
    S.op("act", lambda e: e.copy(out=ob[kp][:, 0:D], in_=us[kp][:]), reads=[b_us[kp]], writes=[b_ob[kp]])
    S.op("act", lambda e: e.copy(out=ob[kp][:, D:2 * D], in_=vs[kp][:]), reads=[b_vs[kp]], writes=[b_ob[kp]])
    S.dma("pool", lambda e: e.dma_start(out=uvtab.ap()[128 * 127:128 * 128, :], in_=ob[kp][:]), reads=[b_ob[kp]], writes=[P.dbufs["uvtab"]])


def phase_norm(P, inp, G):
    nc, S = P.nc, P.S
    nT = G["nT"]; b_nT = G["b_nT"]
    identb, b_idb = G["identb"], G["b_idb"]
    x = inp["x"].ap(); meta = inp["meta"].ap()
    with ExitStack() as st:
        ht = [P.sb("n_h%d" % i, [128, D], F32, st) for i in range(2)]; b_ht = [Buf(), Buf()]
        sq = P.sb("n_sq", [128, D], F32, st); b_sq = Buf()
        nb = [P.sb("n_nb%d" % i, [128, D], BF16, st) for i in range(2)]; b_nb = [Buf(), Buf()]
        ssq = P.sb("n_ss", [128, 2 * NT], F32, st); b_ss = Buf()
        pt = [P.ps("n_pt%d" % i, [128, 8, 128], BF16, st) for i in range(2)]; b_pt = [Buf(), Buf()]
        for j in range(NT):
            k = j % 2
            r = _rows(j)
            if j == 0:
                S.dma("sp", lambda e, k=k: e.dma_start(out=ht[k][0:NMETA, :], in_=meta), writes=[b_ht[k]])
                S.dma("sp", lambda e, k=k: e.dma_start(out=ht[k][NMETA:128, :], in_=x[0:128 - NMETA, :]), writes=[b_ht[k]])
            else:
                S.dma("sp", lambda e, k=k, j=j, r=r: e.dma_start(out=ht[k][0:r, :], in_=x[128 * j - NMETA:128 * j - NMETA + r, :]),
                      writes=[b_ht[k]])
            S.op("act", lambda e, k=k, j=j, r=r: e.activation(out=sq[0:r, :], in_=ht[k][0:r, :], func=AF.Square,
                                                              accum_out=ssq[0:r, 2 * j:2 * j + 1]),
                 reads=[b_ht[k]], writes=[b_sq, b_ss])
            S.op("act", lambda e, j=j, r=r: e.activation(out=ssq[0:r, 2 * j + 1:2 * j + 2], in_=ssq[0:r, 2 * j:2 * j + 1], func=AF.Sqrt,
                                                         scale=1.0 / D, bias=EPS), reads=[b_ss], writes=[b_ss])
            S.op("dve", lambda e, j=j, r=r: e.reciprocal(out=ssq[0:r, 2 * j + 1:2 * j + 2], in_=ssq[0:r, 2 * j + 1:2 * j + 2]),
                 reads=[b_ss], writes=[b_ss])
            S.op("dve", lambda e, k=k, j=j, r=r: e.tensor_scalar(out=nb[k][0:r, :], in0=ht[k][0:r, :], scalar1=ssq[0:r, 2 * j + 1:2 * j + 2],
                                                                  scalar2=None, op0=ALU.mult), reads=[b_ht[k], b_ss], writes=[b_nb[k]])
            for c in range(8):
                S.op("pe", lambda e, k=k, c=c, r=r: e.transpose(pt[k][:, c, 0:r], nb[k][0:r, c * 128:(c + 1) * 128], identb[0:r, 0:r]),
                     reads=[b_nb[k], b_idb], writes=[b_pt[k]])
            S.op("act", lambda e, k=k, j=j, r=r: e.copy(out=nT[:, :, 128 * j:128 * j + r], in_=pt[k][:, :, 0:r]),
                 reads=[b_pt[k]], writes=[b_nT])
        S.barrier()


def phase_mixer_in(P, inp, G):
    nc, S = P.nc, P.S
    nT, b_nT, PC, b_PC, CL, b_CL = G["nT"], G["b_nT"], G["PC"], G["b_PC"], G["CL"], G["b_CL"]
    identb, b_idb = G["identb"], G["b_idb"]
    w_in = inp["w_in"].ap()[0].rearrange("(k p) c -> p k c", p=128)
    yrgT = P.dscr("yrgT", [8, 128, L], BF16)
    sgT = P.dscr("sgT", [16, 128, L], BF16)
    x0cT = P.dscr("x0cT", [4, 128, L], F32)
    zT = P.dscr("zT", [4, 128, L], F32)
    with ExitStack() as st0:
        wst = [P.sb("m_wst%d" % i, [128, 8, 128], F32, st0) for i in range(2)]; b_wst = [Buf(), Buf()]
        wbf = [P.sb("m_wbf%d" % i, [128, 8, 128], BF16, st0) for i in range(2)]; b_wbf = [Buf(), Buf()]
        pp = [P.ps("m_pp%d" % i, [128, 512], F32, st0) for i in range(2)]; b_pp = [Buf(), Buf()]
        pg = [P.ps("m_pg%d" % i, [128, 512], F32, st0) for i in range(2)]; b_pg = [Buf(), Buf()]
        pt = [P.ps("m_pt%d" % i, [128, 8, 128], BF16, st0) for i in range(2)]; b_pt = [Buf(), Buf()]
        cnt = {"w": 0, "p": 0, "g": 0, "t": 0}
        gm_b = PC[:, 116:124].unsqueeze(2).to_broadcast([128, 8, 128])

        def project(cc, evac):
            k = cnt["w"] % 2; cnt["w"] += 1
            S.dma("sp", lambda e: e.dma_start(out=wst[k][:], in_=w_in[:, :, cc * 128:(cc + 1) * 128]), writes=[b_wst[k]])
            S.op("pool", lambda e: e.tensor_tensor(out=wbf[k][:], in0=wst[k][:], in1=gm_b, op=ALU.mult),
                 reads=[b_wst[k], b_PC], writes=[b_wbf[k]])
            for n, (c0, cw) in enumerate(NCH):
                q = cnt["p"] % 2; cnt["p"] += 1
                for kk in range(8):
                    S.op("pe", lambda e, kk=kk, q=q, c0=c0, cw=cw: e.matmul(pp[q][:, 0:cw], lhsT=wbf[k][:, kk, :], rhs=nT[:, kk, c0:c0 + cw],
                                                                             start=(kk == 0), stop=(kk == 7)),
                         reads=[b_wbf[k], b_nT], writes=[b_pp[q]])
                evac(pp[q], b_pp[q], c0, cw)

        with ExitStack() as st:
            xr = P.sb("r_xr", [128, L + 6], F32, st); b_xr = Buf()
            ggs = [P.sb("r_gg%d" % i, [128, L], BF16, st) for i in range(2)]; b_ggs = [Buf(), Buf()]
            xcb = P.sb("r_xcb", [128, L], BF16, st); b_xcb = Buf()
            rr = P.sb("r_rr", [128, L], F32, st); b_rr = Buf()
            ii = P.sb("r_ii", [128, L], F32, st); b_ii = Buf()
            aa = P.sb("r_aa", [128, L], F32, st); b_aa = Buf()
            hacc = P.sb("r_ha", [128, L], F32, st); b_ha = Buf()
            yb = P.sb("r_yb", [128, L], BF16, st); b_yb = Buf()
            gwss = [P.sb("r_gws%d" % i, [128, 4, 128], F32, st) for i in range(2)]; b_gwss = [Buf(), Buf()]
            gwbs = [P.sb("r_gwb%d" % i, [128, 4, 128], BF16, st) for i in range(2)]; b_gwbs = [Buf(), Buf()]
            S.op("dve", lambda e: e.memset(xr[:, 0:3], 0.0), writes=[b_xr])
            S.op("dve", lambda e: e.memset(xr[:, L + 3:L + 6], 0.0), writes=[b_xr])
            gw = inp["rg_gate_w"].ap()[0]
            def prefetch(h):
                gg, b_gg = ggs[h % 2], b_ggs[h % 2]
                gws, b_gws, gwb, b_gwb = gwss[h % 2], b_gwss[h % 2], gwbs[h % 2], b_gwbs[h % 2]
                project(12 + h, lambda p, bp, c0, cw: S.op("act", lambda e: e.copy(out=xr[:, 3 + c0:3 + c0 + cw], in_=p[:, 0:cw]),
                                                          reads=[bp], writes=[b_xr]))
                project(20 + h, lambda p, bp, c0, cw: S.op("act", lambda e: e.activation(out=gg[:, c0:c0 + cw], in_=p[:, 0:cw],
                                                                                        func=AF.Gelu_apprx_tanh),
                                                          reads=[bp], writes=[b_gg]))
                S.dma("sp", lambda e: e.dma_start(out=gws[:], in_=gw[:, :, h].rearrange("d g i j -> i (d g) j")), writes=[b_gws])
                S.op("pool", lambda e: e.tensor_copy(out=gwb[:], in_=gws[:]), reads=[b_gws], writes=[b_gwb])

            prefetch(0)
            for h in range(8):
                gg, b_gg = ggs[h % 2], b_ggs[h % 2]
                gwb, b_gwb = gwbs[h % 2], b_gwbs[h % 2]
                for d in range(2):
                    def xs(j, d=d):
                        off = 3 - j if d == 0 else 3 + j
                        return xr[:, off:off + L]
                    S.op("dve", lambda e, d=d, h=h, xv=xs(0): e.tensor_scalar(out=ii[:], in0=xv, scalar1=PC[:, c_rcw(d, 0, h):c_rcw(d, 0, h) + 1],
                                                                   scalar2=PC[:, c_rcb(d, h):c_rcb(d, h) + 1], op0=ALU.mult, op1=ALU.add),
                         reads=[b_xr, b_PC], writes=[b_ii])
                    for j in (1, 2):
                        S.op("dve", lambda e, d=d, h=h, j=j, xv=xs(j): e.scalar_tensor_tensor(out=ii[:], in0=xv, scalar=PC[:, c_rcw(d, j, h):c_rcw(d, j, h) + 1],
                                                                                   in1=ii[:], op0=ALU.mult, op1=ALU.add),
                             reads=[b_xr, b_PC, b_ii], writes=[b_ii])
                    S.op("dve", lambda e, d=d, h=h, xv=xs(3): e.scalar_tensor_tensor(out=xcb[:], in0=xv, scalar=PC[:, c_rcw(d, 3, h):c_rcw(d, 3, h) + 1],
                                                                          in1=ii[:], op0=ALU.mult, op1=ALU.add),
                         reads=[b_xr, b_PC, b_ii], writes=[b_xcb])
                    for g, (dst, b_dst) in enumerate(((rr, b_rr), (ii, b_ii))):
                        for n, (c0, cw) in enumerate(NCH):
                            q = cnt["g"] % 2; cnt["g"] += 1
                            S.op("pe", lambda e, q=q, c0=c0, cw=cw, d=d, g=g, gwb=gwb: e.matmul(pg[q][:, 0:cw], lhsT=gwb[:, d * 2 + g, :],
                                                                                      rhs=xcb[:, c0:c0 + cw], start=True, stop=True),
                                 reads=[b_gwb, b_xcb], writes=[b_pg[q]])
                            S.op("act", lambda e, q=q, c0=c0, cw=cw, d=d, g=g, h=h, dst=dst: e.activation(
                                out=dst[:, c0:c0 + cw], in_=pg[q][:, 0:cw], func=AF.Sigmoid,
                                bias=PC[:, c_rgb(d, g, h):c_rgb(d, g, h) + 1]), reads=[b_pg[q], b_PC], writes=[b_dst])
                    col = d * 8 + h
                    S.op("act", lambda e, col=col: e.activation(out=aa[:], in_=rr[:], func=AF.Exp, scale=CL[:, col:col + 1]),
                         reads=[b_rr, b_CL], writes=[b_aa])
                    S.op("act", lambda e, col=col: e.activation(out=rr[:], in_=rr[:], func=AF.Exp, scale=CL[:, 16 + col:17 + col]),
                         reads=[b_rr, b_CL], writes=[b_rr])
                    S.op("dve", lambda e: e.tensor_scalar(out=rr[:], in0=rr[:], scalar1=1.0, scalar2=None, op0=ALU.min),
                         reads=[b_rr], writes=[b_rr])
                    S.op("act", lambda e: e.activation(out=rr[:], in_=rr[:], func=AF.Sqrt, scale=-1.0, bias=1.0), reads=[b_rr], writes=[b_rr])
                    if d == 1 and h + 1 < 8:
                        prefetch(h + 1)
                    first = 0 if d == 0 else L - 1
                    S.op("dve", lambda e, first=first: e.memset(rr[:, first:first + 1], 1.0), writes=[b_rr])
                    S.op("dve", lambda e: e.tensor_tensor(out=ii[:], in0=ii[:], in1=rr[:], op=ALU.mult), reads=[b_ii, b_rr], writes=[b_ii])
                    S.op("dve", lambda e: e.tensor_tensor(out=ii[:], in0=ii[:], in1=xcb[:], op=ALU.mult), reads=[b_ii, b_xcb], writes=[b_ii])
                    if d == 0:
                        S.op("dve", lambda e: e.tensor_tensor_scan(out=hacc[:], data0=aa[:], data1=ii[:], initial=0.0,
                                                                   op0=ALU.mult, op1=ALU.add), reads=[b_aa, b_ii], writes=[b_ha])
                    else:
                        S.op("dve", lambda e: e.tensor_tensor_scan(out=rr[:, ::-1], data0=aa[:, ::-1], data1=ii[:, ::-1], initial=0.0,
                                                                   op0=ALU.mult, op1=ALU.add), reads=[b_aa, b_ii], writes=[b_rr])
                S.op("dve", lambda e: e.tensor_tensor(out=hacc[:], in0=hacc[:], in1=rr[:], op=ALU.add), reads=[b_ha, b_rr], writes=[b_ha])
                S.op("dve", lambda e, gg=gg: e.tensor_tensor(out=yb[:], in0=hacc[:], in1=gg[:], op=ALU.mult), reads=[b_ha, b_gg], writes=[b_yb])
                S.dma("sp", lambda e, h=h: e.dma_start(out=yrgT.ap()[h], in_=yb[:]), reads=[b_yb], writes=[P.dbufs["yrgT"]])
            S.barrier()

        with ExitStack() as st:
            sg = [P.sb("g_sg%d" % i, [128, L], BF16, st) for i in range(2)]; b_sg = [Buf(), Buf()]
            for m in range(16):
                k = m % 2
                project(28 + m, lambda p, bp, c0, cw, m=m, k=k: S.op("act", lambda e: e.activation(
                    out=sg[k][:, c0:c0 + cw], in_=p[:, 0:cw], func=AF.Sigmoid, bias=PC[:, c_bg(m):c_bg(m) + 1]),
                    reads=[bp, b_PC], writes=[b_sg[k]]))
                S.dma("sp", lambda e, m=m, k=k: e.dma_start(out=sgT.ap()[m], in_=sg[k][:]), reads=[b_sg[k]], writes=[P.dbufs["sgT"]])
            S.barrier()

        ztd = P.dscr("ztd", [128, NT, 512], BF16)
        with ExitStack() as st:
            zt = P.sb("h_zt", [128, NT, 512], BF16, st); b_zt = Buf()
            S.op("pool", lambda e: e.memset(zt[:, NT - 1, :], 0.0), writes=[b_zt])
            xas = [P.sb("h_xa%d" % i, [128, L + 2], F32, st) for i in range(2)]; b_xas = [Buf(), Buf()]
            xcnt = [0]
            ua = P.sb("h_ua", [128, L], F32, st); b_ua = Buf()
            ub = P.sb("h_ub", [128, L], F32, st); b_ub = Buf()
            zb = P.sb("h_zb", [128, LP], BF16, st); b_zb = Buf()
            for i_ in range(2):
                S.op("dve", lambda e, i_=i_: e.memset(xas[i_][:, 0:1], 0.0), writes=[b_xas[i_]])
                S.op("dve", lambda e, i_=i_: e.memset(xas[i_][:, L + 1:L + 2], 0.0), writes=[b_xas[i_]])

            def conv3(cc, dst, b_dst):
                xa, b_xa = xas[xcnt[0] % 2], b_xas[xcnt[0] % 2]
                xcnt[0] += 1
                project(cc, lambda p, bp, c0, cw: S.op("act", lambda e: e.copy(out=xa[:, 1 + c0:1 + c0 + cw], in_=p[:, 0:cw]),
                                                      reads=[bp], writes=[b_xa]))
                S.op("dve", lambda e: e.tensor_scalar(out=dst[:], in0=xa[:, 1:L + 1], scalar1=PC[:, c_hcw(1, cc):c_hcw(1, cc) + 1],
                                                      scalar2=PC[:, c_hcb(cc):c_hcb(cc) + 1], op0=ALU.mult, op1=ALU.add),
                     reads=[b_xa, b_PC], writes=[b_dst])
                S.op("dve", lambda e: e.scalar_tensor_tensor(out=dst[:], in0=xa[:, 0:L], scalar=PC[:, c_hcw(0, cc):c_hcw(0, cc) + 1],
                                                             in1=dst[:], op0=ALU.mult, op1=ALU.add), reads=[b_xa, b_PC, b_dst], writes=[b_dst])
                S.op("dve", lambda e: e.scalar_tensor_tensor(out=dst[:], in0=xa[:, 2:L + 2], scalar=PC[:, c_hcw(2, cc):c_hcw(2, cc) + 1],
                                                             in1=dst[:], op0=ALU.mult, op1=ALU.add), reads=[b_xa, b_PC, b_dst], writes=[b_dst])

            for i in range(4):
                conv3(i, ua, b_ua)
                S.dma("sp", lambda e, i=i: e.dma_start(out=x0cT.ap()[i], in_=ua[:]), reads=[b_ua], writes=[P.dbufs["x0cT"]])
            for i in range(4):
                conv3(4 + i, ua, b_ua)
                conv3(8 + i, ub, b_ub)
                S.op("dve", lambda e: e.tensor_tensor(out=ua[:], in0=ua[:], in1=ub[:], op=ALU.mult), reads=[b_ua, b_ub], writes=[b_ua])
                S.dma("sp", lambda e, i=i: e.dma_start(out=zT.ap()[i], in_=ua[:]), reads=[b_ua], writes=[P.dbufs["zT"]])
                S.op("act", lambda e: e.copy(out=zb[:, 0:L], in_=ua[:]), reads=[b_ua], writes=[b_zb])
                for g0 in range(0, NT, 8):
                    g1 = min(NT, g0 + 8)
                    k = cnt["t"] % 2; cnt["t"] += 1
                    for t in range(g0, g1):
                        r = _rows(t)
                        S.op("pe", lambda e, t=t, r=r, k=k, g0=g0: e.transpose(pt[k][0:r, t - g0, :], zb[:, t * 128:t * 128 + r], identb[:]),
                             reads=[b_zb, b_idb], writes=[b_pt[k]])
                    nfull = len([t for t in range(g0, g1) if _rows(t) == 128])
                    if nfull:
                        S.op("act", lambda e, k=k, g0=g0, nfull=nfull, i=i: e.copy(out=zt[:, g0:g0 + nfull, i * 128:(i + 1) * 128],
                                                                                 in_=pt[k][:, 0:nfull, :]), reads=[b_pt[k]], writes=[b_zt])
                    if g1 == NT:
                        r = _rows(NT - 1)
                        S.op("act", lambda e, k=k, g0=g0, r=r, i=i: e.copy(out=zt[0:r, NT - 1, i * 128:(i + 1) * 128],
                                                                         in_=pt[k][0:r, NT - 1 - g0, :]), reads=[b_pt[k]], writes=[b_zt])
            S.dma("sp", lambda e: e.dma_start(out=ztd.ap(), in_=zt[:]), reads=[b_zt], writes=[P.dbufs["ztd"]])
            S.barrier()


def phase_dft(P, inp, G):
    nc, S = P.nc, P.S
    D_ = P.dram
    PC, b_PC = G["PC"], G["b_PC"]
    identf, b_idf = G["identf"], G["b_idf"]
    yyT, b_yyT = G["yyT"], G["b_yyT"]
    kspec = D_["kspec"]; bk = P.dbufs["kspec"]
    with ExitStack() as st:
        zt = P.sb("d_zt", [128, NT, 512], BF16, st); b_zt = Buf()
        S.dma("sp", lambda e: e.dma_start(out=zt[:], in_=D_["ztd"].ap()), reads=[P.dbufs["ztd"]], writes=[b_zt])
        Yre = P.sb("d_yre", [128, NT, 512], BF16, st); b_yre = Buf()
        Yim = P.sb("d_yim", [128, NT, 512], BF16, st); b_yim = Buf()
        tabc = [P.sb("d_tabc%d" % i, [128, NT * 128], BF16, st) for i in range(2)]
        tabs = [P.sb("d_tabs%d" % i, [128, NT * 128], BF16, st) for i in range(2)]
        b_tabc = [Buf(), Buf()]; b_tabs = [Buf(), Buf()]
        ksp = [P.sb("d_ksp%d" % i, [128, 2, 512], F32, st) for i in range(2)]; b_ksp = [Buf(), Buf()]
        t1 = P.sb("d_t1", [128, 512], F32, st); b_t1 = Buf()
        t2 = P.sb("d_t2", [128, 512], F32, st); b_t2 = Buf()
        pa = [P.ps("d_pa%d" % i, [128, 512], F32, st) for i in range(2)]; b_pa = [Buf(), Buf()]
        pb = [P.ps("d_pb%d" % i, [128, 512], F32, st) for i in range(2)]; b_pb = [Buf(), Buf()]
        ptf = [P.ps("d_ptf%d" % i, [128, 4, 128], F32, st) for i in range(2)]; b_ptf = [Buf(), Buf()]
        ysb = [P.sb("d_ysb%d" % i, [128, 512], F32, st) for i in range(2)]; b_ysb = [Buf(), Buf()]
        zx = [P.sb("d_zx%d" % i, [128, 2, 4, 128], F32, st) for i in range(2)]; b_zx = [Buf(), Buf()]

        def load_tabs(j, k):
            S.dma("sp", lambda e: e.dma_start(out=tabc[k][:], in_=D_["c_tabC"].ap()[j]), writes=[b_tabc[k]])
            S.dma("sp", lambda e: e.dma_start(out=tabs[k][:], in_=D_["c_tabS"].ap()[j]), writes=[b_tabs[k]])

        for j in range(NT):
            k = j % 2
            load_tabs(j, k)
            S.dma("sp", lambda e, j=j, k=k: e.dma_start(out=ksp[k][:], in_=kspec.ap()[j]), reads=[bk], writes=[b_ksp[k]])
            for (tab, b_tab, ps_, b_ps) in ((tabc, b_tabc, pa, b_pa), (tabs, b_tabs, pb, b_pb)):
                for i in range(NT):
                    r = _rows(i)
                    S.op("pe", lambda e, i=i, r=r, k=k, tab=tab, ps_=ps_: e.matmul(ps_[k][:], lhsT=tab[k][0:r, i * 128:(i + 1) * 128],
                                                                                  rhs=zt[0:r, i, :], start=(i == 0), stop=(i == NT - 1)),
                         reads=[b_tab[k], b_zt], writes=[b_ps[k]])
            S.op("dve", lambda e, k=k: e.tensor_tensor(out=t1[:], in0=pa[k][:], in1=ksp[k][:, 0, :], op=ALU.mult), reads=[b_pa[k], b_ksp[k]], writes=[b_t1])
            S.op("dve", lambda e, k=k: e.tensor_tensor(out=t2[:], in0=pb[k][:], in1=ksp[k][:, 1, :], op=ALU.mult), reads=[b_pb[k], b_ksp[k]], writes=[b_t2])
            S.op("dve", lambda e, j=j: e.tensor_tensor(out=Yre[:, j, :], in0=t1[:], in1=t2[:], op=ALU.subtract), reads=[b_t1, b_t2], writes=[b_yre])
            S.op("dve", lambda e, k=k: e.tensor_tensor(out=t1[:], in0=pb[k][:], in1=ksp[k][:, 0, :], op=ALU.mult), reads=[b_pb[k], b_ksp[k]], writes=[b_t1])
            S.op("dve", lambda e, k=k: e.tensor_tensor(out=t2[:], in0=pa[k][:], in1=ksp[k][:, 1, :], op=ALU.mult), reads=[b_pa[k], b_ksp[k]], writes=[b_t2])
            S.op("dve", lambda e, j=j: e.tensor_tensor(out=Yim[:, j, :], in0=t1[:], in1=t2[:], op=ALU.add), reads=[b_t1, b_t2], writes=[b_yim])

        rf = NF - 128 * (NT - 1)
        skip_b = PC[:, 48:52].unsqueeze(2).to_broadcast([128, 4, 128])
        zTd = D_["zT"].ap(); x0d = D_["x0cT"].ap()
        for j in range(NT):
            k = j % 2
            r = _rows(j)
            load_tabs(j, k)
            S.dma("sp", lambda e, j=j, k=k, r=r: e.dma_start(out=zx[k][:, 0, :, 0:r], in_=zTd[:, :, 128 * j:128 * j + r].rearrange("i p t -> p i t")),
                  reads=[P.dbufs["zT"]], writes=[b_zx[k]])
            S.dma("sp", lambda e, j=j, k=k, r=r: e.dma_start(out=zx[k][:, 1, :, 0:r], in_=x0d[:, :, 128 * j:128 * j + r].rearrange("i p t -> p i t")),
                  reads=[P.dbufs["x0cT"]], writes=[b_zx[k]])
            n_mm = 2 * NT
            c = 0
            for (tab, b_tab, Y, b_Y) in ((tabc, b_tabc, Yre, b_yre), (tabs, b_tabs, Yim, b_yim)):
                for i in range(NT):
                    rk = 128 if i < NT - 1 else rf
                    S.op("pe", lambda e, i=i, rk=rk, k=k, tab=tab, Y=Y, c=c: e.matmul(pa[k][:], lhsT=tab[k][0:rk, i * 128:(i + 1) * 128],
                                                                                     rhs=Y[0:rk, i, :], start=(c == 0), stop=(c == n_mm - 1)),
                         reads=[b_tab[k], b_Y], writes=[b_pa[k]])
                    c += 1
            S.op("act", lambda e, k=k: e.copy(out=ysb[k][:], in_=pa[k][:]), reads=[b_pa[k]], writes=[b_ysb[k]])
            for i in range(4):
                S.op("pe", lambda e, i=i, k=k, r=r: e.transpose(ptf[k][:, i, 0:r], ysb[k][0:r, i * 128:(i + 1) * 128], identf[0:r, 0:r]),
                     reads=[b_ysb[k], b_idf], writes=[b_ptf[k]])
            S.op("dve", lambda e, k=k, r=r: e.tensor_tensor(out=zx[k][:, 0, :, 0:r], in0=zx[k][:, 0, :, 0:r], in1=skip_b[:, :, 0:r], op=ALU.mult),
                 reads=[b_zx[k], b_PC], writes=[b_zx[k]])
            S.op("dve", lambda e, k=k, r=r: e.tensor_tensor(out=zx[k][:, 0, :, 0:r], in0=zx[k][:, 0, :, 0:r], in1=ptf[k][:, :, 0:r], op=ALU.add),
                 reads=[b_zx[k], b_ptf[k]], writes=[b_zx[k]])
            S.op("dve", lambda e, k=k, r=r, j=j: e.tensor_tensor(out=yyT[:, :, 128 * j:128 * j + r], in0=zx[k][:, 0, :, 0:r], in1=zx[k][:, 1, :, 0:r],
                                                                op=ALU.mult), reads=[b_zx[k]], writes=[b_yyT])
        S.barrier()


def phase_merge(P, inp, G):
    nc, S = P.nc, P.S
    D_ = P.dram
    yyT, b_yyT = G["yyT"], G["b_yyT"]
    mgT = P.dscr("mgT", [8, 128, L], BF16)
    hy_out = inp["hy_out"].ap()[0].rearrange("(k p) c -> p k c", p=128)
    rg_out = inp["rg_out"].ap()[0].rearrange("(k p) c -> p k c", p=128)
    with ExitStack() as st:
        yrg = P.sb("g_yrg", [128, 8, L], BF16, st); b_yrg = Buf()
        hws = P.sb("g_hws", [128, 4, 128], F32, st); b_hws = Buf()
        rws = P.sb("g_rws", [128, 8, 128], F32, st); b_rws = Buf()
        hwb = [P.sb("g_hwb%d" % i, [128, 4, 128], BF16, st) for i in range(2)]; b_hwb = [Buf(), Buf()]
        rwb = [P.sb("g_rwb%d" % i, [128, 8, 128], BF16, st) for i in range(2)]; b_rwb = [Buf(), Buf()]
        sga = [P.sb("g_sga%d" % i, [128, L], BF16, st) for i in range(2)]; b_sga = [Buf(), Buf()]
        sgb = [P.sb("g_sgb%d" % i, [128, L], BF16, st) for i in range(2)]; b_sgb = [Buf(), Buf()]
        mg = [P.sb("g_mg%d" % i, [128, L], BF16, st) for i in range(2)]; b_mg = [Buf(), Buf()]
        t1 = P.sb("g_t1", [128, 512], F32, st); b_t1 = Buf()
        t2 = P.sb("g_t2", [128, 512], F32, st); b_t2 = Buf()
        ph = [P.ps("g_ph%d" % i, [128, 512], F32, st) for i in range(2)]; b_ph = [Buf(), Buf()]
        pr = [P.ps("g_pr%d" % i, [128, 512], F32, st) for i in range(2)]; b_pr = [Buf(), Buf()]
        for h in range(8):
            S.dma("sp", lambda e, h=h: e.dma_start(out=yrg[:, h, :], in_=D_["yrgT"].ap()[h]), reads=[P.dbufs["yrgT"]], writes=[b_yrg])
        c = 0
        for m in range(8):
            k = m % 2
            S.dma("sp", lambda e, m=m: e.dma_start(out=hws[:], in_=hy_out[:, :, m * 128:(m + 1) * 128]), writes=[b_hws])
            S.dma("sp", lambda e, m=m: e.dma_start(out=rws[:], in_=rg_out[:, :, m * 128:(m + 1) * 128]), writes=[b_rws])
            S.op("pool", lambda e, k=k: e.tensor_copy(out=hwb[k][:], in_=hws[:]), reads=[b_hws], writes=[b_hwb[k]])
            S.op("pool", lambda e, k=k: e.tensor_copy(out=rwb[k][:], in_=rws[:]), reads=[b_rws], writes=[b_rwb[k]])
            S.dma("sp", lambda e, m=m, k=k: e.dma_start(out=sga[k][:], in_=D_["sgT"].ap()[m]), reads=[P.dbufs["sgT"]], writes=[b_sga[k]])
            S.dma("sp", lambda e, m=m, k=k: e.dma_start(out=sgb[k][:], in_=D_["sgT"].ap()[8 + m]), reads=[P.dbufs["sgT"]], writes=[b_sgb[k]])
            for n, (c0, cw) in enumerate(NCH):
                q = c % 2; c += 1
                for i in range(4):
                    S.op("pe", lambda e, i=i, q=q, k=k, c0=c0, cw=cw: e.matmul(ph[q][:, 0:cw], lhsT=hwb[k][:, i, :], rhs=yyT[:, i, c0:c0 + cw],
                                                                                start=(i == 0), stop=(i == 3)),
                         reads=[b_hwb[k], b_yyT], writes=[b_ph[q]])
                for i in range(8):
                    S.op("pe", lambda e, i=i, q=q, k=k, c0=c0, cw=cw: e.matmul(pr[q][:, 0:cw], lhsT=rwb[k][:, i, :], rhs=yrg[:, i, c0:c0 + cw],
                                                                                start=(i == 0), stop=(i == 7)),
                         reads=[b_rwb[k], b_yrg], writes=[b_pr[q]])
                S.op("dve", lambda e, q=q, k=k, c0=c0, cw=cw: e.tensor_tensor(out=t1[:, 0:cw], in0=ph[q][:, 0:cw], in1=sga[k][:, c0:c0 + cw], op=ALU.mult),
                     reads=[b_ph[q], b_sga[k]], writes=[b_t1])
                S.op("dve", lambda e, q=q, k=k, c0=c0, cw=cw: e.tensor_tensor(out=t2[:, 0:cw], in0=pr[q][:, 0:cw], in1=sgb[k][:, c0:c0 + cw], op=ALU.mult),
                     reads=[b_pr[q], b_sgb[k]], writes=[b_t2])
                S.op("dve", lambda e, k=k, c0=c0, cw=cw: e.tensor_tensor(out=mg[k][:, c0:c0 + cw], in0=t1[:, 0:cw], in1=t2[:, 0:cw], op=ALU.add),
                     reads=[b_t1, b_t2], writes=[b_mg[k]])
            S.dma("sp", lambda e, m=m, k=k: e.dma_start(out=mgT.ap()[m], in_=mg[k][:]), reads=[b_mg[k]], writes=[P.dbufs["mgT"]])
        S.barrier()


def phase_post(P, inp, G):
    nc, S = P.nc, P.S
    D_ = P.dram
    identb, b_idb = G["identb"], G["b_idb"]
    n2Td = P.dscr("n2Td", [128, 8, SEQ], BF16)
    h2d = P.dscr("h2d", [SEQ, D], F32)
    n2d = P.dscr("n2d", [SEQ, D], F32)
    x = inp["x"].ap()
    w_out = inp["w_out"].ap()[0].rearrange("(k p) c -> p k c", p=128)
    with ExitStack() as st:
        mg = P.sb("p_mg", [128, 8, L], BF16, st); b_mg = Buf()
        wos = P.sb("p_wos", [128, D], F32, st); b_wos = Buf()
        wob = P.sb("p_wob", [128, 8, D], BF16, st); b_wob = Buf()
        gf = P.sb("p_gf", [128, D], F32, st); b_gf = Buf()
        xt = [P.sb("p_xt%d" % i, [128, D], F32, st) for i in range(2)]; b_xt = [Buf(), Buf()]
        h2 = [P.sb("p_h2%d" % i, [128, D], F32, st) for i in range(2)]; b_h2 = [Buf(), Buf()]
        n2 = [P.sb("p_n2%d" % i, [128, D], F32, st) for i in range(2)]; b_n2 = [Buf(), Buf()]
        n2b = [P.sb("p_n2b%d" % i, [128, D], BF16, st) for i in range(2)]; b_n2b = [Buf(), Buf()]
        sq = P.sb("p_sq", [128, D], F32, st); b_sq = Buf()
        ssq = P.sb("p_ss", [128, 64], F32, st); b_ss = Buf()
        pm = [P.ps("p_pm%d" % i, [128, 2, 512], F32, st) for i in range(2)]; b_pm = [Buf(), Buf()]
        pt = [P.ps("p_pt%d" % i, [128, 8, 128], BF16, st) for i in range(2)]; b_pt = [Buf(), Buf()]
        n2Tt = [P.sb("p_n2Tt%d" % i, [128, 8, 128], BF16, st) for i in range(2)]; b_n2Tt = [Buf(), Buf()]
        for kk in range(8):
            S.dma("sp", lambda e, kk=kk: e.dma_start(out=mg[:, kk, :], in_=D_["mgT"].ap()[kk]), reads=[P.dbufs["mgT"]], writes=[b_mg])
            S.dma("sp", lambda e, kk=kk: e.dma_start(out=wos[:], in_=w_out[:, kk, :]), writes=[b_wos])
            S.op("pool", lambda e, kk=kk: e.tensor_copy(out=wob[:, kk, :], in_=wos[:]), reads=[b_wos], writes=[b_wob])
        S.dma("sp", lambda e: e.dma_start(out=gf[:], in_=inp["g_ffn"].ap().partition_broadcast(128)), writes=[b_gf])
        for j in range(32):
            k = j % 2
            p0 = NMETA + 128 * j
            S.dma("sp", lambda e, j=j, k=k: e.dma_start(out=xt[k][:], in_=x[128 * j:128 * (j + 1), :]), writes=[b_xt[k]])
            for half in range(2):
                for kk in range(8):
                    S.op("pe", lambda e, kk=kk, half=half, k=k, p0=p0: e.matmul(pm[k][:, half, :], lhsT=mg[:, kk, p0:p0 + 128],
                                                                                 rhs=wob[:, kk, half * 512:(half + 1) * 512],
                                                                                 start=(kk == 0), stop=(kk == 7)),
                         reads=[b_mg, b_wob], writes=[b_pm[k]])
            S.op("dve", lambda e, k=k: e.tensor_tensor(out=h2[k][:], in0=xt[k][:], in1=pm[k][:].rearrange("p a c -> p (a c)"), op=ALU.add),
                 reads=[b_xt[k], b_pm[k]], writes=[b_h2[k]])
            S.dma("sp", lambda e, j=j, k=k: e.dma_start(out=h2d.ap()[128 * j:128 * (j + 1), :], in_=h2[k][:]), reads=[b_h2[k]], writes=[P.dbufs["h2d"]])
            S.op("act", lambda e, k=k, j=j: e.activation(out=sq[:], in_=h2[k][:], func=AF.Square, accum_out=ssq[:, 2 * j:2 * j + 1]),
                 reads=[b_h2[k]], writes=[b_sq, b_ss])
            S.op("act", lambda e, j=j: e.activation(out=ssq[:, 2 * j + 1:2 * j + 2], in_=ssq[:, 2 * j:2 * j + 1], func=AF.Sqrt, scale=1.0 / D, bias=EPS),
                 reads=[b_ss], writes=[b_ss])
            S.op("dve", lambda e, j=j: e.reciprocal(out=ssq[:, 2 * j + 1:2 * j + 2], in_=ssq[:, 2 * j + 1:2 * j + 2]), reads=[b_ss], writes=[b_ss])
            S.op("dve", lambda e, k=k, j=j: e.scalar_tensor_tensor(out=n2[k][:], in0=h2[k][:], scalar=ssq[:, 2 * j + 1:2 * j + 2], in1=gf[:],
                                                                   op0=ALU.mult, op1=ALU.mult), reads=[b_h2[k], b_ss, b_gf], writes=[b_n2[k]])
            S.dma("sp", lambda e, j=j, k=k: e.dma_start(out=n2d.ap()[128 * j:128 * (j + 1), :], in_=n2[k][:]), reads=[b_n2[k]], writes=[P.dbufs["n2d"]])
            S.op("act", lambda e, k=k: e.copy(out=n2b[k][:], in_=n2[k][:]), reads=[b_n2[k]], writes=[b_n2b[k]])
            for c in range(8):
                S.op("pe", lambda e, k=k, c=c: e.transpose(pt[k][:, c, :], n2b[k][:, c * 128:(c + 1) * 128], identb[:]),
                     reads=[b_n2b[k], b_idb], writes=[b_pt[k]])
            S.op("act", lambda e, k=k: e.copy(out=n2Tt[k][:], in_=pt[k][:]), reads=[b_pt[k]], writes=[b_n2Tt[k]])
            S.dma("sp", lambda e, k=k, j=j: e.dma_start(out=n2Td.ap()[:, :, 128 * j:128 * (j + 1)], in_=n2Tt[k][:]), reads=[b_n2Tt[k]],
                  writes=[P.dbufs["n2Td"]])
        S.barrier()


def phase_peer_scores(P, inp, G):
    nc, S = P.nc, P.S
    identb, b_idb = G["identb"], G["b_idb"]
    TS, b_TS, TIf, b_TIf = G["TS"], G["b_TS"], G["TIf"], G["b_TIf"]
    wq = inp["peer_wq"].ap()[0].rearrange("(k p) c -> p k c", p=128)
    keys = inp["peer_keys"].ap()[0]
    with ExitStack() as st:
        n2T = P.sb("s_n2T", [128, 8, SEQ], BF16, st); b_n2T = Buf()
        for kk in range(8):
            S.dma("sp", lambda e, kk=kk: e.dma_start(out=n2T[:, kk, :], in_=P.dram["n2Td"].ap()[:, kk, :]), reads=[P.dbufs["n2Td"]], writes=[b_n2T])
        TIu = P.sb("s_tiu", [128, 32, 16, 16], U32, st); b_TIu = Buf()
        wqs = P.sb("s_wqs", [128, 8, 128], F32, st); b_wqs = Buf()
        wqb = [P.sb("s_wqb%d" % i, [128, 8, 128], BF16, st) for i in range(2)]; b_wqb = [Buf(), Buf()]
        kys = P.sb("s_kys", [128, 128], F32, st); b_kys = Buf()
        kyb = P.sb("s_kyb", [128, 128], BF16, st); b_kyb = Buf()
        kyT = [P.sb("s_kyT%d" % i, [128, 128], BF16, st) for i in range(2)]; b_kyT = [Buf(), Buf()]
        qTb = [P.sb("s_qT%d" % i, [128, SEQ], BF16, st) for i in range(2)]; b_qT = [Buf(), Buf()]
        sc = [P.sb("s_sc%d" % i, [128, 4, 128], F32, st) for i in range(2)]; b_sc = [Buf(), Buf()]
        sc2 = [P.sb("s_sc2_%d" % i, [128, 128], F32, st) for i in range(2)]; b_sc2 = [Buf(), Buf()]
        b_TSx = [Buf(), Buf()]; b_TIx = [Buf(), Buf()]
        pq = [P.ps("s_pq%d" % i, [128, 512], F32, st) for i in range(2)]; b_pq = [Buf(), Buf()]
        psc = [P.ps("s_ps%d" % i, [128, 4, 128], F32, st) for i in range(2)]; b_psc = [Buf(), Buf()]
        pkt = P.ps("s_pkt", [128, 128], BF16, st); b_pkt = Buf()
        c = 0; c2 = 0
        for qc in range(16):
            k = qc % 2
            S.dma("sp", lambda e, qc=qc: e.dma_start(out=wqs[:], in_=wq[:, :, qc * 128:(qc + 1) * 128]), writes=[b_wqs])
            S.op("pool", lambda e, k=k: e.tensor_copy(out=wqb[k][:], in_=wqs[:]), reads=[b_wqs], writes=[b_wqb[k]])
            S.dma("sp", lambda e, qc=qc: e.dma_start(out=kys[:], in_=keys[qc // 2, qc % 2]), writes=[b_kys])
            S.op("pool", lambda e: e.tensor_copy(out=kyb[:], in_=kys[:]), reads=[b_kys], writes=[b_kyb])
            S.op("pe", lambda e: e.transpose(pkt[:], kyb[:], identb[:]), reads=[b_kyb, b_idb], writes=[b_pkt])
            S.op("act", lambda e, k=k: e.copy(out=kyT[k][:], in_=pkt[:]), reads=[b_pkt], writes=[b_kyT[k]])
            for n in range(8):
                q = c % 2; c += 1
                for kk in range(8):
                    S.op("pe", lambda e, kk=kk, q=q, k=k, n=n: e.matmul(pq[q][:], lhsT=wqb[k][:, kk, :], rhs=n2T[:, kk, n * 512:(n + 1) * 512],
                                                                        start=(kk == 0), stop=(kk == 7)),
                         reads=[b_wqb[k], b_n2T], writes=[b_pq[q]])
                S.op("act", lambda e, q=q, k=k, n=n: e.copy(out=qTb[k][:, n * 512:(n + 1) * 512], in_=pq[q][:]), reads=[b_pq[q]], writes=[b_qT[k]])
            for jg in range(8):
                q = c2 % 2; c2 += 1
                for jj in range(4):
                    j = jg * 4 + jj
                    S.op("pe", lambda e, q=q, k=k, j=j, jj=jj: e.matmul(psc[q][:, jj, :], lhsT=qTb[k][:, 128 * j:128 * (j + 1)], rhs=kyT[k][:],
                                                                        start=True, stop=True),
                         reads=[b_qT[k], b_kyT[k]], writes=[b_psc[q]])
                S.op("act", lambda e, q=q: e.copy(out=sc[q][:], in_=psc[q][:]), reads=[b_psc[q]], writes=[b_sc[q]])
                for jp in range(0, 4, 2):
                    pair = [(jg * 4 + jp + u, jp + u, u) for u in range(2)]
                    for (j, jj, u) in pair:
                        S.op("dve", lambda e, q=q, j=j, jj=jj, qc=qc: e.max(out=TS[:, j, qc, 0:8], in_=sc[q][:, jj, :]), reads=[b_sc[q]], writes=[b_TSx[u]])
                    for (j, jj, u) in pair:
                        S.op("dve", lambda e, q=q, j=j, jj=jj, qc=qc: e.max_index(out=TIu[:, j, qc, 0:8], in_max=TS[:, j, qc, 0:8], in_values=sc[q][:, jj, :]),
                             reads=[b_sc[q], b_TSx[u]], writes=[b_TIx[u]])
                    for (j, jj, u) in pair:
                        S.op("dve", lambda e, q=q, j=j, jj=jj, qc=qc, u=u: e.match_replace(out=sc2[u][:], in_to_replace=TS[:, j, qc, 0:8], in_values=sc[q][:, jj, :],
                                                                                      imm_value=-1e30), reads=[b_sc[q], b_TSx[u]], writes=[b_sc2[u]])
                    for (j, jj, u) in pair:
                        S.op("dve", lambda e, j=j, qc=qc, u=u: e.max(out=TS[:, j, qc, 8:16], in_=sc2[u][:]), reads=[b_sc2[u]], writes=[b_TSx[u]])
                    for (j, jj, u) in pair:
                        S.op("dve", lambda e, j=j, qc=qc, u=u: e.max_index(out=TIu[:, j, qc, 8:16], in_max=TS[:, j, qc, 8:16], in_values=sc2[u][:]),
                             reads=[b_sc2[u], b_TSx[u]], writes=[b_TIx[u]])
        for u in range(2):
            S.op("dve", lambda e: e.engine_nop(), reads=[b_TSx[u], b_TIx[u]], writes=[b_TS, b_TIu])
        S.op("dve", lambda e: e.tensor_copy(out=TIf[:].rearrange("p a b c -> p (a b c)"), in_=TIu[:].rearrange("p a b c -> p (a b c)")),
             reads=[b_TIu], writes=[b_TIf])
        S.barrier()


def phase_peer_out(P, inp, G, out):
    nc, S = P.nc, P.S
    D_ = P.dram
    TS, b_TS, TIf, b_TIf = G["TS"], G["b_TS"], G["TIf"], G["b_TIf"]
    utab = inp["peer_u"].ap()[0]
    vtab = inp["peer_v"].ap()[0]
    h2d = D_["h2d"].ap(); n2d = D_["n2d"].ap()
    NEG = -1e30
    with ExitStack() as st:
        iota4 = P.sb("o_iota", [128, 8, 16, 16], F32, st); b_iota = Buf()
        io1 = P.sb("o_io1", [128, 16], I32, st); b_io1 = Buf()
        gfin = P.sb("o_gfin", [128, D], F32, st); b_gfin = Buf()
        cand = P.sb("o_cand", [128, 8, 256], F32, st); b_cand = Buf()
        cand2 = P.sb("o_cand2", [128, 256], F32, st); b_cand2 = Buf()
        cs = P.sb("o_cs", [128, 8, 16], F32, st); b_cs = Buf()
        ci = P.sb("o_ci", [128, 8, 16], U32, st); b_ci = Buf()
        ik = P.sb("o_ik", [128, 8, 16], U32, st); b_ik = Buf()
        jk = P.sb("o_jk", [128, 8, 16], U32, st); b_jk = Buf()
        ikf = P.sb("o_ikf", [128, 8, 16], F32, st); b_ikf = Buf()
        jkf = P.sb("o_jkf", [128, 8, 16], F32, st); b_jkf = Buf()
        oh = P.sb("o_oh", [128, 8, 16, 16], F32, st); b_oh = Buf()
        e1 = P.sb("o_e1", [128, 8, 16], F32, st); b_e1 = Buf()
        e2 = P.sb("o_e2", [128, 8, 16], F32, st); b_e2 = Buf()
        EI = [P.sb("o_ei%d" % i, [128, 128], U32, st) for i in range(2)]; b_EI = [Buf(), Buf()]
        gt = [P.sb("o_g%d" % i, [128, 8, 16], F32, st) for i in range(2)]; b_gt = [Buf(), Buf()]
        sm = P.sb("o_sm", [128, 16], F32, st); b_sm = Buf()
        scr = P.sb("o_scr", [128, 128], F32, st); b_scrc = [Buf() for _ in range(128)]
        act = P.sb("o_act", [128, 128], F32, st); b_actc = [Buf() for _ in range(128)]
        agc = P.sb("o_agc", [128, 128], F32, st); b_agc = [Buf() for _ in range(128)]
        NG = 16
        ug = [P.sb("o_ug%d" % i, [128, 2 * D], BF16, st) for i in range(NG)]; b_ug = [Buf() for _ in range(NG)]
        ND = 8
        dg = [P.sb("o_dg%d" % i, [128, 128], BF16, st) for i in range(ND)]; b_dg = [Buf() for _ in range(ND)]
        pacc = [P.ps("o_pacc%d" % i, [128, 2, 512], F32, st) for i in range(2)]; b_pacc = [Buf(), Buf()]
        identf, b_idf = G["identf"], G["b_idf"]
        uvt = D_["uvtab"].ap()
        junk = P.sb("o_junk", [128, D], F32, st); b_junk = Buf()
        djunk = P.ps("o_djunk", [128, D], F32, st)
        n2t = [P.sb("o_n2%d" % i, [128, D], F32, st) for i in range(2)]; b_n2t = [Buf(), Buf()]
        h2t = [P.sb("o_h2", [128, D], F32, st)] * 2; b_h2t = [Buf()] * 2
        acc = P.sb("o_acc", [128, D], F32, st); b_acc = Buf()
        ssq = P.sb("o_ss", [128, 64], F32, st); b_ss = Buf()

        S.op("pool", lambda e: e.iota(io1[:], pattern=[[1, 16]], base=0, channel_multiplier=0), writes=[b_io1])
        S.op("dve", lambda e: e.tensor_copy(out=iota4[:].rearrange("p a b c -> p (a b) c"),
                                            in_=io1[:].unsqueeze(1).to_broadcast([128, 128, 16])), reads=[b_io1], writes=[b_iota])
        S.dma("sp", lambda e: e.dma_start(out=gfin[:], in_=inp["g_final"].ap().partition_broadcast(128)), writes=[b_gfin])
        gcount = 0

        pend = []

        def QO(*a, **kw):
            pend.append((S.op, a, kw))

        def QD(*a, **kw):
            pend.append((S.dma, a, kw))

        def drain(n):
            for _ in range(min(n, len(pend))):
                f, a, kw = pend.pop(0)
                f(*a, **kw)

        def select(j):
            k = j % 2
            QD("sp", lambda e, j=j, k=k: e.dma_start(out=n2t[k][:], in_=n2d[128 * j:128 * (j + 1), :]), reads=[P.dbufs["n2d"]], writes=[b_n2t[k]])
            tsj = TS[:, j].rearrange("p (h two) i -> p h two i", two=2)
            tij = TIf[:, j].rearrange("p (h two) i -> p h two i", two=2)
            QO("dve", lambda e, tsj=tsj: e.tensor_tensor(out=cand[:].rearrange("p h (a b) -> p h a b", a=16),
                                                           in0=tsj[:, :, 0, :].unsqueeze(3).to_broadcast([128, 8, 16, 16]),
                                                           in1=tsj[:, :, 1, :].unsqueeze(2).to_broadcast([128, 8, 16, 16]), op=ALU.add),
                 reads=[b_TS], writes=[b_cand])
            for h in range(8):
                QO("dve", lambda e, h=h: e.max(out=cs[:, h, 0:8], in_=cand[:, h, :]), reads=[b_cand], writes=[b_cs])
                QO("dve", lambda e, h=h: e.max_index(out=ci[:, h, 0:8], in_max=cs[:, h, 0:8], in_values=cand[:, h, :]), reads=[b_cand, b_cs], writes=[b_ci])
                QO("dve", lambda e, h=h: e.match_replace(out=cand2[:], in_to_replace=cs[:, h, 0:8], in_values=cand[:, h, :], imm_value=NEG),
                     reads=[b_cand, b_cs], writes=[b_cand2])
                QO("dve", lambda e, h=h: e.max(out=cs[:, h, 8:16], in_=cand2[:]), reads=[b_cand2], writes=[b_cs])
                QO("dve", lambda e, h=h: e.max_index(out=ci[:, h, 8:16], in_max=cs[:, h, 8:16], in_values=cand2[:]), reads=[b_cand2, b_cs], writes=[b_ci])
            QO("dve", lambda e: e.tensor_single_scalar(out=ik[:], in_=ci[:], scalar=4, op=ALU.logical_shift_right), reads=[b_ci], writes=[b_ik])
            QO("dve", lambda e: e.tensor_single_scalar(out=jk[:], in_=ci[:], scalar=15, op=ALU.bitwise_and), reads=[b_ci], writes=[b_jk])
            QO("dve", lambda e: e.tensor_copy(out=ikf[:], in_=ik[:]), reads=[b_ik], writes=[b_ikf])
            QO("dve", lambda e: e.tensor_copy(out=jkf[:], in_=jk[:]), reads=[b_jk], writes=[b_jkf])
            for (kf_, b_kf_, half, dst, b_dst) in ((ikf, b_ikf, 0, e1, b_e1), (jkf, b_jkf, 1, e2, b_e2)):
                QO("dve", lambda e, kf_=kf_: e.tensor_tensor(out=oh[:], in0=iota4[:], in1=kf_[:].unsqueeze(3).to_broadcast([128, 8, 16, 16]),
                                                               op=ALU.is_equal), reads=[b_iota, b_kf_], writes=[b_oh])
                QO("dve", lambda e, half=half, tij=tij: e.tensor_tensor(out=oh[:], in0=oh[:],
                                                                          in1=tij[:, :, half, :].unsqueeze(2).to_broadcast([128, 8, 16, 16]), op=ALU.mult),
                     reads=[b_oh, b_TIf], writes=[b_oh])
                QO("dve", lambda e, dst=dst: e.tensor_reduce(out=dst[:], in_=oh[:], axis=AX.X, op=ALU.add), reads=[b_oh], writes=[b_dst])
            QO("dve", lambda e: e.scalar_tensor_tensor(out=e1[:], in0=e1[:], scalar=128.0, in1=e2[:], op0=ALU.mult, op1=ALU.add),
                 reads=[b_e1, b_e2], writes=[b_e1])
            QO("dve", lambda e, k=k: e.tensor_copy(out=EI[k][:], in_=e1[:].rearrange("p h k -> p (h k)")), reads=[b_e1], writes=[b_EI[k]])
            QO("dve", lambda e, k=k: e.tensor_tensor(out=gt[k][:], in0=cs[:], in1=cs[:, :, 0:1].to_broadcast([128, 8, 16]), op=ALU.subtract),
                 reads=[b_cs], writes=[b_gt[k]])
            QO("act", lambda e, k=k: e.activation(out=gt[k][:], in_=gt[k][:], func=AF.Exp), reads=[b_gt[k]], writes=[b_gt[k]])
            QO("dve", lambda e, k=k: e.tensor_reduce(out=sm[:, 0:8], in_=gt[k][:], axis=AX.X, op=ALU.add), reads=[b_gt[k]], writes=[b_sm])
            QO("dve", lambda e: e.reciprocal(out=sm[:, 8:16], in_=sm[:, 0:8]), reads=[b_sm], writes=[b_sm])
            QO("dve", lambda e, k=k: e.tensor_tensor(out=gt[k][:], in0=gt[k][:], in1=sm[:, 8:16].unsqueeze(2).to_broadcast([128, 8, 16]), op=ALU.mult),
                 reads=[b_gt[k], b_sm], writes=[b_gt[k]])

        pool_aligned = (len(S.dma_pool["pool"]) == NG)
        S.dma_next["pool"] = 0
        select(0)
        drain(len(pend))
        for j in range(32):
            k = j % 2
            if j + 1 < 32:
                select(j + 1)
            per_hk = (len(pend) + 95) // 96
            S.dma("sp", lambda e, j=j, k=k: e.dma_start(out=h2t[k][:], in_=h2d[128 * j:128 * (j + 1), :]), reads=[P.dbufs["h2d"]], writes=[b_h2t[k]])
            gflat = gt[k][:].rearrange("p h k -> p (h k)")
            for hk in range(128):
                drain(per_hk)
                g = gcount % NG; dsl = gcount % ND; gcount += 1
                S.dma("pool", lambda e, g=g, k=k, hk=hk: e.indirect_dma_start(out=ug[g][:], out_offset=None, in_=uvt,
                                                                              in_offset=IndirectOffsetOnAxis(ap=EI[k][:, hk:hk + 1], axis=0)),
                      reads=[b_EI[k], P.dbufs["uvtab"]], writes=[b_ug[g]], slot_covered=(gcount > NG and pool_aligned))
                S.op("dve", lambda e, g=g, k=k, hk=hk: e.scalar_tensor_tensor(out=djunk[:], in0=ug[g][:, 0:D], scalar=1.0, in1=n2t[k][:], op0=ALU.mult,
                                                                              op1=ALU.mult, accum_out=scr[:, hk:hk + 1]),
                     reads=[b_n2t[k]], weak_reads=[b_ug[g]], writes=[b_scrc[hk]])
                S.op("act", lambda e, hk=hk: e.activation(out=act[:, hk:hk + 1], in_=scr[:, hk:hk + 1], func=AF.Gelu_apprx_tanh),
                     reads=[b_scrc[hk]], writes=[b_actc[hk]])
                S.op("act", lambda e, hk=hk, gflat=gflat: e.activation(out=agc[:, hk:hk + 1], in_=act[:, hk:hk + 1], func=AF.Identity,
                                                                      scale=gflat[:, hk:hk + 1]),
                     reads=[b_actc[hk], b_gt[k]], writes=[b_agc[hk]])
                S.op("act", lambda e, hk=hk, dsl=dsl: e.activation(out=dg[dsl][:], in_=identf[:], func=AF.Identity, scale=agc[:, hk:hk + 1]),
                     reads=[b_idf, b_agc[hk]], writes=[b_dg[dsl]])
                for half in range(2):
                    S.op("pe", lambda e, g=g, k=k, hk=hk, dsl=dsl, half=half: e.matmul(pacc[k][:, half, :], lhsT=dg[dsl][:],
                                                                                      rhs=ug[g][:, D + half * 512:D + (half + 1) * 512],
                                                                                      start=(hk == 0), stop=(hk == 127)),
                         reads=[b_dg[dsl], b_ug[g]], writes=[b_pacc[k]])
            drain(len(pend))
            S.op("dve", lambda e, k=k: e.tensor_tensor(out=acc[:], in0=h2t[k][:], in1=pacc[k][:].rearrange("p a c -> p (a c)"), op=ALU.add),
                 reads=[b_h2t[k], b_pacc[k]], writes=[b_acc])
            S.op("act", lambda e, j=j: e.activation(out=junk[:], in_=acc[:], func=AF.Square, accum_out=ssq[:, 2 * j:2 * j + 1]),
                 reads=[b_acc], writes=[b_junk, b_ss])
            S.op("act", lambda e, j=j: e.activation(out=ssq[:, 2 * j + 1:2 * j + 2], in_=ssq[:, 2 * j:2 * j + 1], func=AF.Sqrt, scale=1.0 / D, bias=EPS),
                 reads=[b_ss], writes=[b_ss])
            S.op("dve", lambda e, j=j: e.reciprocal(out=ssq[:, 2 * j + 1:2 * j + 2], in_=ssq[:, 2 * j + 1:2 * j + 2]), reads=[b_ss], writes=[b_ss])
            S.op("dve", lambda e, k=k, j=j: e.scalar_tensor_tensor(out=junk[:], in0=acc[:], scalar=ssq[:, 2 * j + 1:2 * j + 2], in1=gfin[:],
                                                                   op0=ALU.mult, op1=ALU.mult), reads=[b_acc, b_ss, b_gfin], writes=[b_junk])
            S.dma("sp", lambda e, j=j, k=k: e.dma_start(out=out.ap()[128 * j:128 * (j + 1), :], in_=junk[:]), reads=[b_junk], writes=[P.dbufs["out"]])
        S.barrier()


INPUT_SPECS = [
    ("x", [SEQ, D]), ("meta", [NMETA, D]), ("g_final", [1, D]), ("g_mix", [1, D]), ("w_in", [1, D, D_IN]),
    ("b_gate", [1, 2 * D]), ("hy_conv_w", [1, 3, 3 * D_HY]), ("hy_conv_b", [1, 3 * D_HY]), ("hy_w1", [1, 33, 64]),
    ("hy_b1", [1, 64]), ("hy_w2", [1, 64, 64]), ("hy_b2", [1, 64]), ("hy_w3", [1, 64, 1024]), ("hy_b3", [1, 1024]),
    ("hy_freq", [1, 2, 64]), ("hy_skip", [1, D_HY]), ("hy_out", [1, D_HY, D]), ("rg_conv_w", [1, 2, 4, D_RG]),
    ("rg_conv_b", [1, 2, D_RG]), ("rg_gate_w", [1, 2, 2, 8, 128, 128]), ("rg_gate_b", [1, 2, 2, D_RG]),
    ("rg_lambda", [1, 2, D_RG]), ("rg_out", [1, D_RG, D]), ("w_out", [1, D, D]), ("g_ffn", [1, D]),
    ("peer_wq", [1, D, 2048]), ("peer_keys", [1, 8, 2, 128, 128]), ("peer_u", [1, 16384, D]), ("peer_v", [1, 16384, D]),
]
CONST_SPECS = [
    ("c_zT", [33, L], F32), ("c_negdelta", [128, 4], F32), ("c_tlin", [1, L], F32), ("c_wf", [128, NT], F32),
    ("c_tabC", [NT, 128, NT * 128], BF16), ("c_tabS", [NT, 128, NT * 128], BF16),
    ("c_identf", [128, 128], F32), ("c_identb", [128, 128], BF16),
]


def _dump(P, src_ap_fn, nrows_tiles, out, width=D, dtype=F32):
    S = P.S
    with ExitStack() as st:
        t = P.sb("dbg_t", [128, width], dtype, st); bt = Buf()
        for j in range(nrows_tiles):
            S.dma("sp", lambda e, j=j: e.dma_start(out=t[:], in_=src_ap_fn(j)), reads=list(P.dbufs.values()), writes=[bt])
            S.dma("sp", lambda e, j=j: e.dma_start(out=out.ap()[128 * j:128 * (j + 1), 0:width], in_=t[:]), reads=[bt], writes=[P.dbufs["out"]])


def build(stage="all"):
    P = Prog()
    nc = P.nc
    inp = {}
    for name, shape in INPUT_SPECS:
        inp[name] = P.din(name, shape, F32)
    for name, shape, dt in CONST_SPECS:
        P.din(name, shape, dt)
    out = P.dout("out", [SEQ, D], F32)
    G = {}
    with P.stack:
        P.S = S = Sched(nc, P.stack)
        phase_filter(P, inp, uv_hook=lambda st: phase_uvtab(P, inp, st))
        phase_params(P, inp, G)
        with ExitStack() as s_nT:
            G["nT"] = P.sb("nT", [128, 8, LP], BF16, s_nT); G["b_nT"] = Buf()
            phase_norm(P, inp, G)
            phase_mixer_in(P, inp, G)
        if stage == "mixin":
            S.finish("sp"); S.emit(); return P
        with ExitStack() as s_yy:
            G["yyT"] = P.sb("yyT", [128, 4, LP], BF16, s_yy); G["b_yyT"] = Buf()
            phase_dft(P, inp, G)
            phase_merge(P, inp, G)
        with ExitStack() as s_ts:
            phase_post(P, inp, G)
            G["TS"] = P.sb("TS", [128, 32, 16, 16], F32, s_ts); G["b_TS"] = Buf()
            G["TIf"] = P.sb("TIf", [128, 32, 16, 16], F32, s_ts); G["b_TIf"] = Buf()
            phase_peer_scores(P, inp, G)
            phase_peer_out(P, inp, G, out)
        S.finish("sp")
        S.emit()
    return P


def make_in_maps(inputs, n_cores=8):
    consts = _host_consts()
    maps = []
    for b in range(n_cores):
        m = {}
        for name, shape in INPUT_SPECS:
            a = inputs[name]
            if name == "x":
                a = a[b]
            m[name] = np.ascontiguousarray(np.asarray(a, dtype=np.float32).reshape(shape))
        for name, shape, dt in CONST_SPECS:
            m[name] = consts[name]
        maps.append(m)
    return maps


def kernel(**inputs):
    nc = build().nc
    in_maps = make_in_maps(inputs)
    res = run_bass_kernel_spmd(nc, in_maps, core_ids=list(range(8)))
    return np.stack([np.asarray(r["out"]).reshape(SEQ, D) for r in res.results], axis=0).astype(np.float32)
```

```python
import math
import os
from contextlib import ExitStack

import numpy as np
import ml_dtypes

import concourse.bass as bass
import concourse.mybir as mybir
from concourse.bass_utils import run_bass_kernel_spmd

F32 = mybir.dt.float32
BF16 = mybir.dt.bfloat16
I32 = mybir.dt.int32
U32 = mybir.dt.uint32
ALU = mybir.AluOpType
AF = mybir.ActivationFunctionType
AX = mybir.AxisListType

D = 1024
SEQ = 4096
NMETA = 16
L = SEQ + NMETA
NT = 33
LP = NT * 128
NFFT = 2 * L
NF = L + 1
D_HY = 512
D_RG = 1024
D_IN = 3 * D_HY + 2 * D_RG + 2 * D
EPS = 1e-6
NCH = [(n * 512, min(512, L - n * 512)) for n in range(9)]


def _rows(i):
    return 128 if i < NT - 1 else L - 128 * (NT - 1)


class Buf:
    __slots__ = ("name", "w", "r")

    def __init__(self, name=""):
        self.name = name
        self.w = None
        self.r = {}


class Sched:
    ENG = ("pe", "act", "dve", "pool", "sp")
    NDMA = {"sp": 24, "pool": 16, "act": 8}

    def __init__(self, nc, stack):
        self.nc = nc
        self.items = {e: [] for e in self.ENG}
        self.sems = {}
        self.cnt = {}
        self.seen = {e: {} for e in self.ENG}
        for e in self.ENG:
            self.sems[e] = stack.enter_context(nc.semaphore("s_" + e))
            self.cnt[e] = 0
        self.dma_pool = {}
        self.dma_next = {}
        for q, n in self.NDMA.items():
            keys = []
            for i in range(n):
                k = "d_%s_%d" % (q, i)
                self.sems[k] = stack.enter_context(nc.semaphore(k))
                self.cnt[k] = 0
                keys.append(k)
            self.dma_pool[q] = keys
            self.dma_next[q] = 0
        self.n_ops = 0

    def _wait(self, eng, ev):
        if ev is None:
            return
        k, v = ev
        if self.seen[eng].get(k, 0) >= v:
            return
        self.seen[eng][k] = v
        self.items[eng].append(("w", k, v))

    def _deps(self, eng, reads, writes):
        for b in reads:
            if b.w is not None:
                if not (eng == "pe" and b.w[0] == "pe"):
                    self._wait(eng, b.w)
        for b in writes:
            if b.w is not None:
                if not (eng == "pe" and b.w[0] == "pe"):
                    self._wait(eng, b.w)
            for k, v in b.r.items():
                if eng == "pe" and k == "pe":
                    continue
                self._wait(eng, (k, v))

    def _commit(self, ev, reads, writes):
        for b in writes:
            b.w = ev
            b.r = {}
        for b in reads:
            if b in writes:
                continue
            if b.r.get(ev[0], 0) < ev[1]:
                b.r[ev[0]] = ev[1]

    def op(self, eng, fn, reads=(), writes=(), weak_reads=()):
        self._deps(eng, list(reads) + list(weak_reads), writes)
        self.cnt[eng] += 1
        ev = (eng, self.cnt[eng])
        self.items[eng].append(("o", fn, eng, 1))
        self._commit(ev, reads, writes)
        self.n_ops += 1
        return ev

    def dma(self, q, fn, reads=(), writes=(), slot_covered=False):
        pool = self.dma_pool[q]
        k = pool[self.dma_next[q] % len(pool)]
        self.dma_next[q] += 1
        if slot_covered:
            self.seen[q][k] = max(self.seen[q].get(k, 0), self.cnt[k])
        else:
            self._wait(q, (k, self.cnt[k]))
        self._deps(q, reads, writes)
        self.cnt[k] += 16
        ev = (k, self.cnt[k])
        self.items[q].append(("o", fn, k, 16))
        self._commit(ev, reads, writes)
        self.n_ops += 1
        return ev

    def barrier(self):
        for e in self.ENG:
            for k, v in self.cnt.items():
                if v > 0 and k != e:
                    self._wait(e, (k, v))

    def finish(self, eng="sp"):
        for k, v in self.cnt.items():
            if v > 0 and k != eng:
                self._wait(eng, (k, v))

    def emit(self):
        nc = self.nc
        sems = self.sems
        items = self.items

        def replay(engine, lst):
            for it in lst:
                if it[0] == "w":
                    engine.wait_ge(sems[it[1]], it[2])
                else:
                    it[1](engine).then_inc(sems[it[2]], it[3])

        with nc.Block() as block:
            @block.sync
            def _(e):
                replay(e, items["sp"])

            @block.tensor
            def _(e):
                replay(e, items["pe"])

            @block.scalar
            def _(e):
                replay(e, items["act"])

            @block.vector
            def _(e):
                replay(e, items["dve"])

            @block.gpsimd
            def _(e):
                replay(e, items["pool"])


_CONST_CACHE = {}


def _host_consts():
    if _CONST_CACHE:
        return _CONST_CACHE
    f32 = np.float32
    t = np.linspace(0.0, 1.0, L, dtype=f32)[:, None]
    w = (f32(2.0 * math.pi / L)) * np.arange(L, dtype=f32)[:, None]
    bands = np.linspace(1e-4, 15, 16, dtype=f32)[None, :]
    z = np.concatenate([t, np.cos(bands * w), -np.sin(bands * w)], axis=-1).astype(f32)
    zT = np.ascontiguousarray(z.T)
    deltas = np.abs(np.linspace(math.log(1e-2) / 0.3, math.log(1e-2) / 1.5, D_HY, dtype=f32)).astype(f32)
    negdelta = np.ascontiguousarray((-deltas).reshape(4, 128).T)
    tlin = np.ascontiguousarray(t.reshape(1, L))
    fidx = np.arange(LP)
    wfv = np.where((fidx == 0) | (fidx == L), 1.0 / NFFT, 2.0 / NFFT)
    wfv = np.where(fidx <= L, wfv, 0.0).astype(f32)
    wf = np.ascontiguousarray(wfv.reshape(NT, 128).T)
    a = np.arange(LP, dtype=np.int64)
    prod = (a[:, None] * a[None, :]) % NFFT
    ang = prod.astype(np.float64) * (2.0 * math.pi / NFFT)
    C = np.cos(ang).astype(f32)
    S = np.sin(ang).astype(f32)
    del ang, prod

    def lay(M):
        M4 = M.reshape(NT, 128, NT, 128)
        return np.ascontiguousarray(M4.transpose(2, 1, 0, 3)).reshape(NT, 128, NT * 128).astype(ml_dtypes.bfloat16)

    _CONST_CACHE.update(dict(
        c_zT=zT, c_negdelta=negdelta, c_tlin=tlin, c_wf=wf, c_tabC=lay(C), c_tabS=lay(S),
        c_identf=np.eye(128, dtype=f32), c_identb=np.eye(128, dtype=f32).astype(ml_dtypes.bfloat16),
    ))
    return _CONST_CACHE


class Prog:
    def __init__(self, dbg=None):
        self.dbg = dbg or ()
        self.nc = bass.Bass("TRN2", target_bir_lowering=False)
        self.stack = ExitStack()
        self.S = None
        self.dram = {}
        self.dbufs = {}

    def din(self, name, shape, dtype=F32):
        t = self.nc.dram_tensor(name, list(shape), dtype, kind="ExternalInput")
        self.dram[name] = t
        self.dbufs[name] = Buf(name)
        return t

    def dout(self, name, shape, dtype=F32):
        t = self.nc.dram_tensor(name, list(shape), dtype, kind="ExternalOutput")
        self.dram[name] = t
        self.dbufs[name] = Buf(name)
        return t

    def dscr(self, name, shape, dtype=F32):
        t = self.nc.dram_tensor(name, list(shape), dtype)
        self.dram[name] = t
        self.dbufs[name] = Buf(name)
        return t

    def sb(self, name, shape, dtype=F32, stack=None):
        t = (stack or self.stack).enter_context(self.nc.sbuf_tensor(name, list(shape), dtype))
        return t

    def ps(self, name, shape, dtype=F32, stack=None):
        t = (stack or self.stack).enter_context(self.nc.psum_tensor(name, list(shape), dtype))
        return t


def _round_reduce(S, arg, tmp, buf_arg, buf_tmp, n):
    MAGIC = 12582912.0
    TWO_PI = 2.0 * math.pi
    S.op("dve", lambda e: e.tensor_scalar(out=tmp, in0=arg, scalar1=1.0 / TWO_PI, scalar2=MAGIC,
                                          op0=ALU.mult, op1=ALU.add), reads=[buf_arg], writes=[buf_tmp])
    S.op("dve", lambda e: e.tensor_scalar(out=tmp, in0=tmp, scalar1=MAGIC, scalar2=-TWO_PI,
                                          op0=ALU.subtract, op1=ALU.mult), reads=[buf_tmp], writes=[buf_tmp])
    S.op("dve", lambda e: e.tensor_tensor(out=arg, in0=arg, in1=tmp, op=ALU.add),
         reads=[buf_arg, buf_tmp], writes=[buf_arg])
    S.op("dve", lambda e: e.tensor_scalar(out=arg, in0=arg, scalar1=3.1415925, scalar2=-3.1415925,
                                          op0=ALU.min, op1=ALU.max), reads=[buf_arg], writes=[buf_arg])


def phase_filter(P, inp, uv_hook=None):
    nc, S = P.nc, P.S
    kspec = P.dscr("kspec", [NT, 128, 2, 512])
    bk = P.dbufs["kspec"]
    with ExitStack() as st0, ExitStack() as st:
        kst = P.sb("fkst", [128, NT, 512], BF16, st0); b_kst = Buf()
        kdt = P.sb("fkdt", [128, NT, 512], BF16, st0); b_kdt = Buf()
        wf = P.sb("fwf", [128, NT], F32, st0); b_wf = Buf()
        w1 = P.sb("fw1", [33, 64], F32, st); b_w1 = Buf()
        w2 = P.sb("fw2", [64, 64], F32, st); b_w2 = Buf()
        w3 = P.sb("fw3", [64, 1024], F32, st); b_w3 = Buf()
        sm = P.sb("fsm", [64, 8], F32, st); b_sm = Buf()
        b3 = P.sb("fb3", [128, 8], F32, st); b_b3 = Buf()
        ndl = P.sb("fndl", [128, 4], F32, st); b_ndl = Buf()
        tl = P.sb("ftl", [128, L], F32, st); b_tl = Buf()
        identb = P.sb("fidb", [128, 128], BF16, st); b_idb = Buf()
        kf = P.sb("fkf", [128, L], F32, st); b_kf = Buf()
        kb = P.sb("fkb", [128, L], F32, st); b_kb = Buf()
        hd2 = P.sb("fhd2", [64, L], F32, st); b_hd2 = Buf()
        win = P.sb("fwin", [128, L], F32, st); b_win = Buf()
        z_sb, b_z = kf, b_kf
        hd1, b_hd1 = kb, b_kb
        tmp, b_tmp = win, b_win
        ksb = P.sb("fksb", [128, LP], BF16, st); b_ksb = Buf()
        kdb = P.sb("fkdb", [128, LP], BF16, st); b_kdb = Buf()
        pa = [P.ps("fpa%d" % i, [128, 512], F32, st0) for i in range(2)]
        b_pa = [Buf(), Buf()]
        pb = [P.ps("fpb%d" % i, [128, 512], F32, st0) for i in range(2)]
        b_pb = [Buf(), Buf()]
        pt = [P.ps("fpt%d" % i, [128, 8, 128], BF16, st0) for i in range(2)]
        b_pt = [Buf(), Buf()]

        D_ = P.dram
        ld = lambda o, i, bw, name=None: S.dma("sp", lambda e: e.dma_start(out=o, in_=i), writes=[bw])
        ld(z_sb[0:33, :], D_["c_zT"].ap(), b_z)
        ld(w1[:], inp["hy_w1"].ap()[0], b_w1)
        ld(w2[:], inp["hy_w2"].ap()[0], b_w2)
        ld(w3[:], inp["hy_w3"].ap()[0], b_w3)
        ld(sm[:, 0:1], inp["hy_b1"].ap().rearrange("o (p u) -> (o p) u", u=1), b_sm)
        ld(sm[:, 1:2], inp["hy_b2"].ap().rearrange("o (p u) -> (o p) u", u=1), b_sm)
        ld(sm[:, 2:3], inp["hy_freq"].ap()[0, 0:1, :].rearrange("o (p u) -> (o p) u", u=1), b_sm)
        ld(sm[:, 3:4], inp["hy_freq"].ap()[0, 1:2, :].rearrange("o (p u) -> (o p) u", u=1), b_sm)
        for cc in range(8):
            ld(b3[:, cc:cc + 1], inp["hy_b3"].ap()[0:1, cc * 128:(cc + 1) * 128].rearrange("o (p u) -> (o p) u", u=1), b_b3)
        ld(ndl[:], D_["c_negdelta"].ap(), b_ndl)
        ld(tl[:], D_["c_tlin"].ap().partition_broadcast(128), b_tl)
        ld(wf[:], D_["c_wf"].ap(), b_wf)
        ld(identb[:], D_["c_identb"].ap(), b_idb)

        S.op("dve", lambda e: e.tensor_tensor(out=sm[:, 4:6], in0=sm[:, 0:2], in1=sm[:, 2:4], op=ALU.mult),
             reads=[b_sm], writes=[b_sm])

        def layer(wt, b_wt, kdim, src, b_src, arg, b_arg, fcol, fbcol):
            for n, (c0, cw) in enumerate(NCH):
                p = pa[n % 2]; bp = b_pa[n % 2]
                S.op("pe", lambda e, p=p, c0=c0, cw=cw: e.matmul(p[0:64, 0:cw], lhsT=wt[0:kdim, :], rhs=src[0:kdim, c0:c0 + cw],
                                                                   start=True, stop=True),
                     reads=[b_wt, b_src], writes=[bp])
                S.op("dve", lambda e, p=p, c0=c0, cw=cw: e.tensor_scalar(out=arg[0:64, c0:c0 + cw], in0=p[0:64, 0:cw],
                                                                       scalar1=sm[:, fcol:fcol + 1], scalar2=sm[:, fbcol:fbcol + 1],
                                                                       op0=ALU.mult, op1=ALU.add),
                     reads=[bp, b_sm], writes=[b_arg])
            _round_reduce(S, arg[0:64, :], tmp[0:64, :], b_arg, b_tmp, L)
            S.op("act", lambda e: e.activation(out=arg[0:64, :], in_=arg[0:64, :], func=AF.Sin), reads=[b_arg], writes=[b_arg])

        layer(w1, b_w1, 33, z_sb, b_z, hd1, b_hd1, 2, 4)
        layer(w2, b_w2, 64, hd1, b_hd1, hd2, b_hd2, 3, 5)

        tcount = [0]
        for q in range(4):
            S.op("act", lambda e, q=q: e.activation(out=win[:], in_=tl[:], func=AF.Exp, scale=ndl[:, q:q + 1]),
                 reads=[b_tl, b_ndl], writes=[b_win])
            for (cc, dst, b_dst) in ((q, kf, b_kf), (q + 4, kb, b_kb)):
                for n, (c0, cw) in enumerate(NCH):
                    p = pa[n % 2]; bp = b_pa[n % 2]
                    S.op("pe", lambda e, p=p, c0=c0, cw=cw, cc=cc: e.matmul(p[:, 0:cw], lhsT=w3[:, cc * 128:(cc + 1) * 128],
                                                                              rhs=hd2[:, c0:c0 + cw], start=True, stop=True),
                         reads=[b_w3, b_hd2], writes=[bp])
                    S.op("dve", lambda e, p=p, c0=c0, cw=cw, cc=cc, dst=dst: e.scalar_tensor_tensor(
                        out=dst[:, c0:c0 + cw], in0=p[:, 0:cw], scalar=b3[:, cc:cc + 1], in1=win[:, c0:c0 + cw],
                        op0=ALU.add, op1=ALU.mult), reads=[bp, b_b3, b_win], writes=[b_dst])
            S.op("dve", lambda e: e.memset(kb[:, 0:1], 0.0), writes=[b_kb])
            S.op("dve", lambda e: e.tensor_tensor(out=ksb[:, 0:L], in0=kf[:], in1=kb[:], op=ALU.add),
                 reads=[b_kf, b_kb], writes=[b_ksb])
            S.op("dve", lambda e: e.tensor_tensor(out=kdb[:, 0:L], in0=kf[:], in1=kb[:], op=ALU.subtract),
                 reads=[b_kf, b_kb], writes=[b_kdb])
            for (src, b_src, dstt, b_dstt) in ((ksb, b_ksb, kst, b_kst), (kdb, b_kdb, kdt, b_kdt)):
                for g0 in range(0, NT, 8):
                    g1 = min(NT, g0 + 8)
                    k = tcount[0] % 2; tcount[0] += 1
                    for i in range(g0, g1):
                        r = _rows(i)
                        S.op("pe", lambda e, i=i, r=r, k=k, g0=g0, src=src: e.transpose(pt[k][0:r, i - g0, :], src[:, i * 128:i * 128 + r],
                                                                                        identb[:]),
                             reads=[b_src, b_idb], writes=[b_pt[k]])
                    full = [i for i in range(g0, g1) if _rows(i) == 128]
                    if full:
                        nfull = len(full)
                        S.op("act", lambda e, k=k, g0=g0, nfull=nfull, q=q, dstt=dstt: e.copy(
                            out=dstt[:, g0:g0 + nfull, q * 128:(q + 1) * 128], in_=pt[k][:, 0:nfull, :]),
                            reads=[b_pt[k]], writes=[b_dstt])
                    if g1 == NT:
                        r = _rows(NT - 1)
                        S.op("act", lambda e, k=k, g0=g0, r=r, q=q, dstt=dstt: e.copy(
                            out=dstt[0:r, NT - 1, q * 128:(q + 1) * 128], in_=pt[k][0:r, NT - 1 - g0, :]),
                            reads=[b_pt[k]], writes=[b_dstt])

        S.barrier()
        st.close()
        tabc = [P.sb("ftabc%d" % i, [128, NT * 128], BF16, st0) for i in range(2)]
        tabs = [P.sb("ftabs%d" % i, [128, NT * 128], BF16, st0) for i in range(2)]
        b_tabc = [Buf(), Buf()]; b_tabs = [Buf(), Buf()]
        osp = [P.sb("fosp%d" % i, [128, 2, 512], F32, st0) for i in range(2)]
        b_osp = [Buf(), Buf()]
        if uv_hook is not None:
            uv_hook(st0)
        for j in range(NT):
            k = j % 2
            S.dma("sp", lambda e, j=j, k=k: e.dma_start(out=tabc[k][:], in_=D_["c_tabC"].ap()[j]), writes=[b_tabc[k]])
            S.dma("sp", lambda e, j=j, k=k: e.dma_start(out=tabs[k][:], in_=D_["c_tabS"].ap()[j]), writes=[b_tabs[k]])
            for i in range(NT):
                r = _rows(i)
                S.op("pe", lambda e, i=i, r=r, k=k: e.matmul(pa[k][:], lhsT=tabc[k][0:r, i * 128:(i + 1) * 128], rhs=kst[0:r, i, :],
                                                             start=(i == 0), stop=(i == NT - 1)),
                     reads=[b_tabc[k], b_kst], writes=[b_pa[k]])
            for i in range(NT):
                r = _rows(i)
                S.op("pe", lambda e, i=i, r=r, k=k: e.matmul(pb[k][:], lhsT=tabs[k][0:r, i * 128:(i + 1) * 128], rhs=kdt[0:r, i, :],
                                                             start=(i == 0), stop=(i == NT - 1)),
                     reads=[b_tabs[k], b_kdt], writes=[b_pb[k]])
            S.op("dve", lambda e, j=j, k=k: e.tensor_scalar(out=osp[k][:, 0, :], in0=pa[k][:], scalar1=wf[:, j:j + 1], scalar2=None,
                                                            op0=ALU.mult), reads=[b_pa[k], b_wf], writes=[b_osp[k]])
            S.op("dve", lambda e, j=j, k=k: e.tensor_scalar(out=osp[k][:, 1, :], in0=pb[k][:], scalar1=wf[:, j:j + 1], scalar2=None,
                                                            op0=ALU.mult), reads=[b_pb[k], b_wf], writes=[b_osp[k]])
            S.dma("sp", lambda e, j=j, k=k: e.dma_start(out=kspec.ap()[j], in_=osp[k][:]), reads=[b_osp[k]], writes=[bk])
        S.barrier()
    return kspec


from concourse.bass import IndirectOffsetOnAxis

def c_hcw(k, cc): return k * 12 + cc
def c_hcb(cc): return 36 + cc
def c_skip(i): return 48 + i
def c_rcw(d, j, h): return 52 + (d * 4 + j) * 8 + h
def c_gmix(k): return 116 + k
def c_rcb(d, h): return 128 + d * 8 + h
def c_rgb(d, g, h): return 144 + (d * 2 + g) * 8 + h
def c_lam(d, h): return 176 + d * 8 + h
def c_bg(m): return 192 + m
NPC = 208


def phase_params(P, inp, G):
    nc, S = P.nc, P.S
    st = P.stack
    PC = P.sb("PC", [128, NPC], F32, st); b_PC = Buf()
    CL = P.sb("CL", [128, 32], F32, st); b_CL = Buf()
    identf = P.sb("identf", [128, 128], F32, st); b_idf = Buf()
    identb = P.sb("identb", [128, 128], BF16, st); b_idb = Buf()
    G.update(PC=PC, b_PC=b_PC, CL=CL, b_CL=b_CL, identf=identf, b_idf=b_idf, identb=identb, b_idb=b_idb)
    S.dma("sp", lambda e: e.dma_start(out=identf[:], in_=P.dram["c_identf"].ap()), writes=[b_idf])
    S.dma("sp", lambda e: e.dma_start(out=identb[:], in_=P.dram["c_identb"].ap()), writes=[b_idb])
    with ExitStack() as s2:
        PR = [P.sb("PR0", [128, 128], F32, s2), P.sb("PR1", [128, 128], F32, s2)]
        b_PR = [Buf(), Buf()]
        pp = P.ps("pprm", [128, 128], F32, s2); b_pp = Buf()
        tmp = P.sb("prm_t", [128, 16 * 6], F32, s2); b_tmp = Buf()
        S.op("dve", lambda e: e.memset(PR[0][:], 0.0), writes=[b_PR[0]])
        S.op("dve", lambda e: e.memset(PR[1][:], 0.0), writes=[b_PR[1]])

        def ldrows(t, r0, src, n):
            S.dma("sp", lambda e: e.dma_start(out=PR[t][r0:r0 + n, :], in_=src), writes=[b_PR[t]])
        ldrows(0, 0, inp["hy_conv_w"].ap().rearrange("o k (c p) -> (o k c) p", p=128), 36)
        ldrows(0, 36, inp["hy_conv_b"].ap().rearrange("o (c p) -> (o c) p", p=128), 12)
        ldrows(0, 48, inp["hy_skip"].ap().rearrange("o (c p) -> (o c) p", p=128), 4)
        ldrows(0, 52, inp["rg_conv_w"].ap().rearrange("o d j (c p) -> (o d j c) p", p=128), 64)
        ldrows(0, 116, inp["g_mix"].ap().rearrange("o (c p) -> (o c) p", p=128), 8)
        ldrows(1, 0, inp["rg_conv_b"].ap().rearrange("o d (c p) -> (o d c) p", p=128), 16)
        ldrows(1, 16, inp["rg_gate_b"].ap().rearrange("o d g (c p) -> (o d g c) p", p=128), 32)
        ldrows(1, 48, inp["rg_lambda"].ap().rearrange("o d (c p) -> (o d c) p", p=128), 16)
        ldrows(1, 64, inp["b_gate"].ap().rearrange("o (c p) -> (o c) p", p=128), 16)
        for t, ncol in ((0, 128), (1, 80)):
            S.op("pe", lambda e, t=t, ncol=ncol: e.transpose(pp[:, 0:ncol], PR[t][0:ncol, :], identf[0:ncol, 0:ncol]),
                 reads=[b_PR[t], b_idf], writes=[b_pp])
            S.op("dve", lambda e, t=t, ncol=ncol: e.tensor_copy(out=PC[:, t * 128:t * 128 + ncol], in_=pp[:, 0:ncol]),
                 reads=[b_pp], writes=[b_PC])
        lam = PC[:, 176:192]
        al, xx, ss, s2_, acc, pw = [tmp[:, i * 16:(i + 1) * 16] for i in range(6)]
        R = [b_PC, b_tmp]; W = [b_tmp]
        S.op("dve", lambda e: e.tensor_scalar(out=al, in0=lam, scalar1=-1.0, scalar2=None, op0=ALU.mult), reads=R, writes=W)
        S.op("dve", lambda e: e.tensor_tensor(out=al, in0=al, in1=lam, op=ALU.max), reads=R, writes=W)
        S.op("act", lambda e: e.activation(out=xx, in_=al, func=AF.Exp, scale=-1.0), reads=R, writes=W)
        S.op("dve", lambda e: e.tensor_scalar(out=ss, in0=xx, scalar1=2.0, scalar2=None, op0=ALU.add), reads=R, writes=W)
        S.op("dve", lambda e: e.reciprocal(out=ss, in_=ss), reads=R, writes=W)
        S.op("dve", lambda e: e.tensor_tensor(out=ss, in0=ss, in1=xx, op=ALU.mult), reads=R, writes=W)
        S.op("dve", lambda e: e.tensor_tensor(out=s2_, in0=ss, in1=ss, op=ALU.mult), reads=R, writes=W)
        S.op("dve", lambda e: e.tensor_copy(out=acc, in_=ss), reads=R, writes=W)
        S.op("dve", lambda e: e.tensor_copy(out=pw, in_=ss), reads=R, writes=W)
        for kk in (3, 5, 7, 9, 11, 13):
            S.op("dve", lambda e: e.tensor_tensor(out=pw, in0=pw, in1=s2_, op=ALU.mult), reads=R, writes=W)
            S.op("dve", lambda e, kk=kk: e.scalar_tensor_tensor(out=acc, in0=pw, scalar=1.0 / kk, in1=acc, op0=ALU.mult, op1=ALU.add),
                 reads=R, writes=W)
        S.op("dve", lambda e: e.tensor_scalar(out=al, in0=lam, scalar1=-1.0, scalar2=0.0, op0=ALU.mult, op1=ALU.max), reads=R, writes=W)
        S.op("dve", lambda e: e.scalar_tensor_tensor(out=acc, in0=acc, scalar=2.0, in1=al, op0=ALU.mult, op1=ALU.add), reads=R, writes=W)
        S.op("dve", lambda e: e.tensor_scalar(out=CL[:, 0:16], in0=acc, scalar1=-8.0, scalar2=None, op0=ALU.mult), reads=R, writes=[b_CL])
        S.op("dve", lambda e: e.tensor_scalar(out=CL[:, 16:32], in0=acc, scalar1=-16.0, scalar2=None, op0=ALU.mult), reads=R, writes=[b_CL])
        S.barrier()


def phase_uvtab(P, inp, st):
    nc, S = P.nc, P.S
    uvtab = P.dscr("uvtab", [16384, 2048], BF16)
    utab = inp["peer_u"].ap()[0]; vtab = inp["peer_v"].ap()[0]
    us = [P.sb("t_us%d" % i, [128, D], F32, st) for i in range(2)]; b_us = [Buf(), Buf()]
    vs = [P.sb("t_vs%d" % i, [128, D], F32, st) for i in range(2)]; b_vs = [Buf(), Buf()]
    ob = [P.sb("t_ob%d" % i, [128, 2 * D], BF16, st) for i in range(2)]; b_ob = [Buf(), Buf()]
    for c in range(128):
        k = c % 2
        S.dma("pool", lambda e, c=c, k=k: e.dma_start(out=us[k][:], in_=utab[128 * c:128 * (c + 1), :]), writes=[b_us[k]])
        S.dma("pool", lambda e, c=c, k=k: e.dma_start(out=vs[k][:], in_=vtab[128 * c:128 * (c + 1), :]), writes=[b_vs[k]])
        if c >= 1:
            kp = (c - 1) % 2
            S.op("act", lambda e, kp=kp: e.copy(out=ob[kp][:, 0:D], in_=us[kp][:]), reads=[b_us[kp]], writes=[b_ob[kp]])
            S.op("act", lambda e, kp=kp: e.copy(out=ob[kp][:, D:2 * D], in_=vs[kp][:]), reads=[b_vs[kp]], writes=[b_ob[kp]])
            S.dma("pool", lambda e, c=c, kp=kp: e.dma_start(out=uvtab.ap()[128 * (c - 1):128 * c, :], in_=ob[kp][:]), reads=[b_ob[kp]],
                  writes=[P.dbufs["uvtab"]])
    kp = 127 % 2
    S.op("act", lambda e: e.copy(out=ob[kp][:, 0:D], in_=us[kp][:]), reads=[b_us[kp]], writes=[b_ob[kp]])
    S.op("act", lambda e: e.copy(out=ob[kp][:, D:2 * D], in_=vs[kp][:]), reads=[b_vs[kp]], writes=[b_ob[kp]])
    S.dma("pool", lambda e: e.dma_start(out=uvtab.ap()[128 * 127:128 * 128, :], in_=ob[kp][:]), reads=[b_ob[kp]], writes=[P.dbufs["uvtab"]])


def phase_norm(P, inp, G):
    nc, S = P.nc, P.S
    nT = G["nT"]; b_nT = G["b_nT"]
    identb, b_idb = G["identb"], G["b_idb"]
    x = inp["x"].ap(); meta = inp["meta"].ap()
    with ExitStack() as st:
        ht = [P.sb("n_h%d" % i, [128, D], F32, st) for i in range(2)]; b_ht = [Buf(), Buf()]
        sq = P.sb("n_sq", [128, D], F32, st); b_sq = Buf()
        nb = [P.sb("n_nb%d" % i, [128, D], BF16, st) for i in range(2)]; b_nb = [Buf(), Buf()]
        ssq = P.sb("n_ss", [128, 2 * NT], F32, st); b_ss = Buf()
        pt = [P.ps("n_pt%d" % i, [128, 8, 128], BF16, st) for i in range(2)]; b_pt = [Buf(), Buf()]
        for j in range(NT):
            k = j % 2
            r = _rows(j)
            if j == 0:
                S.dma("sp", lambda e, k=k: e.dma_start(out=ht[k][0:NMETA, :], in_=meta), writes=[b_ht[k]])
                S.dma("sp", lambda e, k=k: e.dma_start(out=ht[k][NMETA:128, :], in_=x[0:128 - NMETA, :]), writes=[b_ht[k]])
            else:
                S.dma("sp", lambda e, k=k, j=j, r=r: e.dma_start(out=ht[k][0:r, :], in_=x[128 * j - NMETA:128 * j - NMETA + r, :]),
                      writes=[b_ht[k]])
            S.op("act", lambda e, k=k, j=j, r=r: e.activation(out=sq[0:r, :], in_=ht[k][0:r, :], func=AF.Square,
                                                              accum_out=ssq[0:r, 2 * j:2 * j + 1]),
                 reads=[b_ht[k]], writes=[b_sq, b_ss])
            S.op("act", lambda e, j=j, r=r: e.activation(out=ssq[0:r, 2 * j + 1:2 * j + 2], in_=ssq[0:r, 2 * j:2 * j + 1], func=AF.Sqrt,
                                                         scale=1.0 / D, bias=EPS), reads=[b_ss], writes=[b_ss])
            S.op("dve", lambda e, j=j, r=r: e.reciprocal(out=ssq[0:r, 2 * j + 1:2 * j + 2], in_=ssq[0:r, 2 * j + 1:2 * j + 2]),
                 reads=[b_ss], writes=[b_ss])
            S.op("dve", lambda e, k=k, j=j, r=r: e.tensor_scalar(out=nb[k][0:r, :], in0=ht[k][0:r, :], scalar1=ssq[0:r, 2 * j + 1:2 * j + 2],
                                                                  scalar2=None, op0=ALU.mult), reads=[b_ht[k], b_ss], writes=[b_nb[k]])
            for c in range(8):
                S.op("pe", lambda e, k=k, c=c, r=r: e.transpose(pt[k][:, c, 0:r], nb[k][0:r, c * 128:(c + 1) * 128], identb[0:r, 0:r]),
                     reads=[b_nb[k], b_idb], writes=[b_pt[k]])
            S.op("act", lambda e, k=k, j=j, r=r: e.copy(out=nT[:, :, 128 * j:128 * j + r], in_=pt[k][:, :, 0:r]),
                 reads=[b_pt[k]], writes=[b_nT])
        S.barrier()


def phase_mixer_in(P, inp, G):
    nc, S = P.nc, P.S
    nT, b_nT, PC, b_PC, CL, b_CL = G["nT"], G["b_nT"], G["PC"], G["b_PC"], G["CL"], G["b_CL"]
    identb, b_idb = G["identb"], G["b_idb"]
    w_in = inp["w_in"].ap()[0].rearrange("(k p) c -> p k c", p=128)
    yrgT = P.dscr("yrgT", [8, 128, L], BF16)
    sgT = P.dscr("sgT", [16, 128, L], BF16)
    x0cT = P.dscr("x0cT", [4, 128, L], F32)
    zT = P.dscr("zT", [4, 128, L], F32)
    with ExitStack() as st0:
        wst = [P.sb("m_wst%d" % i, [128, 8, 128], F32, st0) for i in range(2)]; b_wst = [Buf(), Buf()]
        wbf = [P.sb("m_wbf%d" % i, [128, 8, 128], BF16, st0) for i in range(2)]; b_wbf = [Buf(), Buf()]
        pp = [P.ps("m_pp%d" % i, [128, 512], F32, st0) for i in range(2)]; b_pp = [Buf(), Buf()]
        pg = [P.ps("m_pg%d" % i, [128, 512], F32, st0) for i in range(2)]; b_pg = [Buf(), Buf()]
        pt = [P.ps("m_pt%d" % i, [128, 8, 128], BF16, st0) for i in range(2)]; b_pt = [Buf(), Buf()]
        cnt = {"w": 0, "p": 0, "g": 0, "t": 0}
        gm_b = PC[:, 116:124].unsqueeze(2).to_broadcast([128, 8, 128])

        def project(cc, evac):
            k = cnt["w"] % 2; cnt["w"] += 1
            S.dma("sp", lambda e: e.dma_start(out=wst[k][:], in_=w_in[:, :, cc * 128:(cc + 1) * 128]), writes=[b_wst[k]])
            S.op("pool", lambda e: e.tensor_tensor(out=wbf[k][:], in0=wst[k][:], in1=gm_b, op=ALU.mult),
                 reads=[b_wst[k], b_PC], writes=[b_wbf[k]])
            for n, (c0, cw) in enumerate(NCH):
                q = cnt["p"] % 2; cnt["p"] += 1
                for kk in range(8):
                    S.op("pe", lambda e, kk=kk, q=q, c0=c0, cw=cw: e.matmul(pp[q][:, 0:cw], lhsT=wbf[k][:, kk, :], rhs=nT[:, kk, c0:c0 + cw],
                                                                             start=(kk == 0), stop=(kk == 7)),
                         reads=[b_wbf[k], b_nT], writes=[b_pp[q]])
                evac(pp[q], b_pp[q], c0, cw)

        with ExitStack() as st:
            xr = P.sb("r_xr", [128, L + 6], F32, st); b_xr = Buf()
            ggs = [P.sb("r_gg%d" % i, [128, L], BF16, st) for i in range(2)]; b_ggs = [Buf(), Buf()]
            xcb = P.sb("r_xcb", [128, L], BF16, st); b_xcb = Buf()
            rr = P.sb("r_rr", [128, L], F32, st); b_rr = Buf()
            ii = P.sb("r_ii", [128, L], F32, st); b_ii = Buf()
            aa = P.sb("r_aa", [128, L], F32, st); b_aa = Buf()
            hacc = P.sb("r_ha", [128, L], F32, st); b_ha = Buf()
            yb = P.sb("r_yb", [128, L], BF16, st); b_yb = Buf()
            gwss = [P.sb("r_gws%d" % i, [128, 4, 128], F32, st) for i in range(2)]; b_gwss = [Buf(), Buf()]
            gwbs = [P.sb("r_gwb%d" % i, [128, 4, 128], BF16, st) for i in range(2)]; b_gwbs = [Buf(), Buf()]
            S.op("dve", lambda e: e.memset(xr[:, 0:3], 0.0), writes=[b_xr])
            S.op("dve", lambda e: e.memset(xr[:, L + 3:L + 6], 0.0), writes=[b_xr])
            gw = inp["rg_gate_w"].ap()[0]
            def prefetch(h):
                gg, b_gg = ggs[h % 2], b_ggs[h % 2]
                gws, b_gws, gwb, b_gwb = gwss[h % 2], b_gwss[h % 2], gwbs[h % 2], b_gwbs[h % 2]
                project(12 + h, lambda p, bp, c0, cw: S.op("act", lambda e: e.copy(out=xr[:, 3 + c0:3 + c0 + cw], in_=p[:, 0:cw]),
                                                          reads=[bp], writes=[b_xr]))
                project(20 + h, lambda p, bp, c0, cw: S.op("act", lambda e: e.activation(out=gg[:, c0:c0 + cw], in_=p[:, 0:cw],
                                                                                        func=AF.Gelu_apprx_tanh),
                                                          reads=[bp], writes=[b_gg]))
                S.dma("sp", lambda e: e.dma_start(out=gws[:], in_=gw[:, :, h].rearrange("d g i j -> i (d g) j")), writes=[b_gws])
                S.op("pool", lambda e: e.tensor_copy(out=gwb[:], in_=gws[:]), reads=[b_gws], writes=[b_gwb])

            prefetch(0)
            for h in range(8):
                gg, b_gg = ggs[h % 2], b_ggs[h % 2]
                gwb, b_gwb = gwbs[h % 2], b_gwbs[h % 2]
                for d in range(2):
                    def xs(j, d=d):
                        off = 3 - j if d == 0 else 3 + j
                        return xr[:, off:off + L]
                    S.op("dve", lambda e, d=d, h=h, xv=xs(0): e.tensor_scalar(out=ii[:], in0=xv, scalar1=PC[:, c_rcw(d, 0, h):c_rcw(d, 0, h) + 1],
                                                                   scalar2=PC[:, c_rcb(d, h):c_rcb(d, h) + 1], op0=ALU.mult, op1=ALU.add),
                         reads=[b_xr, b_PC], writes=[b_ii])
                    for j in (1, 2):
                        S.op("dve", lambda e, d=d, h=h, j=j, xv=xs(j): e.scalar_tensor_tensor(out=ii[:], in0=xv, scalar=PC[:, c_rcw(d, j, h):c_rcw(d, j, h) + 1],
                                                                                   in1=ii[:], op0=ALU.mult, op1=ALU.add),
                             reads=[b_xr, b_PC, b_ii], writes=[b_ii])
                    S.op("dve", lambda e, d=d, h=h, xv=xs(3): e.scalar_tensor_tensor(out=xcb[:], in0=xv, scalar=PC[:, c_rcw(d, 3, h):c_rcw(d, 3, h) + 1],
                                                                          in1=ii[:], op0=ALU.mult, op1=ALU.add),
                         reads=[b_xr, b_PC, b_ii], writes=[b_xcb])
                    for g, (dst, b_dst) in enumerate(((rr, b_rr), (ii, b_ii))):
                        for n, (c0, cw) in enumerate(NCH):
                            q = cnt["g"] % 2; cnt["g"] += 1
                            S.op("pe", lambda e, q=q, c0=c0, cw=cw, d=d, g=g, gwb=gwb: e.matmul(pg[q][:, 0:cw], lhsT=gwb[:, d * 2 + g, :],
                                                                                      rhs=xcb[:, c0:c0 + cw], start=True, stop=True),
                                 reads=[b_gwb, b_xcb], writes=[b_pg[q]])
                            S.op("act", lambda e, q=q, c0=c0, cw=cw, d=d, g=g, h=h, dst=dst: e.activation(
                                out=dst[:, c0:c0 + cw], in_=pg[q][:, 0:cw], func=AF.Sigmoid,
                                bias=PC[:, c_rgb(d, g, h):c_rgb(d, g, h) + 1]), reads=[b_pg[q], b_PC], writes=[b_dst])
                    col = d * 8 + h
                    S.op("act", lambda e, col=col: e.activation(out=aa[:], in_=rr[:], func=AF.Exp, scale=CL[:, col:col + 1]),
                         reads=[b_rr, b_CL], writes=[b_aa])
                    S.op("act", lambda e, col=col: e.activation(out=rr[:], in_=rr[:], func=AF.Exp, scale=CL[:, 16 + col:17 + col]),
                         reads=[b_rr, b_CL], writes=[b_rr])
                    S.op("dve", lambda e: e.tensor_scalar(out=rr[:], in0=rr[:], scalar1=1.0, scalar2=None, op0=ALU.min),
                         reads=[b_rr], writes=[b_rr])
                    S.op("act", lambda e: e.activation(out=rr[:], in_=rr[:], func=AF.Sqrt, scale=-1.0, bias=1.0), reads=[b_rr], writes=[b_rr])
                    if d == 1 and h + 1 < 8:
                        prefetch(h + 1)
                    first = 0 if d == 0 else L - 1
                    S.op("dve", lambda e, first=first: e.memset(rr[:, first:first + 1], 1.0), writes=[b_rr])
                    S.op("dve", lambda e: e.tensor_tensor(out=ii[:], in0=ii[:], in1=rr[:], op=ALU.mult), reads=[b_ii, b_rr], writes=[b_ii])
                    S.op("dve", lambda e: e.tensor_tensor(out=ii[:], in0=ii[:], in1=xcb[:], op=ALU.mult), reads=[b_ii, b_xcb], writes=[b_ii])
                    if d == 0:
                        S.op("dve", lambda e: e.tensor_tensor_scan(out=hacc[:], data0=aa[:], data1=ii[:], initial=0.0,
                                                                   op0=ALU.mult, op1=ALU.add), reads=[b_aa, b_ii], writes=[b_ha])
                    else:
                        S.op("dve", lambda e: e.tensor_tensor_scan(out=rr[:, ::-1], data0=aa[:, ::-1], data1=ii[:, ::-1], initial=0.0,
                                                                   op0=ALU.mult, op1=ALU.add), reads=[b_aa, b_ii], writes=[b_rr])
                S.op("dve", lambda e: e.tensor_tensor(out=hacc[:], in0=hacc[:], in1=rr[:], op=ALU.add), reads=[b_ha, b_rr], writes=[b_ha])
                S.op("dve", lambda e, gg=gg: e.tensor_tensor(out=yb[:], in0=hacc[:], in1=gg[:], op=ALU.mult), reads=[b_ha, b_gg], writes=[b_yb])
                S.dma("sp", lambda e, h=h: e.dma_start(out=yrgT.ap()[h], in_=yb[:]), reads=[b_yb], writes=[P.dbufs["yrgT"]])
            S.barrier()

        with ExitStack() as st:
            sg = [P.sb("g_sg%d" % i, [128, L], BF16, st) for i in range(2)]; b_sg = [Buf(), Buf()]
            for m in range(16):
                k = m % 2
                project(28 + m, lambda p, bp, c0, cw, m=m, k=k: S.op("act", lambda e: e.activation(
                    out=sg[k][:, c0:c0 + cw], in_=p[:, 0:cw], func=AF.Sigmoid, bias=PC[:, c_bg(m):c_bg(m) + 1]),
                    reads=[bp, b_PC], writes=[b_sg[k]]))
                S.dma("sp", lambda e, m=m, k=k: e.dma_start(out=sgT.ap()[m], in_=sg[k][:]), reads=[b_sg[k]], writes=[P.dbufs["sgT"]])
            S.barrier()

        ztd = P.dscr("ztd", [128, NT, 512], BF16)
        with ExitStack() as st:
            zt = P.sb("h_zt", [128, NT, 512], BF16, st); b_zt = Buf()
            S.op("pool", lambda e: e.memset(zt[:, NT - 1, :], 0.0), writes=[b_zt])
            xas = [P.sb("h_xa%d" % i, [128, L + 2], F32, st) for i in range(2)]; b_xas = [Buf(), Buf()]
            xcnt = [0]
            ua = P.sb("h_ua", [128, L], F32, st); b_ua = Buf()
            ub = P.sb("h_ub", [128, L], F32, st); b_ub = Buf()
            zb = P.sb("h_zb", [128, LP], BF16, st); b_zb = Buf()
            for i_ in range(2):
                S.op("dve", lambda e, i_=i_: e.memset(xas[i_][:, 0:1], 0.0), writes=[b_xas[i_]])
                S.op("dve", lambda e, i_=i_: e.memset(xas[i_][:, L + 1:L + 2], 0.0), writes=[b_xas[i_]])

            def conv3(cc, dst, b_dst):
                xa, b_xa = xas[xcnt[0] % 2], b_xas[xcnt[0] % 2]
                xcnt[0] += 1
                project(cc, lambda p, bp, c0, cw: S.op("act", lambda e: e.copy(out=xa[:, 1 + c0:1 + c0 + cw], in_=p[:, 0:cw]),
                                                      reads=[bp], writes=[b_xa]))
                S.op("dve", lambda e: e.tensor_scalar(out=dst[:], in0=xa[:, 1:L + 1], scalar1=PC[:, c_hcw(1, cc):c_hcw(1, cc) + 1],
                                                      scalar2=PC[:, c_hcb(cc):c_hcb(cc) + 1], op0=ALU.mult, op1=ALU.add),
                     reads=[b_xa, b_PC], writes=[b_dst])
                S.op("dve", lambda e: e.scalar_tensor_tensor(out=dst[:], in0=xa[:, 0:L], scalar=PC[:, c_hcw(0, cc):c_hcw(0, cc) + 1],
                                                             in1=dst[:], op0=ALU.mult, op1=ALU.add), reads=[b_xa, b_PC, b_dst], writes=[b_dst])
                S.op("dve", lambda e: e.scalar_tensor_tensor(out=dst[:], in0=xa[:, 2:L + 2], scalar=PC[:, c_hcw(2, cc):c_hcw(2, cc) + 1],
                                                             in1=dst[:], op0=ALU.mult, op1=ALU.add), reads=[b_xa, b_PC, b_dst], writes=[b_dst])

            for i in range(4):
                conv3(i, ua, b_ua)
                S.dma("sp", lambda e, i=i: e.dma_start(out=x0cT.ap()[i], in_=ua[:]), reads=[b_ua], writes=[P.dbufs["x0cT"]])
            for i in range(4):
                conv3(4 + i, ua, b_ua)
                conv3(8 + i, ub, b_ub)
                S.op("dve", lambda e: e.tensor_tensor(out=ua[:], in0=ua[:], in1=ub[:], op=ALU.mult), reads=[b_ua, b_ub], writes=[b_ua])
                S.dma("sp", lambda e, i=i: e.dma_start(out=zT.ap()[i], in_=ua[:]), reads=[b_ua], writes=[P.dbufs["zT"]])
                S.op("act", lambda e: e.copy(out=zb[:, 0:L], in_=ua[:]), reads=[b_ua], writes=[b_zb])
                for g0 in range(0, NT, 8):
                    g1 = min(NT, g0 + 8)
                    k = cnt["t"] % 2; cnt["t"] += 1
                    for t in range(g0, g1):
                        r = _rows(t)
                        S.op("pe", lambda e, t=t, r=r, k=k, g0=g0: e.transpose(pt[k][0:r, t - g0, :], zb[:, t * 128:t * 128 + r], identb[:]),
                             reads=[b_zb, b_idb], writes=[b_pt[k]])
                    nfull = len([t for t in range(g0, g1) if _rows(t) == 128])
                    if nfull:
                        S.op("act", lambda e, k=k, g0=g0, nfull=nfull, i=i: e.copy(out=zt[:, g0:g0 + nfull, i * 128:(i + 1) * 128],
                                                                                 in_=pt[k][:, 0:nfull, :]), reads=[b_pt[k]], writes=[b_zt])
                    if g1 == NT:
                        r = _rows(NT - 1)
                        S.op("act", lambda e, k=k, g0=g0, r=r, i=i: e.copy(out=zt[0:r, NT - 1, i * 128:(i + 1) * 128],
                                                                         in_=pt[k][0:r, NT - 1 - g0, :]), reads=[b_pt[k]], writes=[b_zt])
            S.dma("sp", lambda e: e.dma_start(out=ztd.ap(), in_=zt[:]), reads=[b_zt], writes=[P.dbufs["ztd"]])
            S.barrier()


def phase_dft(P, inp, G):
    nc, S = P.nc, P.S
    D_ = P.dram
    PC, b_PC = G["PC"], G["b_PC"]
    identf, b_idf = G["identf"], G["b_idf"]
    yyT, b_yyT = G["yyT"], G["b_yyT"]
    kspec = D_["kspec"]; bk = P.dbufs["kspec"]
    with ExitStack() as st:
        zt = P.sb("d_zt", [128, NT, 512], BF16, st); b_zt = Buf()
        S.dma("sp", lambda e: e.dma_start(out=zt[:], in_=D_["ztd"].ap()), reads=[P.dbufs["ztd"]], writes=[b_zt])
        Yre = P.sb("d_yre", [128, NT, 512], BF16, st); b_yre = Buf()
        Yim = P.sb("d_yim", [128, NT, 512], BF16, st); b_yim = Buf()
        tabc = [P.sb("d_tabc%d" % i, [128, NT * 128], BF16, st) for i in range(2)]
        tabs = [P.sb("d_tabs%d" % i, [128, NT * 128], BF16, st) for i in range(2)]
        b_tabc = [Buf(), Buf()]; b_tabs = [Buf(), Buf()]
        ksp = [P.sb("d_ksp%d" % i, [128, 2, 512], F32, st) for i in range(2)]; b_ksp = [Buf(), Buf()]
        t1 = P.sb("d_t1", [128, 512], F32, st); b_t1 = Buf()
        t2 = P.sb("d_t2", [128, 512], F32, st); b_t2 = Buf()
        pa = [P.ps("d_pa%d" % i, [128, 512], F32, st) for i in range(2)]; b_pa = [Buf(), Buf()]
        pb = [P.ps("d_pb%d" % i, [128, 512], F32, st) for i in range(2)]; b_pb = [Buf(), Buf()]
        ptf = [P.ps("d_ptf%d" % i, [128, 4, 128], F32, st) for i in range(2)]; b_ptf = [Buf(), Buf()]
        ysb = [P.sb("d_ysb%d" % i, [128, 512], F32, st) for i in range(2)]; b_ysb = [Buf(), Buf()]
        zx = [P.sb("d_zx%d" % i, [128, 2, 4, 128], F32, st) for i in range(2)]; b_zx = [Buf(), Buf()]

        def load_tabs(j, k):
            S.dma("sp", lambda e: e.dma_start(out=tabc[k][:], in_=D_["c_tabC"].ap()[j]), writes=[b_tabc[k]])
            S.dma("sp", lambda e: e.dma_start(out=tabs[k][:], in_=D_["c_tabS"].ap()[j]), writes=[b_tabs[k]])

        for j in range(NT):
            k = j % 2
            load_tabs(j, k)
            S.dma("sp", lambda e, j=j, k=k: e.dma_start(out=ksp[k][:], in_=kspec.ap()[j]), reads=[bk], writes=[b_ksp[k]])
            for (tab, b_tab, ps_, b_ps) in ((tabc, b_tabc, pa, b_pa), (tabs, b_tabs, pb, b_pb)):
                for i in range(NT):
                    r = _rows(i)
                    S.op("pe", lambda e, i=i, r=r, k=k, tab=tab, ps_=ps_: e.matmul(ps_[k][:], lhsT=tab[k][0:r, i * 128:(i + 1) * 128],
                                                                                  rhs=zt[0:r, i, :], start=(i == 0), stop=(i == NT - 1)),
                         reads=[b_tab[k], b_zt], writes=[b_ps[k]])
            S.op("dve", lambda e, k=k: e.tensor_tensor(out=t1[:], in0=pa[k][:], in1=ksp[k][:, 0, :], op=ALU.mult), reads=[b_pa[k], b_ksp[k]], writes=[b_t1])
            S.op("dve", lambda e, k=k: e.tensor_tensor(out=t2[:], in0=pb[k][:], in1=ksp[k][:, 1, :], op=ALU.mult), reads=[b_pb[k], b_ksp[k]], writes=[b_t2])
            S.op("dve", lambda e, j=j: e.tensor_tensor(out=Yre[:, j, :], in0=t1[:], in1=t2[:], op=ALU.subtract), reads=[b_t1, b_t2], writes=[b_yre])
            S.op("dve", lambda e, k=k: e.tensor_tensor(out=t1[:], in0=pb[k][:], in1=ksp[k][:, 0, :], op=ALU.mult), reads=[b_pb[k], b_ksp[k]], writes=[b_t1])
            S.op("dve", lambda e, k=k: e.tensor_tensor(out=t2[:], in0=pa[k][:], in1=ksp[k][:, 1, :], op=ALU.mult), reads=[b_pa[k], b_ksp[k]], writes=[b_t2])
            S.op("dve", lambda e, j=j: e.tensor_tensor(out=Yim[:, j, :], in0=t1[:], in1=t2[:], op=ALU.add), reads=[b_t1, b_t2], writes=[b_yim])

        rf = NF - 128 * (NT - 1)
        skip_b = PC[:, 48:52].unsqueeze(2).to_broadcast([128, 4, 128])
        zTd = D_["zT"].ap(); x0d = D_["x0cT"].ap()
        for j in range(NT):
            k = j % 2
            r = _rows(j)
            load_tabs(j, k)
            S.dma("sp", lambda e, j=j, k=k, r=r: e.dma_start(out=zx[k][:, 0, :, 0:r], in_=zTd[:, :, 128 * j:128 * j + r].rearrange("i p t -> p i t")),
                  reads=[P.dbufs["zT"]], writes=[b_zx[k]])
            S.dma("sp", lambda e, j=j, k=k, r=r: e.dma_start(out=zx[k][:, 1, :, 0:r], in_=x0d[:, :, 128 * j:128 * j + r].rearrange("i p t -> p i t")),
                  reads=[P.dbufs["x0cT"]], writes=[b_zx[k]])
            n_mm = 2 * NT
            c = 0
            for (tab, b_tab, Y, b_Y) in ((tabc, b_tabc, Yre, b_yre), (tabs, b_tabs, Yim, b_yim)):
                for i in range(NT):
                    rk = 128 if i < NT - 1 else rf
                    S.op("pe", lambda e, i=i, rk=rk, k=k, tab=tab, Y=Y, c=c: e.matmul(pa[k][:], lhsT=tab[k][0:rk, i * 128:(i + 1) * 128],
                                                                                     rhs=Y[0:rk, i, :], start=(c == 0), stop=(c == n_mm - 1)),
                         reads=[b_tab[k], b_Y], writes=[b_pa[k]])
                    c += 1
            S.op("act", lambda e, k=k: e.copy(out=ysb[k][:], in_=pa[k][:]), reads=[b_pa[k]], writes=[b_ysb[k]])
            for i in range(4):
                S.op("pe", lambda e, i=i, k=k, r=r: e.transpose(ptf[k][:, i, 0:r], ysb[k][0:r, i * 128:(i + 1) * 128], identf[0:r, 0:r]),
                     reads=[b_ysb[k], b_idf], writes=[b_ptf[k]])
            S.op("dve", lambda e, k=k, r=r: e.tensor_tensor(out=zx[k][:, 0, :, 0:r], in0=zx[k][:, 0, :, 0:r], in1=skip_b[:, :, 0:r], op=ALU.mult),
                 reads=[b_zx[k], b_PC], writes=[b_zx[k]])
            S.op("dve", lambda e, k=k, r=r: e.tensor_tensor(out=zx[k][:, 0, :, 0:r], in0=zx[k][:, 0, :, 0:r], in1=ptf[k][:, :, 0:r], op=ALU.add),
                 reads=[b_zx[k], b_ptf[k]], writes=[b_zx[k]])
            S.op("dve", lambda e, k=k, r=r, j=j: e.tensor_tensor(out=yyT[:, :, 128 * j:128 * j + r], in0=zx[k][:, 0, :, 0:r], in1=zx[k][:, 1, :, 0:r],
                                                                op=ALU.mult), reads=[b_zx[k]], writes=[b_yyT])
        S.barrier()


def phase_merge(P, inp, G):
    nc, S = P.nc, P.S
    D_ = P.dram
    yyT, b_yyT = G["yyT"], G["b_yyT"]
    mgT = P.dscr("mgT", [8, 128, L], BF16)
    hy_out = inp["hy_out"].ap()[0].rearrange("(k p) c -> p k c", p=128)
    rg_out = inp["rg_out"].ap()[0].rearrange("(k p) c -> p k c", p=128)
    with ExitStack() as st:
        yrg = P.sb("g_yrg", [128, 8, L], BF16, st); b_yrg = Buf()
        hwss = [P.sb("g_hws%d" % i, [128, 4, 128], F32, st) for i in range(2)]; b_hwss = [Buf(), Buf()]
        rwss = [P.sb("g_rws%d" % i, [128, 8, 128], F32, st) for i in range(2)]; b_rwss = [Buf(), Buf()]
        hwb = [P.sb("g_hwb%d" % i, [128, 4, 128], BF16, st) for i in range(2)]; b_hwb = [Buf(), Buf()]
        rwb = [P.sb("g_rwb%d" % i, [128, 8, 128], BF16, st) for i in range(2)]; b_rwb = [Buf(), Buf()]
        sga = [P.sb("g_sga%d" % i, [128, L], BF16, st) for i in range(2)]; b_sga = [Buf(), Buf()]
        sgb = [P.sb("g_sgb%d" % i, [128, L], BF16, st) for i in range(2)]; b_sgb = [Buf(), Buf()]
        mg = [P.sb("g_mg%d" % i, [128, L], BF16, st) for i in range(2)]; b_mg = [Buf(), Buf()]
        t1 = P.sb("g_t1", [128, 512], F32, st); b_t1 = Buf()
        t2 = P.sb("g_t2", [128, 512], F32, st); b_t2 = Buf()
        ph = [P.ps("g_ph%d" % i, [128, 512], F32, st) for i in range(2)]; b_ph = [Buf(), Buf()]
        pr = [P.ps("g_pr%d" % i, [128, 512], F32, st) for i in range(2)]; b_pr = [Buf(), Buf()]
        for h in range(8):
            S.dma("sp", lambda e, h=h: e.dma_start(out=yrg[:, h, :], in_=D_["yrgT"].ap()[h]), reads=[P.dbufs["yrgT"]], writes=[b_yrg])
        c = 0
        for m in range(8):
            k = m % 2
            S.dma("sp", lambda e, m=m, k=k: e.dma_start(out=hwss[k][:], in_=hy_out[:, :, m * 128:(m + 1) * 128]), writes=[b_hwss[k]])
            S.dma("sp", lambda e, m=m, k=k: e.dma_start(out=rwss[k][:], in_=rg_out[:, :, m * 128:(m + 1) * 128]), writes=[b_rwss[k]])
            S.op("pool", lambda e, k=k: e.tensor_copy(out=hwb[k][:], in_=hwss[k][:]), reads=[b_hwss[k]], writes=[b_hwb[k]])
            S.op("pool", lambda e, k=k: e.tensor_copy(out=rwb[k][:], in_=rwss[k][:]), reads=[b_rwss[k]], writes=[b_rwb[k]])
            S.dma("sp", lambda e, m=m, k=k: e.dma_start(out=sga[k][:], in_=D_["sgT"].ap()[m]), reads=[P.dbufs["sgT"]], writes=[b_sga[k]])
            S.dma("sp", lambda e, m=m, k=k: e.dma_start(out=sgb[k][:], in_=D_["sgT"].ap()[8 + m]), reads=[P.dbufs["sgT"]], writes=[b_sgb[k]])
            for n, (c0, cw) in enumerate(NCH):
                q = c % 2; c += 1
                for i in range(4):
                    S.op("pe", lambda e, i=i, q=q, k=k, c0=c0, cw=cw: e.matmul(ph[q][:, 0:cw], lhsT=hwb[k][:, i, :], rhs=yyT[:, i, c0:c0 + cw],
                                                                                start=(i == 0), stop=(i == 3)),
                         reads=[b_hwb[k], b_yyT], writes=[b_ph[q]])
                for i in range(8):
                    S.op("pe", lambda e, i=i, q=q, k=k, c0=c0, cw=cw: e.matmul(pr[q][:, 0:cw], lhsT=rwb[k][:, i, :], rhs=yrg[:, i, c0:c0 + cw],
                                                                                start=(i == 0), stop=(i == 7)),
                         reads=[b_rwb[k], b_yrg], writes=[b_pr[q]])
                S.op("dve", lambda e, q=q, k=k, c0=c0, cw=cw: e.tensor_tensor(out=t1[:, 0:cw], in0=ph[q][:, 0:cw], in1=sga[k][:, c0:c0 + cw], op=ALU.mult),
                     reads=[b_ph[q], b_sga[k]], writes=[b_t1])
                S.op("dve", lambda e, q=q, k=k, c0=c0, cw=cw: e.tensor_tensor(out=t2[:, 0:cw], in0=pr[q][:, 0:cw], in1=sgb[k][:, c0:c0 + cw], op=ALU.mult),
                     reads=[b_pr[q], b_sgb[k]], writes=[b_t2])
                S.op("dve", lambda e, k=k, c0=c0, cw=cw: e.tensor_tensor(out=mg[k][:, c0:c0 + cw], in0=t1[:, 0:cw], in1=t2[:, 0:cw], op=ALU.add),
                     reads=[b_t1, b_t2], writes=[b_mg[k]])
            S.dma("sp", lambda e, m=m, k=k: e.dma_start(out=mgT.ap()[m], in_=mg[k][:]), reads=[b_mg[k]], writes=[P.dbufs["mgT"]])
        S.barrier()


def phase_post(P, inp, G):
    nc, S = P.nc, P.S
    D_ = P.dram
    identb, b_idb = G["identb"], G["b_idb"]
    n2Td = P.dscr("n2Td", [128, 8, SEQ], BF16)
    h2d = P.dscr("h2d", [SEQ, D], F32)
    n2d = P.dscr("n2d", [SEQ, D], F32)
    x = inp["x"].ap()
    w_out = inp["w_out"].ap()[0].rearrange("(k p) c -> p k c", p=128)
    with ExitStack() as st:
        mg = P.sb("p_mg", [128, 8, L], BF16, st); b_mg = Buf()
        wos = P.sb("p_wos", [128, D], F32, st); b_wos = Buf()
        wob = P.sb("p_wob", [128, 8, D], BF16, st); b_wob = Buf()
        gf = P.sb("p_gf", [128, D], F32, st); b_gf = Buf()
        xt = [P.sb("p_xt%d" % i, [128, D], F32, st) for i in range(2)]; b_xt = [Buf(), Buf()]
        h2 = [P.sb("p_h2%d" % i, [128, D], F32, st) for i in range(2)]; b_h2 = [Buf(), Buf()]
        n2 = [P.sb("p_n2%d" % i, [128, D], F32, st) for i in range(2)]; b_n2 = [Buf(), Buf()]
        n2b = [P.sb("p_n2b%d" % i, [128, D], BF16, st) for i in range(2)]; b_n2b = [Buf(), Buf()]
        sq = P.sb("p_sq", [128, D], F32, st); b_sq = Buf()
        ssq = P.sb("p_ss", [128, 64], F32, st); b_ss = Buf()
        pm = [P.ps("p_pm%d" % i, [128, 2, 512], F32, st) for i in range(2)]; b_pm = [Buf(), Buf()]
        pt = [P.ps("p_pt%d" % i, [128, 8, 128], BF16, st) for i in range(2)]; b_pt = [Buf(), Buf()]
        n2Tt = [P.sb("p_n2Tt%d" % i, [128, 8, 128], BF16, st) for i in range(2)]; b_n2Tt = [Buf(), Buf()]
        for kk in range(8):
            S.dma("sp", lambda e, kk=kk: e.dma_start(out=mg[:, kk, :], in_=D_["mgT"].ap()[kk]), reads=[P.dbufs["mgT"]], writes=[b_mg])
            S.dma("sp", lambda e, kk=kk: e.dma_start(out=wos[:], in_=w_out[:, kk, :]), writes=[b_wos])
            S.op("pool", lambda e, kk=kk: e.tensor_copy(out=wob[:, kk, :], in_=wos[:]), reads=[b_wos], writes=[b_wob])
        S.dma("sp", lambda e: e.dma_start(out=gf[:], in_=inp["g_ffn"].ap().partition_broadcast(128)), writes=[b_gf])
        for j in range(32):
            k = j % 2
            p0 = NMETA + 128 * j
            S.dma("sp", lambda e, j=j, k=k: e.dma_start(out=xt[k][:], in_=x[128 * j:128 * (j + 1), :]), writes=[b_xt[k]])
            for half in range(2):
                for kk in range(8):
                    S.op("pe", lambda e, kk=kk, half=half, k=k, p0=p0: e.matmul(pm[k][:, half, :], lhsT=mg[:, kk, p0:p0 + 128],
                                                                                 rhs=wob[:, kk, half * 512:(half + 1) * 512],
                                                                                 start=(kk == 0), stop=(kk == 7)),
                         reads=[b_mg, b_wob], writes=[b_pm[k]])
            S.op("dve", lambda e, k=k: e.tensor_tensor(out=h2[k][:], in0=xt[k][:], in1=pm[k][:].rearrange("p a c -> p (a c)"), op=ALU.add),
                 reads=[b_xt[k], b_pm[k]], writes=[b_h2[k]])
            S.dma("sp", lambda e, j=j, k=k: e.dma_start(out=h2d.ap()[128 * j:128 * (j + 1), :], in_=h2[k][:]), reads=[b_h2[k]], writes=[P.dbufs["h2d"]])
            S.op("act", lambda e, k=k, j=j: e.activation(out=sq[:], in_=h2[k][:], func=AF.Square, accum_out=ssq[:, 2 * j:2 * j + 1]),
                 reads=[b_h2[k]], writes=[b_sq, b_ss])
            S.op("act", lambda e, j=j: e.activation(out=ssq[:, 2 * j + 1:2 * j + 2], in_=ssq[:, 2 * j:2 * j + 1], func=AF.Sqrt, scale=1.0 / D, bias=EPS),
                 reads=[b_ss], writes=[b_ss])
            S.op("dve", lambda e, j=j: e.reciprocal(out=ssq[:, 2 * j + 1:2 * j + 2], in_=ssq[:, 2 * j + 1:2 * j + 2]), reads=[b_ss], writes=[b_ss])
            S.op("dve", lambda e, k=k, j=j: e.scalar_tensor_tensor(out=n2[k][:], in0=h2[k][:], scalar=ssq[:, 2 * j + 1:2 * j + 2], in1=gf[:],
                                                                   op0=ALU.mult, op1=ALU.mult), reads=[b_h2[k], b_ss, b_gf], writes=[b_n2[k]])
            S.dma("sp", lambda e, j=j, k=k: e.dma_start(out=n2d.ap()[128 * j:128 * (j + 1), :], in_=n2[k][:]), reads=[b_n2[k]], writes=[P.dbufs["n2d"]])
            S.op("act", lambda e, k=k: e.copy(out=n2b[k][:], in_=n2[k][:]), reads=[b_n2[k]], writes=[b_n2b[k]])
            for c in range(8):
                S.op("pe", lambda e, k=k, c=c: e.transpose(pt[k][:, c, :], n2b[k][:, c * 128:(c + 1) * 128], identb[:]),
                     reads=[b_n2b[k], b_idb], writes=[b_pt[k]])
            S.op("act", lambda e, k=k: e.copy(out=n2Tt[k][:], in_=pt[k][:]), reads=[b_pt[k]], writes=[b_n2Tt[k]])
            S.dma("sp", lambda e, k=k, j=j: e.dma_start(out=n2Td.ap()[:, :, 128 * j:128 * (j + 1)], in_=n2Tt[k][:]), reads=[b_n2Tt[k]],
                  writes=[P.dbufs["n2Td"]])
        S.barrier()


def phase_peer_scores(P, inp, G):
    nc, S = P.nc, P.S
    identb, b_idb = G["identb"], G["b_idb"]
    TS, b_TS, TIf, b_TIf = G["TS"], G["b_TS"], G["TIf"], G["b_TIf"]
    wq = inp["peer_wq"].ap()[0].rearrange("(k p) c -> p k c", p=128)
    keys = inp["peer_keys"].ap()[0]
    with ExitStack() as st:
        n2T = P.sb("s_n2T", [128, 8, SEQ], BF16, st); b_n2T = Buf()
        for kk in range(8):
            S.dma("sp", lambda e, kk=kk: e.dma_start(out=n2T[:, kk, :], in_=P.dram["n2Td"].ap()[:, kk, :]), reads=[P.dbufs["n2Td"]], writes=[b_n2T])
        TIu = P.sb("s_tiu", [128, 32, 16, 16], U32, st); b_TIu = Buf()
        wqss = [P.sb("s_wqs%d" % i, [128, 8, 128], F32, st) for i in range(2)]; b_wqss = [Buf(), Buf()]
        wqb = [P.sb("s_wqb%d" % i, [128, 8, 128], BF16, st) for i in range(2)]; b_wqb = [Buf(), Buf()]
        kys = P.sb("s_kys", [128, 128], F32, st); b_kys = Buf()
        kyb = P.sb("s_kyb", [128, 128], BF16, st); b_kyb = Buf()
        kyT = [P.sb("s_kyT%d" % i, [128, 128], BF16, st) for i in range(2)]; b_kyT = [Buf(), Buf()]
        qTb = [P.sb("s_qT%d" % i, [128, SEQ], BF16, st) for i in range(2)]; b_qT = [Buf(), Buf()]
        sc = [P.sb("s_sc%d" % i, [128, 4, 128], F32, st) for i in range(2)]; b_sc = [Buf(), Buf()]
        sc2 = [P.sb("s_sc2_%d" % i, [128, 128], F32, st) for i in range(2)]; b_sc2 = [Buf(), Buf()]
        b_TSx = [Buf(), Buf()]; b_TIx = [Buf(), Buf()]
        pq = [P.ps("s_pq%d" % i, [128, 512], F32, st) for i in range(2)]; b_pq = [Buf(), Buf()]
        psc = [P.ps("s_ps%d" % i, [128, 4, 128], F32, st) for i in range(2)]; b_psc = [Buf(), Buf()]
        pkt = P.ps("s_pkt", [128, 128], BF16, st); b_pkt = Buf()
        c = 0; c2 = 0
        for qc in range(16):
            k = qc % 2
            S.dma("sp", lambda e, qc=qc, k=k: e.dma_start(out=wqss[k][:], in_=wq[:, :, qc * 128:(qc + 1) * 128]), writes=[b_wqss[k]])
            S.op("pool", lambda e, k=k: e.tensor_copy(out=wqb[k][:], in_=wqss[k][:]), reads=[b_wqss[k]], writes=[b_wqb[k]])
            S.dma("sp", lambda e, qc=qc: e.dma_start(out=kys[:], in_=keys[qc // 2, qc % 2]), writes=[b_kys])
            S.op("pool", lambda e: e.tensor_copy(out=kyb[:], in_=kys[:]), reads=[b_kys], writes=[b_kyb])
            S.op("pe", lambda e: e.transpose(pkt[:], kyb[:], identb[:]), reads=[b_kyb, b_idb], writes=[b_pkt])
            S.op("act", lambda e, k=k: e.copy(out=kyT[k][:], in_=pkt[:]), reads=[b_pkt], writes=[b_kyT[k]])
            for n in range(8):
                q = c % 2; c += 1
                for kk in range(8):
                    S.op("pe", lambda e, kk=kk, q=q, k=k, n=n: e.matmul(pq[q][:], lhsT=wqb[k][:, kk, :], rhs=n2T[:, kk, n * 512:(n + 1) * 512],
                                                                        start=(kk == 0), stop=(kk == 7)),
                         reads=[b_wqb[k], b_n2T], writes=[b_pq[q]])
                S.op("act", lambda e, q=q, k=k, n=n: e.copy(out=qTb[k][:, n * 512:(n + 1) * 512], in_=pq[q][:]), reads=[b_pq[q]], writes=[b_qT[k]])
            for jg in range(8):
                q = c2 % 2; c2 += 1
                for jj in range(4):
                    j = jg * 4 + jj
                    S.op("pe", lambda e, q=q, k=k, j=j, jj=jj: e.matmul(psc[q][:, jj, :], lhsT=qTb[k][:, 128 * j:128 * (j + 1)], rhs=kyT[k][:],
                                                                        start=True, stop=True),
                         reads=[b_qT[k], b_kyT[k]], writes=[b_psc[q]])
                S.op("act", lambda e, q=q: e.copy(out=sc[q][:], in_=psc[q][:]), reads=[b_psc[q]], writes=[b_sc[q]])
                for jp in range(0, 4, 2):
                    pair = [(jg * 4 + jp + u, jp + u, u) for u in range(2)]
                    for (j, jj, u) in pair:
                        S.op("dve", lambda e, q=q, j=j, jj=jj, qc=qc: e.max(out=TS[:, j, qc, 0:8], in_=sc[q][:, jj, :]), reads=[b_sc[q]], writes=[b_TSx[u]])
                    for (j, jj, u) in pair:
                        S.op("dve", lambda e, q=q, j=j, jj=jj, qc=qc: e.max_index(out=TIu[:, j, qc, 0:8], in_max=TS[:, j, qc, 0:8], in_values=sc[q][:, jj, :]),
                             reads=[b_sc[q], b_TSx[u]], writes=[b_TIx[u]])
                    for (j, jj, u) in pair:
                        S.op("dve", lambda e, q=q, j=j, jj=jj, qc=qc, u=u: e.match_replace(out=sc2[u][:], in_to_replace=TS[:, j, qc, 0:8], in_values=sc[q][:, jj, :],
                                                                                      imm_value=-1e30), reads=[b_sc[q], b_TSx[u]], writes=[b_sc2[u]])
                    for (j, jj, u) in pair:
                        S.op("dve", lambda e, j=j, qc=qc, u=u: e.max(out=TS[:, j, qc, 8:16], in_=sc2[u][:]), reads=[b_sc2[u]], writes=[b_TSx[u]])
                    for (j, jj, u) in pair:
                        S.op("dve", lambda e, j=j, qc=qc, u=u: e.max_index(out=TIu[:, j, qc, 8:16], in_max=TS[:, j, qc, 8:16], in_values=sc2[u][:]),
                             reads=[b_sc2[u], b_TSx[u]], writes=[b_TIx[u]])
        for u in range(2):
            S.op("dve", lambda e: e.engine_nop(), reads=[b_TSx[u], b_TIx[u]], writes=[b_TS, b_TIu])
        S.op("dve", lambda e: e.tensor_copy(out=TIf[:].rearrange("p a b c -> p (a b c)"), in_=TIu[:].rearrange("p a b c -> p (a b c)")),
             reads=[b_TIu], writes=[b_TIf])
        S.barrier()


def phase_peer_out(P, inp, G, out):
    nc, S = P.nc, P.S
    D_ = P.dram
    TS, b_TS, TIf, b_TIf = G["TS"], G["b_TS"], G["TIf"], G["b_TIf"]
    utab = inp["peer_u"].ap()[0]
    vtab = inp["peer_v"].ap()[0]
    h2d = D_["h2d"].ap(); n2d = D_["n2d"].ap()
    NEG = -1e30
    with ExitStack() as st:
        iota4 = P.sb("o_iota", [128, 8, 16, 16], F32, st); b_iota = Buf()
        io1 = P.sb("o_io1", [128, 16], I32, st); b_io1 = Buf()
        gfin = P.sb("o_gfin", [128, D], F32, st); b_gfin = Buf()
        cand = P.sb("o_cand", [128, 8, 256], F32, st); b_cand = Buf()
        cand2 = P.sb("o_cand2", [128, 256], F32, st); b_cand2 = Buf()
        cs = P.sb("o_cs", [128, 8, 16], F32, st); b_cs = Buf()
        ci = P.sb("o_ci", [128, 8, 16], U32, st); b_ci = Buf()
        ik = P.sb("o_ik", [128, 8, 16], U32, st); b_ik = Buf()
        jk = P.sb("o_jk", [128, 8, 16], U32, st); b_jk = Buf()
        ikf = P.sb("o_ikf", [128, 8, 16], F32, st); b_ikf = Buf()
        jkf = P.sb("o_jkf", [128, 8, 16], F32, st); b_jkf = Buf()
        oh = P.sb("o_oh", [128, 8, 16, 16], F32, st); b_oh = Buf()
        e1 = P.sb("o_e1", [128, 8, 16], F32, st); b_e1 = Buf()
        e2 = P.sb("o_e2", [128, 8, 16], F32, st); b_e2 = Buf()
        EI = [P.sb("o_ei%d" % i, [128, 128], U32, st) for i in range(2)]; b_EI = [Buf(), Buf()]
        gt = [P.sb("o_g%d" % i, [128, 8, 16], F32, st) for i in range(2)]; b_gt = [Buf(), Buf()]
        sm = P.sb("o_sm", [128, 16], F32, st); b_sm = Buf()
        scr = P.sb("o_scr", [128, 128], F32, st); b_scrc = [Buf() for _ in range(128)]
        act = P.sb("o_act", [128, 128], F32, st); b_actc = [Buf() for _ in range(128)]
        agc = P.sb("o_agc", [128, 128], F32, st); b_agc = [Buf() for _ in range(128)]
        NG = 16
        ug = [P.sb("o_ug%d" % i, [128, 2 * D], BF16, st) for i in range(NG)]; b_ug = [Buf() for _ in range(NG)]
        ND = 8
        dg = [P.sb("o_dg%d" % i, [128, 128], BF16, st) for i in range(ND)]; b_dg = [Buf() for _ in range(ND)]
        pacc = [P.ps("o_pacc%d" % i, [128, 2, 512], F32, st) for i in range(2)]; b_pacc = [Buf(), Buf()]
        identf, b_idf = G["identf"], G["b_idf"]
        uvt = D_["uvtab"].ap()
        junk = P.sb("o_junk", [128, D], F32, st); b_junk = Buf()
        djunk = P.ps("o_djunk", [128, D], F32, st)
        n2t = [P.sb("o_n2%d" % i, [128, D], F32, st) for i in range(2)]; b_n2t = [Buf(), Buf()]
        h2t = [P.sb("o_h2", [128, D], F32, st)] * 2; b_h2t = [Buf()] * 2
        acc = P.sb("o_acc", [128, D], F32, st); b_acc = Buf()
        ssq = P.sb("o_ss", [128, 64], F32, st); b_ss = Buf()

        S.op("pool", lambda e: e.iota(io1[:], pattern=[[1, 16]], base=0, channel_multiplier=0), writes=[b_io1])
        S.op("dve", lambda e: e.tensor_copy(out=iota4[:].rearrange("p a b c -> p (a b) c"),
                                            in_=io1[:].unsqueeze(1).to_broadcast([128, 128, 16])), reads=[b_io1], writes=[b_iota])
        S.dma("sp", lambda e: e.dma_start(out=gfin[:], in_=inp["g_final"].ap().partition_broadcast(128)), writes=[b_gfin])
        gcount = 0

        pend = []

        def QO(*a, **kw):
            pend.append((S.op, a, kw))

        def QD(*a, **kw):
            pend.append((S.dma, a, kw))

        def drain(n):
            for _ in range(min(n, len(pend))):
                f, a, kw = pend.pop(0)
                f(*a, **kw)

        def select(j):
            k = j % 2
            QD("sp", lambda e, j=j, k=k: e.dma_start(out=n2t[k][:], in_=n2d[128 * j:128 * (j + 1), :]), reads=[P.dbufs["n2d"]], writes=[b_n2t[k]])
            tsj = TS[:, j].rearrange("p (h two) i -> p h two i", two=2)
            tij = TIf[:, j].rearrange("p (h two) i -> p h two i", two=2)
            QO("dve", lambda e, tsj=tsj: e.tensor_tensor(out=cand[:].rearrange("p h (a b) -> p h a b", a=16),
                                                           in0=tsj[:, :, 0, :].unsqueeze(3).to_broadcast([128, 8, 16, 16]),
                                                           in1=tsj[:, :, 1, :].unsqueeze(2).to_broadcast([128, 8, 16, 16]), op=ALU.add),
                 reads=[b_TS], writes=[b_cand])
            for h in range(8):
                QO("dve", lambda e, h=h: e.max(out=cs[:, h, 0:8], in_=cand[:, h, :]), reads=[b_cand], writes=[b_cs])
                QO("dve", lambda e, h=h: e.max_index(out=ci[:, h, 0:8], in_max=cs[:, h, 0:8], in_values=cand[:, h, :]), reads=[b_cand, b_cs], writes=[b_ci])
                QO("dve", lambda e, h=h: e.match_replace(out=cand2[:], in_to_replace=cs[:, h, 0:8], in_values=cand[:, h, :], imm_value=NEG),
                     reads=[b_cand, b_cs], writes=[b_cand2])
                QO("dve", lambda e, h=h: e.max(out=cs[:, h, 8:16], in_=cand2[:]), reads=[b_cand2], writes=[b_cs])
                QO("dve", lambda e, h=h: e.max_index(out=ci[:, h, 8:16], in_max=cs[:, h, 8:16], in_values=cand2[:]), reads=[b_cand2, b_cs], writes=[b_ci])
            QO("dve", lambda e: e.tensor_single_scalar(out=ik[:], in_=ci[:], scalar=4, op=ALU.logical_shift_right), reads=[b_ci], writes=[b_ik])
            QO("dve", lambda e: e.tensor_single_scalar(out=jk[:], in_=ci[:], scalar=15, op=ALU.bitwise_and), reads=[b_ci], writes=[b_jk])
            QO("dve", lambda e: e.tensor_copy(out=ikf[:], in_=ik[:]), reads=[b_ik], writes=[b_ikf])
            QO("dve", lambda e: e.tensor_copy(out=jkf[:], in_=jk[:]), reads=[b_jk], writes=[b_jkf])
            for (kf_, b_kf_, half, dst, b_dst) in ((ikf, b_ikf, 0, e1, b_e1), (jkf, b_jkf, 1, e2, b_e2)):
                QO("dve", lambda e, kf_=kf_: e.tensor_tensor(out=oh[:], in0=iota4[:], in1=kf_[:].unsqueeze(3).to_broadcast([128, 8, 16, 16]),
                                                               op=ALU.is_equal), reads=[b_iota, b_kf_], writes=[b_oh])
                QO("dve", lambda e, half=half, tij=tij: e.tensor_tensor(out=oh[:], in0=oh[:],
                                                                          in1=tij[:, :, half, :].unsqueeze(2).to_broadcast([128, 8, 16, 16]), op=ALU.mult),
                     reads=[b_oh, b_TIf], writes=[b_oh])
                QO("dve", lambda e, dst=dst: e.tensor_reduce(out=dst[:], in_=oh[:], axis=AX.X, op=ALU.add), reads=[b_oh], writes=[b_dst])
            QO("dve", lambda e: e.scalar_tensor_tensor(out=e1[:], in0=e1[:], scalar=128.0, in1=e2[:], op0=ALU.mult, op1=ALU.add),
                 reads=[b_e1, b_e2], writes=[b_e1])
            QO("dve", lambda e, k=k: e.tensor_copy(out=EI[k][:], in_=e1[:].rearrange("p h k -> p (h k)")), reads=[b_e1], writes=[b_EI[k]])
            QO("dve", lambda e, k=k: e.tensor_tensor(out=gt[k][:], in0=cs[:], in1=cs[:, :, 0:1].to_broadcast([128, 8, 16]), op=ALU.subtract),
                 reads=[b_cs], writes=[b_gt[k]])
            QO("act", lambda e, k=k: e.activation(out=gt[k][:], in_=gt[k][:], func=AF.Exp), reads=[b_gt[k]], writes=[b_gt[k]])
            QO("dve", lambda e, k=k: e.tensor_reduce(out=sm[:, 0:8], in_=gt[k][:], axis=AX.X, op=ALU.add), reads=[b_gt[k]], writes=[b_sm])
            QO("dve", lambda e: e.reciprocal(out=sm[:, 8:16], in_=sm[:, 0:8]), reads=[b_sm], writes=[b_sm])
            QO("dve", lambda e, k=k: e.tensor_tensor(out=gt[k][:], in0=gt[k][:], in1=sm[:, 8:16].unsqueeze(2).to_broadcast([128, 8, 16]), op=ALU.mult),
                 reads=[b_gt[k], b_sm], writes=[b_gt[k]])

        pool_aligned = (len(S.dma_pool["pool"]) == NG)
        S.dma_next["pool"] = 0
        select(0)
        drain(len(pend))
        for j in range(32):
            k = j % 2
            if j + 1 < 32:
                select(j + 1)
            per_hk = (len(pend) + 95) // 96
            S.dma("sp", lambda e, j=j, k=k: e.dma_start(out=h2t[k][:], in_=h2d[128 * j:128 * (j + 1), :]), reads=[P.dbufs["h2d"]], writes=[b_h2t[k]])
            gflat = gt[k][:].rearrange("p h k -> p (h k)")
            for hk in range(128):
                drain(per_hk)
                g = gcount % NG; dsl = gcount % ND; gcount += 1
                S.dma("pool", lambda e, g=g, k=k, hk=hk: e.indirect_dma_start(out=ug[g][:], out_offset=None, in_=uvt,
                                                                              in_offset=IndirectOffsetOnAxis(ap=EI[k][:, hk:hk + 1], axis=0)),
                      reads=[b_EI[k], P.dbufs["uvtab"]], writes=[b_ug[g]], slot_covered=(gcount > NG and pool_aligned))
                S.op("dve", lambda e, g=g, k=k, hk=hk: e.scalar_tensor_tensor(out=djunk[:], in0=ug[g][:, 0:D], scalar=1.0, in1=n2t[k][:], op0=ALU.mult,
                                                                              op1=ALU.mult, accum_out=scr[:, hk:hk + 1]),
                     reads=[b_n2t[k]], weak_reads=[b_ug[g]], writes=[b_scrc[hk]])
                S.op("act", lambda e, hk=hk: e.activation(out=act[:, hk:hk + 1], in_=scr[:, hk:hk + 1], func=AF.Gelu_apprx_tanh),
                     reads=[b_scrc[hk]], writes=[b_actc[hk]])
                S.op("act", lambda e, hk=hk, gflat=gflat: e.activation(out=agc[:, hk:hk + 1], in_=act[:, hk:hk + 1], func=AF.Identity,
                                                                      scale=gflat[:, hk:hk + 1]),
                     reads=[b_actc[hk], b_gt[k]], writes=[b_agc[hk]])
                S.op("act", lambda e, hk=hk, dsl=dsl: e.activation(out=dg[dsl][:], in_=identf[:], func=AF.Identity, scale=agc[:, hk:hk + 1]),
                     reads=[b_idf, b_agc[hk]], writes=[b_dg[dsl]])
                for half in range(2):
                    S.op("pe", lambda e, g=g, k=k, hk=hk, dsl=dsl, half=half: e.matmul(pacc[k][:, half, :], lhsT=dg[dsl][:],
                                                                                      rhs=ug[g][:, D + half * 512:D + (half + 1) * 512],
                                                                                      start=(hk == 0), stop=(hk == 127)),
                         reads=[b_dg[dsl], b_ug[g]], writes=[b_pacc[k]])
            drain(len(pend))
            S.op("dve", lambda e, k=k: e.tensor_tensor(out=acc[:], in0=h2t[k][:], in1=pacc[k][:].rearrange("p a c -> p (a c)"), op=ALU.add),
                 reads=[b_h2t[k], b_pacc[k]], writes=[b_acc])
            S.op("act", lambda e, j=j: e.activation(out=junk[:], in_=acc[:], func=AF.Square, accum_out=ssq[:, 2 * j:2 * j + 1]),
                 reads=[b_acc], writes=[b_junk, b_ss])
            S.op("act", lambda e, j=j: e.activation(out=ssq[:, 2 * j + 1:2 * j + 2], in_=ssq[:, 2 * j:2 * j + 1], func=AF.Sqrt, scale=1.0 / D, bias=EPS),
                 reads=[b_ss], writes=[b_ss])
            S.op("dve", lambda e, j=j: e.reciprocal(out=ssq[:, 2 * j + 1:2 * j + 2], in_=ssq[:, 2 * j + 1:2 * j + 2]), reads=[b_ss], writes=[b_ss])
            S.op("dve", lambda e, k=k, j=j: e.scalar_tensor_tensor(out=junk[:], in0=acc[:], scalar=ssq[:, 2 * j + 1:2 * j + 2], in1=gfin[:],
                                                                   op0=ALU.mult, op1=ALU.mult), reads=[b_acc, b_ss, b_gfin], writes=[b_junk])
            S.dma("sp", lambda e, j=j, k=k: e.dma_start(out=out.ap()[128 * j:128 * (j + 1), :], in_=junk[:]), reads=[b_junk], writes=[P.dbufs["out"]])
        S.barrier()


INPUT_SPECS = [
    ("x", [SEQ, D]), ("meta", [NMETA, D]), ("g_final", [1, D]), ("g_mix", [1, D]), ("w_in", [1, D, D_IN]),
    ("b_gate", [1, 2 * D]), ("hy_conv_w", [1, 3, 3 * D_HY]), ("hy_conv_b", [1, 3 * D_HY]), ("hy_w1", [1, 33, 64]),
    ("hy_b1", [1, 64]), ("hy_w2", [1, 64, 64]), ("hy_b2", [1, 64]), ("hy_w3", [1, 64, 1024]), ("hy_b3", [1, 1024]),
    ("hy_freq", [1, 2, 64]), ("hy_skip", [1, D_HY]), ("hy_out", [1, D_HY, D]), ("rg_conv_w", [1, 2, 4, D_RG]),
    ("rg_conv_b", [1, 2, D_RG]), ("rg_gate_w", [1, 2, 2, 8, 128, 128]), ("rg_gate_b", [1, 2, 2, D_RG]),
    ("rg_lambda", [1, 2, D_RG]), ("rg_out", [1, D_RG, D]), ("w_out", [1, D, D]), ("g_ffn", [1, D]),
    ("peer_wq", [1, D, 2048]), ("peer_keys", [1, 8, 2, 128, 128]), ("peer_u", [1, 16384, D]), ("peer_v", [1, 16384, D]),
]
CONST_SPECS = [
    ("c_zT", [33, L], F32), ("c_negdelta", [128, 4], F32), ("c_tlin", [1, L], F32), ("c_wf", [128, NT], F32),
    ("c_tabC", [NT, 128, NT * 128], BF16), ("c_tabS", [NT, 128, NT * 128], BF16),
    ("c_identf", [128, 128], F32), ("c_identb", [128, 128], BF16),
]


def _dump(P, src_ap_fn, nrows_tiles, out, width=D, dtype=F32):
    S = P.S
    with ExitStack() as st:
        t = P.sb("dbg_t", [128, width], dtype, st); bt = Buf()
        for j in range(nrows_tiles):
            S.dma("sp", lambda e, j=j: e.dma_start(out=t[:], in_=src_ap_fn(j)), reads=list(P.dbufs.values()), writes=[bt])
            S.dma("sp", lambda e, j=j: e.dma_start(out=out.ap()[128 * j:128 * (j + 1), 0:width], in_=t[:]), reads=[bt], writes=[P.dbufs["out"]])


def build(stage="all"):
    P = Prog()
    nc = P.nc
    inp = {}
    for name, shape in INPUT_SPECS:
        inp[name] = P.din(name, shape, F32)
    for name, shape, dt in CONST_SPECS:
        P.din(name, shape, dt)
    out = P.dout("out", [SEQ, D], F32)
    G = {}
    with P.stack:
        P.S = S = Sched(nc, P.stack)
        phase_filter(P, inp, uv_hook=lambda st: phase_uvtab(P, inp, st))
        phase_params(P, inp, G)
        with ExitStack() as s_nT:
            G["nT"] = P.sb("nT", [128, 8, LP], BF16, s_nT); G["b_nT"] = Buf()
            phase_norm(P, inp, G)
            phase_mixer_in(P, inp, G)
        if stage == "mixin":
            S.finish("sp"); S.emit(); return P
        with ExitStack() as s_yy:
            G["yyT"] = P.sb("yyT", [128, 4, LP], BF16, s_yy); G["b_yyT"] = Buf()
            phase_dft(P, inp, G)
            phase_merge(P, inp, G)
        with ExitStack() as s_ts:
            phase_post(P, inp, G)
            G["TS"] = P.sb("TS", [128, 32, 16, 16], F32, s_ts); G["b_TS"] = Buf()
            G["TIf"] = P.sb("TIf", [128, 32, 16, 16], F32, s_ts); G["b_TIf"] = Buf()
            phase_peer_scores(P, inp, G)
            phase_peer_out(P, inp, G, out)
        S.finish("sp")
        S.emit()
    return P


def make_in_maps(inputs, n_cores=8):
    consts = _host_consts()
    maps = []
    for b in range(n_cores):
        m = {}
        for name, shape in INPUT_SPECS:
            a = inputs[name]
            if name == "x":
                a = a[b]
            m[name] = np.ascontiguousarray(np.asarray(a, dtype=np.float32).reshape(shape))
        for name, shape, dt in CONST_SPECS:
            m[name] = consts[name]
        maps.append(m)
    return maps


def kernel(**inputs):
    nc = build().nc
    in_maps = make_in_maps(inputs)
    res = run_bass_kernel_spmd(nc, in_maps, core_ids=list(range(8)))
    return np.stack([np.asarray(r["out"]).reshape(SEQ, D) for r in res.results], axis=0).astype(np.float32)
```
